# Optimizing a Trainium2 kernel written in Bass

```python
import math
import jax, jax.numpy as jnp
from jax import lax
import numpy as np

D_MODEL = 1024
BATCH = 8
SEQ = 4096
DEPTH = 2

N_MIXERS = 2
N_SB = (DEPTH + 1) // 2
N_RET = DEPTH // 2
SB_HEADS = 16
SB_HEAD_DIM = D_MODEL // SB_HEADS
SB_BLOCK = 128
RET_HEADS = D_MODEL // 256
RET_KEY_DIM = D_MODEL // RET_HEADS
RET_VAL_DIM = 2 * D_MODEL // RET_HEADS
RET_CHUNK = 128
ROPE_BASE = 10000.0
D_FF = 4 * D_MODEL
PLE_DIM = 256
EPS = 1e-6

kernel_name = "hybrid_stickbreak_retention_trunk"


def rms_norm(x, g):
    xf = x.astype(jnp.float32)
    y = xf * lax.rsqrt(jnp.mean(xf * xf, axis=-1, keepdims=True) + EPS)
    return (y * g.astype(jnp.float32)).astype(x.dtype)


def stick_breaking_attention(h, w_in, w_out):
    B, S, _ = h.shape
    qkv = h @ w_in
    q, k, v = jnp.split(qkv, 3, axis=-1)
    q = q.reshape(B, S, SB_HEADS, SB_HEAD_DIM).astype(jnp.float32)
    k = k.reshape(B, S, SB_HEADS, SB_HEAD_DIM).astype(jnp.float32)
    v = v.reshape(B, S, SB_HEADS, SB_HEAD_DIM).astype(jnp.float32)
    scale = SB_HEAD_DIM ** -0.5
    outs = []
    for blk in range(S // SB_BLOCK):
        start = blk * SB_BLOCK
        end = start + SB_BLOCK
        qb = q[:, start:end]
        kb = k[:, :end]
        vb = v[:, :end]
        z = jnp.einsum('bthd,bshd->bhts', qb, kb) * scale
        t_pos = start + jnp.arange(SB_BLOCK)
        s_pos = jnp.arange(end)
        mask = s_pos[None, :] < t_pos[:, None]
        log_1m_beta = jnp.where(mask, jax.nn.log_sigmoid(-z), 0.0)
        suffix = lax.cumsum(log_1m_beta, axis=3, reverse=True) - log_1m_beta
        a = jnp.where(mask, jnp.exp(jax.nn.log_sigmoid(z) + suffix), 0.0)
        outs.append(jnp.einsum('bhts,bshd->bthd', a, vb))
    o = jnp.concatenate(outs, axis=1).reshape(B, S, D_MODEL).astype(h.dtype)
    return o @ w_out


def apply_rope(x, pos):
    d = x.shape[-1]
    inv_freq = ROPE_BASE ** (-jnp.arange(0, d, 2, dtype=jnp.float32) / d)
    ang = pos.astype(jnp.float32)[:, None] * inv_freq[None, :]
    cos = jnp.cos(ang)[None, :, None, :]
    sin = jnp.sin(ang)[None, :, None, :]
    x1, x2 = jnp.split(x, 2, axis=-1)
    return jnp.concatenate([x1 * cos - x2 * sin, x1 * sin + x2 * cos], axis=-1)


def retention(h, w_in, w_out):
    B, S, _ = h.shape
    proj = h @ w_in
    q, k, v, g = jnp.split(proj, [D_MODEL, 2 * D_MODEL, 4 * D_MODEL], axis=-1)
    pos = jnp.arange(S)
    q = apply_rope(q.reshape(B, S, RET_HEADS, RET_KEY_DIM).astype(jnp.float32), pos)
    k = apply_rope(k.reshape(B, S, RET_HEADS, RET_KEY_DIM).astype(jnp.float32), pos) * (RET_KEY_DIM ** -0.5)
    v = v.reshape(B, S, RET_HEADS, RET_VAL_DIM).astype(jnp.float32)

    n_chunks = S // RET_CHUNK
    def to_chunks(t):
        return t.reshape(B, n_chunks, RET_CHUNK, RET_HEADS, t.shape[-1]).transpose(1, 0, 3, 2, 4)
    qc, kc, vc = to_chunks(q), to_chunks(k), to_chunks(v)

    log_gamma = jnp.log1p(-jnp.exp2(-5.0 - jnp.arange(RET_HEADS, dtype=jnp.float32)))
    idx = jnp.arange(RET_CHUNK, dtype=jnp.float32)
    diff = idx[:, None] - idx[None, :]
    intra_decay = jnp.where(diff >= 0, jnp.exp(log_gamma[:, None, None] * jnp.maximum(diff, 0.0)), 0.0)
    cross_decay = jnp.exp(log_gamma[:, None] * (idx + 1.0))[None, :, :, None]
    kv_decay = jnp.exp(log_gamma[:, None] * (RET_CHUNK - 1.0 - idx))[None, :, :, None]
    chunk_decay = jnp.exp(log_gamma * RET_CHUNK)[None, :, None, None]

    def step(state, inp):
        qb, kb, vb = inp
        scores = jnp.einsum('bhnd,bhmd->bhnm', qb, kb) * intra_decay[None]
        inner = jnp.einsum('bhnm,bhme->bhne', scores, vb)
        cross = jnp.einsum('bhnd,bhde->bhne', qb, state) * cross_decay
        state = state * chunk_decay + jnp.einsum('bhmd,bhme->bhde', kb * kv_decay, vb)
        return state, inner + cross

    state0 = jnp.zeros((B, RET_HEADS, RET_KEY_DIM, RET_VAL_DIM), jnp.float32)
    _, ys = lax.scan(step, state0, (qc, kc, vc))
    y = ys.transpose(1, 0, 3, 2, 4).reshape(B, S, RET_HEADS, RET_VAL_DIM)
    y = y * lax.rsqrt(jnp.mean(y * y, axis=-1, keepdims=True) + EPS)
    y = y.reshape(B, S, RET_HEADS * RET_VAL_DIM)
    y = (jax.nn.silu(g.astype(jnp.float32)) * y).astype(h.dtype)
    return y @ w_out


def sqrelu_mlp(h, w_up, w_down):
    a = jax.nn.relu(h @ w_up)
    return (a * a) @ w_down


def setup_inputs(seed: int = 0) -> dict:
    key = jax.random.key(seed)
    ks = jax.random.split(key, 16)
    f32 = jnp.float32

    def w(k, shape, fan_in):
        return jax.random.normal(k, shape, f32) * (fan_in ** -0.5)

    def gain(k, shape):
        return 1.0 + 0.05 * jax.random.normal(k, shape, f32)

    return {
        "x": jax.random.normal(ks[0], (BATCH, SEQ, D_MODEL), f32),
        "p": jax.random.normal(ks[1], (DEPTH, BATCH, SEQ, PLE_DIM), f32),
        "mix_norm": gain(ks[2], (DEPTH, D_MODEL)),
        "sb_w_in": w(ks[3], (N_SB, D_MODEL, 3 * D_MODEL), D_MODEL),
        "sb_w_out": w(ks[4], (N_SB, D_MODEL, D_MODEL), D_MODEL),
        "ret_w_in": w(ks[5], (N_RET, D_MODEL, 6 * D_MODEL), D_MODEL),
        "ret_w_out": w(ks[6], (N_RET, 2 * D_MODEL, D_MODEL), 2 * D_MODEL),
        "mlp_norm": gain(ks[7], (DEPTH, D_MODEL)),
        "mlp_w_up": w(ks[8], (DEPTH, D_MODEL, D_FF), D_MODEL),
        "mlp_w_down": w(ks[9], (DEPTH, D_FF, D_MODEL), D_FF),
        "ple_norm": gain(ks[10], (DEPTH, D_MODEL)),
        "ple_w_gate": w(ks[11], (DEPTH, D_MODEL, D_MODEL), D_MODEL),
        "ple_w_up": w(ks[12], (DEPTH, PLE_DIM, D_MODEL), PLE_DIM),
        "final_norm": gain(ks[13], (D_MODEL,)),
    }


def reference(x, p, mix_norm, sb_w_in, sb_w_out, ret_w_in, ret_w_out,
              mlp_norm, mlp_w_up, mlp_w_down, ple_norm, ple_w_gate, ple_w_up,
              final_norm):
    h = x
    for i in range(DEPTH):
        hn = rms_norm(h, mix_norm[i])
        if i % N_MIXERS == 0:
            h = h + stick_breaking_attention(hn, sb_w_in[i // N_MIXERS], sb_w_out[i // N_MIXERS])
        else:
            h = h + retention(hn, ret_w_in[i // N_MIXERS], ret_w_out[i // N_MIXERS])
        h = h + sqrelu_mlp(rms_norm(h, mlp_norm[i]), mlp_w_up[i], mlp_w_down[i])
        gate = jax.nn.sigmoid((rms_norm(h, ple_norm[i]) @ ple_w_gate[i]).astype(jnp.float32))
        h = h + ((p[i] @ ple_w_up[i]).astype(jnp.float32) * gate).astype(h.dtype)
    return rms_norm(h, final_norm).astype(x.dtype)
```

```python
import numpy as np
import concourse.bass as bass
import concourse.mybir as mybir
from concourse.bass_utils import run_bass_kernel_spmd

F32 = mybir.dt.float32
BF16 = mybir.dt.bfloat16
ALU = mybir.AluOpType
AF = mybir.ActivationFunctionType
AX = mybir.AxisListType

EPOCH = 30000
SAME_ENGINE_SYNC = True


class Sem:
    _n = 0

    def __init__(self, handle):
        self.h = handle
        Sem._n += 1
        self.id = Sem._n


class Buf:
    __slots__ = ("name", "lw", "rd")

    def __init__(self, name=""):
        self.name = name
        self.lw = None
        self.rd = []


class Eng:
    def __init__(self, ctx, name, is_pe=False):
        self.ctx = ctx
        self.name = name
        self.same_sync = SAME_ENGINE_SYNC and not is_pe
        self.prog = []
        self.sem = ctx.new_sem(name + "_s0")
        self.count = 0
        self.nsem = 1
        self.waited = {}
        self.pending = False
        self.dma_sems = None
        self.dma_cum = None
        self.dma_i = 0

    def _need(self, dep, waits):
        if dep is None:
            return
        sem, val = dep
        if sem is self.sem and not self.same_sync:
            return
        if self.waited.get(sem.id, 0) >= val:
            return
        self.waited[sem.id] = val
        waits[sem.id] = (sem, val)

    def _collect(self, r, w):
        waits = {}
        for b in r:
            self._need(b.lw, waits)
        for b in w:
            self._need(b.lw, waits)
            for d in b.rd:
                self._need(d, waits)
        return list(waits.values())

    def _rotate(self):
        if self.count >= EPOCH:
            self.sem = self.ctx.new_sem("%s_s%d" % (self.name, self.nsem))
            self.nsem += 1
            self.count = 0

    def op(self, fn, r=(), w=(), inc=True):
        waits = self._collect(r, w)
        if inc:
            self._rotate()
            self.count += 1
            sem = self.sem
            dep = (sem, self.count)
            self.pending = False

            def run(eng, fn=fn, waits=waits, sem=sem):
                for s, v in waits:
                    eng.wait_ge(s.h, v)
                fn(eng).then_inc(sem.h, 1)
        else:
            self._rotate()
            dep = (self.sem, self.count + 1)
            self.pending = True

            def run(eng, fn=fn, waits=waits):
                for s, v in waits:
                    eng.wait_ge(s.h, v)
                fn(eng)
        self.prog.append(run)
        for b in w:
            b.lw = dep
            b.rd = []
        for b in r:
            b.rd.append(dep)
        return dep

    def dma(self, out, in_, r=(), w=(), **kw):
        if self.dma_sems is None:
            K = 8
            self.dma_sems = [self.ctx.new_sem("%s_d%d" % (self.name, i)) for i in range(K)]
            self.dma_cum = [0] * K
        slot = self.dma_i % len(self.dma_sems)
        self.dma_i += 1
        sem = self.dma_sems[slot]
        waits = {}
        if self.dma_cum[slot] > 0:
            self._need((sem, self.dma_cum[slot]), waits)
        for b in r:
            self._need(b.lw, waits)
        for b in w:
            self._need(b.lw, waits)
            for d in b.rd:
                self._need(d, waits)
        waits = list(waits.values())
        self.dma_cum[slot] += 16
        dep = (sem, self.dma_cum[slot])

        def run(eng, waits=waits, sem=sem, out=out, in_=in_, kw=kw):
            for s, v in waits:
                eng.wait_ge(s.h, v)
            eng.dma_start(out=out, in_=in_, **kw).then_inc(sem.h, 16)
        self.prog.append(run)
        for b in w:
            b.lw = dep
            b.rd = []
        for b in r:
            b.rd.append(dep)
        return dep

    def wait_dep(self, dep):
        waits = {}
        self._need(dep, waits)
        waits = list(waits.values())
        if waits:
            def run(eng, waits=waits):
                for s, v in waits:
                    eng.wait_ge(s.h, v)
            self.prog.append(run)

    def cur(self):
        assert not self.pending, self.name
        deps = [(self.sem, self.count)] if self.count > 0 else []
        if self.dma_sems is not None:
            for s, c in zip(self.dma_sems, self.dma_cum):
                if c > 0:
                    deps.append((s, c))
        return deps


class Ctx:
    def __init__(self, nc, stack, sbuf_bytes=190 * 1024):
        self.nc = nc
        self.stack = stack
        self.nsems = 0
        self.PE = Eng(self, "pe", is_pe=True)
        self.ACT = Eng(self, "act")
        self.DVE = Eng(self, "dve")
        self.POOL = Eng(self, "pool")
        self.SP = Eng(self, "sp")
        self.engs = [self.PE, self.ACT, self.DVE, self.POOL, self.SP]
        self.arena_words = sbuf_bytes // 4
        self.arena = stack.enter_context(nc.sbuf_tensor("arena", [128, self.arena_words], F32))
        self.bump = 0
        self.psum = [stack.enter_context(nc.psum_tensor("ps%d" % i, [128, 512], F32)) for i in range(8)]

    def new_sem(self, name):
        self.nsems += 1
        return Sem(self.stack.enter_context(self.nc.semaphore(name)))

    def alloc(self, shape, dtype, name=""):
        esz = 4 if dtype == F32 else 2
        n = int(np.prod(shape[1:]))
        words = (n * esz + 3) // 4
        words = (words + 7) // 8 * 8
        assert self.bump + words <= self.arena_words, ("SBUF overflow", name, self.bump, words)
        ap = self.arena[0:shape[0], self.bump:self.bump + words]
        self.bump += words
        if dtype != F32:
            ap = ap.bitcast(dtype)
        ap = ap[:, 0:n]
        if len(shape) == 3:
            ap = ap.rearrange("p (a b) -> p a b", a=shape[1])
        elif len(shape) == 4:
            ap = ap.rearrange("p (a b c) -> p a b c", a=shape[1], b=shape[2])
        return ap

    def mark(self):
        return self.bump

    def release(self, mark):
        self.bump = mark

    def barrier(self):
        deps = []
        for e in self.engs:
            deps += e.cur()
        for e in self.engs:
            for d in deps:
                e.wait_dep(d)

    def emit(self):
        nc = self.nc
        with nc.Block() as block:
            @block.tensor
            def _(eng):
                for f in self.PE.prog:
                    f(eng)

            @block.scalar
            def _(eng):
                for f in self.ACT.prog:
                    f(eng)

            @block.vector
            def _(eng):
                for f in self.DVE.prog:
                    f(eng)

            @block.gpsimd
            def _(eng):
                for f in self.POOL.prog:
                    f(eng)

            @block.sync
            def _(eng):
                for f in self.SP.prog:
                    f(eng)


EPS = 1e-6
D = 1024


class NormTmp:
    def __init__(self, c, nbuf=2):
        self.n = nbuf
        self.i = 0
        self.junk = [c.alloc([128, D], BF16) for _ in range(nbuf)]
        self.ss = [c.alloc([128, 1], F32) for _ in range(nbuf)]
        self.rstd = [c.alloc([128, 1], F32) for _ in range(nbuf)]
        self.hn = [c.alloc([128, D], BF16) for _ in range(nbuf)]
        self.b = [[Buf() for _ in range(4)] for _ in range(nbuf)]


def norm_T(c, xt, bx, g, bg, ident, bid, out3, bout, tmp, tp, btp):
    PE, ACT, DVE = c.PE, c.ACT, c.DVE
    i = tmp.i % tmp.n
    tmp.i += 1
    junk, ss, rstd, hn = tmp.junk[i], tmp.ss[i], tmp.rstd[i], tmp.hn[i]
    bj, bss, brs, bhn = tmp.b[i]
    ACT.op(lambda e: e.activation(junk, xt, AF.Square, scale=1.0 / 32.0, accum_out=ss), r=[bx], w=[bj, bss])
    ACT.op(lambda e: e.activation(rstd, ss, AF.Ln, bias=EPS), r=[bss], w=[brs])
    ACT.op(lambda e: e.activation(rstd, rstd, AF.Exp, scale=-0.5), r=[brs], w=[brs])
    DVE.op(lambda e: e.scalar_tensor_tensor(hn, xt, rstd, g, ALU.mult, ALU.mult), r=[bx, brs, bg], w=[bhn])
    for k in range(8):
        PE.op(lambda e, k=k: e.transpose(tp[:, k * 128:(k + 1) * 128], hn[:, k * 128:(k + 1) * 128], ident),
              r=[bhn, bid], w=[btp], inc=(k == 7))
    ACT.op(lambda e: e.copy(out3, tp.rearrange("p (k t) -> p k t", k=8)), r=[btp], w=[bout])


def phase1(c, S, x, mixg, w_in, ident_d, qkT_d, v_d):
    PE, ACT, DVE, POOL, SP = c.PE, c.ACT, c.DVE, c.POOL, c.SP
    mk = c.mark()
    NT = S // 512
    W = c.alloc([128, 8, 3072], BF16)
    bW = [Buf() for _ in range(8)]
    for k in range(8):
        POOL.dma(W[:, k, :], w_in[k * 128:(k + 1) * 128, :], w=[bW[k]])
    g = c.alloc([128, D], F32)
    bg = Buf()
    SP.dma(g, mixg.partition_broadcast(128), w=[bg])
    ident = c.alloc([128, 128], BF16)
    bid = Buf()
    SP.dma(ident, ident_d, w=[bid])
    tmp = NormTmp(c)
    xt = [c.alloc([128, D], F32) for _ in range(2)]
    bxt = [Buf() for _ in range(2)]
    hnT = [c.alloc([128, 8, 512], BF16) for _ in range(2)]
    bhnT = [[Buf() for _ in range(4)] for _ in range(2)]
    qk_st = [c.alloc([128, 16, 512], BF16) for _ in range(2)]
    bqk = [[Buf() for _ in range(16)] for _ in range(2)]
    v_st = [c.alloc([128, 4, D], BF16) for _ in range(2)]
    bv = [[Buf() for _ in range(8)] for _ in range(2)]
    tps = [c.psum[0][:, :].bitcast(BF16), c.psum[1][:, :].bitcast(BF16)]
    btps = [Buf(), Buf()]
    acc = [c.psum[2 + i][:, :] for i in range(4)]
    bacc = [Buf() for _ in range(4)]
    ai = 0
    xi = 0
    for tt in range(NT):
        p = tt % 2
        for tb in range(4):
            j = xi % 2
            xi += 1
            r0 = tt * 512 + tb * 128
            SP.dma(xt[j], x[r0:r0 + 128, :], w=[bxt[j]])
            norm_T(c, xt[j], bxt[j], g, bg, ident, bid, hnT[p][:, :, tb * 128:(tb + 1) * 128], bhnT[p][tb],
                   tmp, tps[j], btps[j])
        for oc in range(16):
            a = ai % 4
            ai += 1
            for k in range(8):
                PE.op(lambda e, a=a, k=k, oc=oc, p=p: e.matmul(acc[a], W[:, k, oc * 128:(oc + 1) * 128], hnT[p][:, k, :],
                                                               start=(k == 0), stop=(k == 7)),
                      r=[bW[k]] + bhnT[p], w=[bacc[a]], inc=(k == 7))
            sc = 0.125 if oc < 8 else 1.0
            if oc % 2 == 0:
                ACT.op(lambda e, a=a, oc=oc, p=p, sc=sc: e.mul(qk_st[p][:, oc, :], acc[a], sc), r=[bacc[a]], w=[bqk[p][oc]])
            else:
                DVE.op(lambda e, a=a, oc=oc, p=p, sc=sc: e.tensor_scalar(qk_st[p][:, oc, :], acc[a], sc, None, ALU.mult),
                       r=[bacc[a]], w=[bqk[p][oc]])
        SP.dma(qkT_d[:, :, tt * 512:(tt + 1) * 512].rearrange("c p t -> p c t"), qk_st[p], r=bqk[p])
        for tb in range(4):
            for hf in range(2):
                a = ai % 4
                ai += 1
                for k in range(8):
                    PE.op(lambda e, a=a, k=k, tb=tb, hf=hf, p=p: e.matmul(
                        acc[a], hnT[p][:, k, tb * 128:(tb + 1) * 128], W[:, k, 2048 + hf * 512:2048 + (hf + 1) * 512],
                        start=(k == 0), stop=(k == 7)),
                        r=[bW[k], bhnT[p][tb]], w=[bacc[a]], inc=(k == 7))
                if hf == 0:
                    ACT.op(lambda e, a=a, tb=tb, hf=hf, p=p: e.copy(v_st[p][:, tb, hf * 512:(hf + 1) * 512], acc[a]),
                           r=[bacc[a]], w=[bv[p][tb * 2 + hf]])
                else:
                    DVE.op(lambda e, a=a, tb=tb, hf=hf, p=p: e.tensor_copy(v_st[p][:, tb, hf * 512:(hf + 1) * 512], acc[a]),
                           r=[bacc[a]], w=[bv[p][tb * 2 + hf]])
        SP.dma(v_d[tt * 4:(tt + 1) * 4, :, :].rearrange("b p f -> p b f"), v_st[p], r=bv[p])
    c.barrier()
    c.release(mk)


def phase2(c, S, qkT_d, v_d, oT_d, ident_d, negtri_d, negones_d, maskb_d, heads=8):
    PE, ACT, DVE, POOL, SP = c.PE, c.ACT, c.DVE, c.POOL, c.SP
    mk = c.mark()
    NQ = S // 512
    NB = S // 128
    ident = c.alloc([128, 128], BF16)
    negtri = c.alloc([128, 128], BF16)
    negones = c.alloc([128, 128], BF16)
    maskb = c.alloc([128, 4, 512], BF16)
    bcst = Buf()
    SP.dma(ident, ident_d, w=[bcst])
    SP.dma(negtri, negtri_d, w=[bcst])
    SP.dma(negones, negones_d, w=[bcst])
    SP.dma(maskb, maskb_d, w=[bcst])
    q2 = [c.alloc([128, S], BF16) for _ in range(2)]
    k2 = [c.alloc([128, S], BF16) for _ in range(2)]
    v2 = [c.alloc([128, NB, 2, 128], BF16) for _ in range(2)]
    bq2 = [Buf() for _ in range(2)]
    bk2 = [Buf() for _ in range(2)]
    bv2 = [Buf() for _ in range(2)]
    for p in range(2):
        POOL.op(lambda e, p=p: e.memset(v2[p], 0.0), w=[bv2[p]])
    oT_st = [c.alloc([128, S], BF16) for _ in range(2)]
    boT = [Buf() for _ in range(2)]
    NE1, NM, NA = 3, 4, 3
    e1 = [c.alloc([128, 512], F32) for _ in range(NE1)]
    be1 = [Buf() for _ in range(NE1)]
    mm = [c.alloc([128, 512], BF16) for _ in range(NM)]
    bm = [Buf() for _ in range(NM)]
    aT = [c.alloc([128, 512], BF16) for _ in range(NA)]
    baT = [Buf() for _ in range(NA)]
    Sf = [c.alloc([128, 512], F32) for _ in range(2)]
    bSf = [Buf() for _ in range(2)]
    Sb = [[c.alloc([128, 512], BF16) for _ in range(2)] for _ in range(2)]
    bSb = [[Buf() for _ in range(2)] for _ in range(2)]
    NAB = 6
    A = [c.psum[i][:, :] for i in range(NAB)]
    bA = [Buf() for _ in range(NAB)]
    O = [c.psum[6 + i][:, :] for i in range(2)]
    bO = [Buf() for _ in range(2)]

    for hp in range(heads):
        p = hp % 2
        SP.dma(q2[p], qkT_d[hp], w=[bq2[p]])
        SP.dma(k2[p], qkT_d[8 + hp], w=[bk2[p]])
        for e in range(2):
            SP.dma(v2[p][:, :, e, e * 64:(e + 1) * 64],
                   v_d[:, :, hp * 128 + e * 64: hp * 128 + (e + 1) * 64].rearrange("b p f -> p b f"), w=[bv2[p]])
        items = []
        for qt in range(NQ):
            top = 4 * qt + 3
            for kb in range(top, -1, -1):
                for e in range(2):
                    items.append((qt, kb, e))
        N = len(items)

        def S1(n, p=p, items=items):
            qt, kb, e = items[n]
            a = n % NAB
            diag = kb >= 4 * qt
            PE.op(lambda en: en.matmul(A[a], k2[p][e * 64:(e + 1) * 64, kb * 128:(kb + 1) * 128],
                                       q2[p][e * 64:(e + 1) * 64, qt * 512:(qt + 1) * 512],
                                       start=True, stop=False, skip_group_check=True),
                  r=[bq2[p], bk2[p]], w=[bA[a]], inc=not diag)
            if diag:
                PE.op(lambda en: en.matmul(A[a], ident, maskb[:, kb - 4 * qt, :], start=False, stop=False,
                                           skip_group_check=True),
                      r=[bcst], w=[bA[a]], inc=True)

        def S2(n, p=p, items=items):
            a = n % NAB
            ACT.op(lambda en: en.activation(e1[n % NE1], A[a], AF.Exp), r=[bA[a]], w=[be1[n % NE1]])
            ACT.op(lambda en: en.activation(mm[n % NM], e1[n % NE1], AF.Ln, bias=1.0), r=[be1[n % NE1]], w=[bm[n % NM]])

        def S3(n, p=p, items=items):
            qt, kb, e = items[n]
            a = n % NAB
            first = kb == 4 * qt + 3
            PE.op(lambda en: en.matmul(A[a], negtri, mm[n % NM], start=False, stop=first, skip_group_check=True),
                  r=[bcst, bm[n % NM]], w=[bA[a]], inc=first)
            if not first:
                j = (kb + 1) % 2
                PE.op(lambda en: en.matmul(A[a], negones, Sb[e][j], start=False, stop=True, skip_group_check=True),
                      r=[bcst, bSb[e][j]], w=[bA[a]], inc=True)

        def S4(n, p=p, items=items):
            qt, kb, e = items[n]
            first = kb == 4 * qt + 3
            if kb == 0:
                return
            if first:
                POOL.op(lambda en: en.tensor_copy(Sf[e], mm[n % NM]), r=[bm[n % NM]], w=[bSf[e]])
            else:
                POOL.op(lambda en: en.tensor_tensor(Sf[e], Sf[e], mm[n % NM], ALU.add), r=[bm[n % NM], bSf[e]], w=[bSf[e]])
            j = kb % 2
            DVE.op(lambda en: en.tensor_copy(Sb[e][j], Sf[e]), r=[bSf[e]], w=[bSb[e][j]])

        def S5(n, p=p, items=items):
            a = n % NAB
            ACT.op(lambda en: en.activation(aT[n % NA], A[a], AF.Exp), r=[bA[a]], w=[baT[n % NA]])

        def S6(n, p=p, items=items):
            qt, kb, e = items[n]
            o = qt % 2
            first = (kb == 4 * qt + 3) and e == 0
            last = kb == 0 and e == 1
            PE.op(lambda en: en.matmul(O[o], v2[p][:, kb, e, :], aT[n % NA], start=first, stop=last,
                                       skip_group_check=True),
                  r=[bv2[p], baT[n % NA]], w=[bO[o]], inc=last)
            if last:
                DVE.op(lambda en: en.tensor_copy(oT_st[p][:, qt * 512:(qt + 1) * 512], O[o]), r=[bO[o]], w=[boT[p]])

        for n in range(N + 4):
            if n < N:
                S1(n)
            if 0 <= n - 1 < N:
                S2(n - 1)
            if 0 <= n - 2 < N:
                S3(n - 2)
                S4(n - 2)
            if 0 <= n - 3 < N:
                S5(n - 3)
            if 0 <= n - 4 < N:
                S6(n - 4)
        SP.dma(oT_d[hp], oT_st[p], r=[boT[p]])
    c.barrier()
    c.release(mk)


def rstd_of(c, xt, bx, junk, bj, ss, bss, rstd, brs):
    ACT = c.ACT
    ACT.op(lambda e: e.activation(junk, xt, AF.Square, scale=1.0 / 32.0, accum_out=ss), r=[bx], w=[bj, bss])
    ACT.op(lambda e: e.activation(rstd, ss, AF.Ln, bias=EPS), r=[bss], w=[brs])
    ACT.op(lambda e: e.activation(rstd, rstd, AF.Exp, scale=-0.5), r=[brs], w=[brs])


def phase_post(c, S, h_in, h_out, oT_d, KO, wb_out, g_mlp, wb_up, wb_down, g_ple, wb_gate, p_d, wb_pu, ident_d,
               g_final=None):
    PE, ACT, DVE, POOL, SP = c.PE, c.ACT, c.DVE, c.POOL, c.SP
    mk = c.mark()
    NT = S // 512
    ident = c.alloc([128, 128], BF16)
    bid = Buf()
    POOL.dma(ident, ident_d, w=[bid])
    gm = c.alloc([128, D], F32)
    gp = c.alloc([128, D], F32)
    bgm, bgp, bgf = Buf(), Buf(), Buf()
    POOL.dma(gm, g_mlp.partition_broadcast(128), w=[bgm])
    POOL.dma(gp, g_ple.partition_broadcast(128), w=[bgp])
    if g_final is not None:
        gf = c.alloc([128, D], F32)
        POOL.dma(gf, g_final.partition_broadcast(128), w=[bgf])
    tmp = NormTmp(c)
    NW = 3
    wr = [c.alloc([128, 8, 1024], BF16) for _ in range(NW)]
    bwr = [Buf() for _ in range(NW)]
    wi = [0]

    def wload(src3):
        s = wi[0] % NW
        wi[0] += 1
        nk = src3.shape[1]
        SP.dma(wr[s][:, 0:nk, :], src3, w=[bwr[s]])
        return wr[s], bwr[s]

    ht = [c.alloc([128, D], F32) for _ in range(4)]
    bht = [Buf() for _ in range(4)]
    oTt = c.alloc([128, KO, 512], BF16)
    boTt = [Buf() for _ in range(KO // 8)]
    hnT = c.alloc([128, 8, 512], BF16)
    bhnT = [Buf() for _ in range(4)]
    aT = c.alloc([128, 32, 512], BF16)
    baT = [Buf() for _ in range(32)]
    rl = [c.alloc([128, 512], F32) for _ in range(2)]
    brl = [Buf() for _ in range(2)]
    gs = [c.alloc([128, 512], F32) for _ in range(2)]
    bgs = [Buf() for _ in range(2)]
    pt = [c.alloc([128, 256], F32) for _ in range(2)]
    bpt = [Buf() for _ in range(2)]
    pb = [c.alloc([128, 256], BF16) for _ in range(2)]
    bpb = [Buf() for _ in range(2)]
    pT = [c.alloc([128, 2, 128], BF16) for _ in range(2)]
    bpT = [Buf() for _ in range(2)]
    yo = [c.alloc([128, D], F32) for _ in range(2)]
    byo = [Buf() for _ in range(2)]
    tps = [c.psum[0][:, :].bitcast(BF16), c.psum[1][:, :].bitcast(BF16)]
    btps = [Buf(), Buf()]
    acc = [c.psum[2 + i][:, :] for i in range(6)]
    bacc = [Buf() for _ in range(6)]
    cnt = {"a": 0, "n": 0, "r": 0, "g": 0, "p": 0, "y": 0}

    def nxt(key, n):
        v = cnt[key] % n
        cnt[key] += 1
        return v

    def tok_mm(lhs3, blhs, nk, w3, bw, add):
        for tb in range(4):
            for hf in range(2):
                a = nxt("a", 6)
                for k in range(nk):
                    PE.op(lambda e, a=a, k=k, tb=tb, hf=hf: e.matmul(
                        acc[a], lhs3[:, k, tb * 128:(tb + 1) * 128], w3[:, k, hf * 512:(hf + 1) * 512],
                        start=(k == 0), stop=(k == nk - 1)),
                        r=[bw] + blhs(tb), w=[bacc[a]], inc=(k == nk - 1))
                add(tb, hf, a)

    def resid_add(tb, hf, a):
        DVE.op(lambda e: e.tensor_tensor(ht[tb][:, hf * 512:(hf + 1) * 512], ht[tb][:, hf * 512:(hf + 1) * 512],
                                         acc[a], ALU.add), r=[bacc[a], bht[tb]], w=[bht[tb]])

    for tt in range(NT):
        t0 = tt * 512
        for g in range(KO // 8):
            POOL.dma(oTt[:, g * 8:(g + 1) * 8, :], oT_d[g * 8:(g + 1) * 8, :, t0:t0 + 512].rearrange("c p t -> p c t"),
                     w=[boTt[g]])
        for tb in range(4):
            POOL.dma(ht[tb], h_in[t0 + tb * 128:t0 + (tb + 1) * 128, :], w=[bht[tb]])
        for g in range(KO // 8):
            w3, bw = wload(wb_out[g * 1024:(g + 1) * 1024, :].rearrange("(k p) c -> p k c", p=128))
            tok_mm(oTt[:, g * 8:(g + 1) * 8, :], lambda tb, g=g: [boTt[g]], 8, w3, bw, resid_add)
        for tb in range(4):
            j = nxt("n", 2)
            norm_T(c, ht[tb], bht[tb], gm, bgm, ident, bid, hnT[:, :, tb * 128:(tb + 1) * 128], bhnT[tb], tmp,
                   tps[j], btps[j])
        for cg in range(4):
            w3, bw = wload(wb_up[:, cg * 1024:(cg + 1) * 1024].rearrange("(k p) c -> p k c", p=128))
            for oc in range(8):
                a = nxt("a", 6)
                for k in range(8):
                    PE.op(lambda e, a=a, k=k, oc=oc, w3=w3: e.matmul(acc[a], w3[:, k, oc * 128:(oc + 1) * 128], hnT[:, k, :],
                                                                    start=(k == 0), stop=(k == 7)),
                          r=[bw] + bhnT, w=[bacc[a]], inc=(k == 7))
                r_ = nxt("r", 2)
                ACT.op(lambda e, a=a, r_=r_: e.activation(rl[r_], acc[a], AF.Relu), r=[bacc[a]], w=[brl[r_]])
                ff = cg * 8 + oc
                DVE.op(lambda e, r_=r_, ff=ff: e.tensor_tensor(aT[:, ff, :], rl[r_], rl[r_], ALU.mult),
                       r=[brl[r_]], w=[baT[ff]])
        for g in range(4):
            w3, bw = wload(wb_down[g * 1024:(g + 1) * 1024, :].rearrange("(k p) c -> p k c", p=128))
            tok_mm(aT[:, g * 8:(g + 1) * 8, :], lambda tb, g=g: baT[g * 8:(g + 1) * 8], 8, w3, bw, resid_add)
        for tb in range(4):
            j = nxt("n", 2)
            norm_T(c, ht[tb], bht[tb], gp, bgp, ident, bid, hnT[:, :, tb * 128:(tb + 1) * 128], bhnT[tb], tmp,
                   tps[j], btps[j])
        wg3, bwg = wload(wb_gate.rearrange("(k p) c -> p k c", p=128))
        wp3, bwp = wload(wb_pu.rearrange("(k p) c -> p k c", p=128))
        for tb in range(4):
            pi = nxt("p", 2)
            POOL.dma(pt[pi], p_d[t0 + tb * 128:t0 + (tb + 1) * 128, :], w=[bpt[pi]])
            DVE.op(lambda e, pi=pi: e.tensor_copy(pb[pi], pt[pi]), r=[bpt[pi]], w=[bpb[pi]])
            j = nxt("n", 2)
            for k in range(2):
                PE.op(lambda e, k=k, pi=pi, j=j: e.transpose(tps[j][:, k * 128:(k + 1) * 128], pb[pi][:, k * 128:(k + 1) * 128], ident),
                      r=[bpb[pi], bid], w=[btps[j]], inc=(k == 1))
            ACT.op(lambda e, pi=pi, j=j: e.copy(pT[pi], tps[j][:, 0:256].rearrange("p (k t) -> p k t", k=2)),
                   r=[btps[j]], w=[bpT[pi]])
            for hf in range(2):
                a = nxt("a", 6)
                for k in range(8):
                    PE.op(lambda e, a=a, k=k, tb=tb, hf=hf, wg3=wg3: e.matmul(
                        acc[a], hnT[:, k, tb * 128:(tb + 1) * 128], wg3[:, k, hf * 512:(hf + 1) * 512],
                        start=(k == 0), stop=(k == 7)), r=[bwg, bhnT[tb]], w=[bacc[a]], inc=(k == 7))
                gi = nxt("g", 2)
                ACT.op(lambda e, a=a, gi=gi: e.activation(gs[gi], acc[a], AF.Sigmoid), r=[bacc[a]], w=[bgs[gi]])
                a2 = nxt("a", 6)
                for k in range(2):
                    PE.op(lambda e, a2=a2, k=k, pi=pi, hf=hf, wp3=wp3: e.matmul(
                        acc[a2], pT[pi][:, k, :], wp3[:, k, hf * 512:(hf + 1) * 512],
                        start=(k == 0), stop=(k == 1)), r=[bwp, bpT[pi]], w=[bacc[a2]], inc=(k == 1))
                DVE.op(lambda e, a2=a2, gi=gi: e.tensor_tensor(gs[gi], gs[gi], acc[a2], ALU.mult),
                       r=[bacc[a2], bgs[gi]], w=[bgs[gi]])
                DVE.op(lambda e, gi=gi, tb=tb, hf=hf: e.tensor_tensor(
                    ht[tb][:, hf * 512:(hf + 1) * 512], ht[tb][:, hf * 512:(hf + 1) * 512], gs[gi], ALU.add),
                    r=[bgs[gi], bht[tb]], w=[bht[tb]])
            if g_final is None:
                POOL.dma(h_out[t0 + tb * 128:t0 + (tb + 1) * 128, :], ht[tb], r=[bht[tb]])
            else:
                i = tmp.i % tmp.n
                tmp.i += 1
                bj, bss, brs, _ = tmp.b[i]
                rstd_of(c, ht[tb], bht[tb], tmp.junk[i], bj, tmp.ss[i], bss, tmp.rstd[i], brs)
                yi = nxt("y", 2)
                DVE.op(lambda e, i=i, yi=yi, tb=tb: e.scalar_tensor_tensor(yo[yi], ht[tb], tmp.rstd[i], gf, ALU.mult, ALU.mult),
                       r=[bht[tb], brs, bgf], w=[byo[yi]])
                POOL.dma(h_out[t0 + tb * 128:t0 + (tb + 1) * 128, :], yo[yi], r=[byo[yi]])
    c.barrier()
    c.release(mk)


def phase_retpre(c, S, h_in, g_mix, wb_in, ident_d, cos_d, sin_d, dqk_d, qT_d, kT_d, ktok_d, v_d, gs_d):
    PE, ACT, DVE, POOL, SP = c.PE, c.ACT, c.DVE, c.POOL, c.SP
    mk = c.mark()
    NT = S // 512
    ident = c.alloc([128, 128], BF16)
    bid = Buf()
    POOL.dma(ident, ident_d, w=[bid])
    gm = c.alloc([128, D], F32)
    bgm = Buf()
    POOL.dma(gm, g_mix.partition_broadcast(128), w=[bgm])
    dqk = c.alloc([128, 2, 4, 512], F32)
    bdqk = Buf()
    POOL.dma(dqk[:, 0], dqk_d[:, 0], w=[bdqk])
    POOL.dma(dqk[:, 1], dqk_d[:, 1], w=[bdqk])
    tmp = NormTmp(c)
    NW = 3
    wr = [c.alloc([128, 8, 1024], BF16) for _ in range(NW)]
    bwr = [Buf() for _ in range(NW)]
    wi = [0]

    def wload(src3):
        s = wi[0] % NW
        wi[0] += 1
        SP.dma(wr[s], src3, w=[bwr[s]])
        return wr[s], bwr[s]

    ht = [c.alloc([128, D], F32) for _ in range(2)]
    bht = [Buf() for _ in range(2)]
    hnT = c.alloc([128, 8, 512], BF16)
    bhnT = [Buf() for _ in range(4)]
    cs = [c.alloc([128, 2, 512], F32) for _ in range(2)]
    bcs = [Buf() for _ in range(2)]
    x12 = [c.alloc([128, 2, 512], F32) for _ in range(2)]
    bx12 = [[Buf(), Buf()] for _ in range(2)]
    tA = [c.alloc([128, 3, 512], F32) for _ in range(2)]
    btA = [Buf() for _ in range(2)]
    tB = [c.alloc([128, 3, 512], F32) for _ in range(2)]
    btB = [Buf() for _ in range(2)]
    qk_st = [c.alloc([128, 8, 512], BF16) for _ in range(2)]
    bqk = [[Buf() for _ in range(8)] for _ in range(2)]
    ktok_st = c.alloc([128, 4, 1024], BF16)
    bktok = [Buf() for _ in range(4)]
    vg_st = [c.alloc([128, 4, 2048], BF16) for _ in range(2)]
    bvg = [[Buf() for _ in range(16)] for _ in range(2)]
    tps = [c.psum[0][:, :].bitcast(BF16), c.psum[1][:, :].bitcast(BF16)]
    btps = [Buf(), Buf()]
    acc = [c.psum[2 + i][:, :] for i in range(6)]
    bacc = [Buf() for _ in range(6)]
    cnt = {"a": 0, "n": 0, "x": 0, "h": 0}

    def nxt(key, n):
        v = cnt[key] % n
        cnt[key] += 1
        return v

    for tt in range(NT):
        t0 = tt * 512
        ci = tt % 2
        POOL.dma(cs[ci][:, 0, :], cos_d[:, t0:t0 + 512], w=[bcs[ci]])
        POOL.dma(cs[ci][:, 1, :], sin_d[:, t0:t0 + 512], w=[bcs[ci]])
        for tb in range(4):
            hi = nxt("h", 2)
            POOL.dma(ht[hi], h_in[t0 + tb * 128:t0 + (tb + 1) * 128, :], w=[bht[hi]])
            j = nxt("n", 2)
            norm_T(c, ht[hi], bht[hi], gm, bgm, ident, bid, hnT[:, :, tb * 128:(tb + 1) * 128], bhnT[tb], tmp,
                   tps[j], btps[j])
        cos_t = cs[ci][:, 0, :]
        sin_t = cs[ci][:, 1, :]
        for which in range(2):
            w3, bw = wload(wb_in[:, which * 1024:(which + 1) * 1024].rearrange("(k p) c -> p k c", p=128))
            st = qk_st[which]
            for h in range(4):
                xi = nxt("x", 2)
                for half in range(2):
                    oc = 2 * h + half
                    a = nxt("a", 6)
                    for k in range(8):
                        PE.op(lambda e, a=a, k=k, oc=oc, w3=w3: e.matmul(acc[a], w3[:, k, oc * 128:(oc + 1) * 128], hnT[:, k, :],
                                                                        start=(k == 0), stop=(k == 7)),
                              r=[bw] + bhnT, w=[bacc[a]], inc=(k == 7))
                    ACT.op(lambda e, a=a, xi=xi, half=half: e.copy(x12[xi][:, half, :], acc[a]),
                           r=[bacc[a]], w=[bx12[xi][half]])
                x1 = x12[xi][:, 0, :]
                x2 = x12[xi][:, 1, :]
                dtab = dqk[:, which, h, :]
                ta, tb_ = tA[xi], tB[xi]
                rd = [bx12[xi][0], bx12[xi][1], bcs[ci], bdqk]
                DVE.op(lambda e, ta=ta, x1=x1, cos_t=cos_t: e.tensor_tensor(ta[:, 0, :], x1, cos_t, ALU.mult), r=rd, w=[btA[xi]])
                DVE.op(lambda e, ta=ta, x2=x2, sin_t=sin_t: e.tensor_tensor(ta[:, 1, :], x2, sin_t, ALU.mult), r=rd, w=[btA[xi]])
                DVE.op(lambda e, ta=ta: e.tensor_tensor(ta[:, 2, :], ta[:, 0, :], ta[:, 1, :], ALU.subtract), r=[btA[xi]], w=[btA[xi]])
                DVE.op(lambda e, ta=ta, st=st, h=h, dtab=dtab: e.tensor_tensor(st[:, 2 * h, :], ta[:, 2, :], dtab, ALU.mult),
                       r=[btA[xi], bdqk], w=[bqk[which][2 * h]])
                POOL.op(lambda e, tb_=tb_, x1=x1, sin_t=sin_t: e.tensor_tensor(tb_[:, 0, :], x1, sin_t, ALU.mult), r=rd, w=[btB[xi]])
                POOL.op(lambda e, tb_=tb_, x2=x2, cos_t=cos_t: e.tensor_tensor(tb_[:, 1, :], x2, cos_t, ALU.mult), r=rd, w=[btB[xi]])
                POOL.op(lambda e, tb_=tb_: e.tensor_tensor(tb_[:, 2, :], tb_[:, 0, :], tb_[:, 1, :], ALU.add), r=[btB[xi]], w=[btB[xi]])
                POOL.op(lambda e, tb_=tb_, st=st, h=h, dtab=dtab: e.tensor_tensor(st[:, 2 * h + 1, :], tb_[:, 2, :], dtab, ALU.mult),
                        r=[btB[xi], bdqk], w=[bqk[which][2 * h + 1]])
            dst = qT_d if which == 0 else kT_d
            POOL.dma(dst[:, :, t0:t0 + 512].rearrange("c p t -> p c t"), st, r=bqk[which])
            if which == 1:
                for tb in range(4):
                    j = nxt("n", 2)
                    for k in range(8):
                        PE.op(lambda e, k=k, tb=tb, j=j, st=st: e.transpose(tps[j][:, k * 128:(k + 1) * 128],
                                                                     st[:, k, tb * 128:(tb + 1) * 128], ident),
                              r=[bqk[1][k], bid], w=[btps[j]], inc=(k == 7))
                    ACT.op(lambda e, tb=tb, j=j: e.copy(ktok_st[:, tb, :], tps[j]), r=[btps[j]], w=[bktok[tb]])
                POOL.dma(ktok_d[tt * 4:(tt + 1) * 4, :, :].rearrange("b p f -> p b f"), ktok_st, r=bktok)
        for which in range(2):
            st = vg_st[which]
            for g in range(2):
                c0 = 2048 + which * 2048 + g * 1024
                w3, bw = wload(wb_in[:, c0:c0 + 1024].rearrange("(k p) c -> p k c", p=128))
                for tb in range(4):
                    for hf in range(2):
                        a = nxt("a", 6)
                        for k in range(8):
                            PE.op(lambda e, a=a, k=k, tb=tb, hf=hf, w3=w3: e.matmul(
                                acc[a], hnT[:, k, tb * 128:(tb + 1) * 128], w3[:, k, hf * 512:(hf + 1) * 512],
                                start=(k == 0), stop=(k == 7)), r=[bw, bhnT[tb]], w=[bacc[a]], inc=(k == 7))
                        col = g * 1024 + hf * 512
                        bi = tb * 4 + g * 2 + hf
                        if which == 1:
                            ACT.op(lambda e, a=a, tb=tb, col=col, st=st: e.activation(st[:, tb, col:col + 512], acc[a], AF.Silu),
                                   r=[bacc[a]], w=[bvg[which][bi]])
                        else:
                            DVE.op(lambda e, a=a, tb=tb, col=col, st=st: e.tensor_copy(st[:, tb, col:col + 512], acc[a]),
                                   r=[bacc[a]], w=[bvg[which][bi]])
            dst = v_d if which == 0 else gs_d
            for tb in range(4):
                POOL.dma(dst[tt * 4 + tb], st[:, tb, :], r=bvg[which][tb * 4:(tb + 1) * 4])
    c.barrier()
    c.release(mk)


def phase_ret(c, S, qT_d, kT_d, ktok_d, v_d, gs_d, ident_d, cmask_d, yT_d, g128):
    PE, ACT, DVE, POOL, SP = c.PE, c.ACT, c.DVE, c.POOL, c.SP
    mk = c.mark()
    NC = S // 128
    ident = c.alloc([128, 128], BF16)
    cmask = c.alloc([128, 128], F32)
    bcst = Buf()
    SP.dma(ident, ident_d, w=[bcst])
    SP.dma(cmask, cmask_d, w=[bcst])
    NB_ = 3
    qTc = [c.alloc([128, 8, 128], BF16) for _ in range(NB_)]
    kTc = [c.alloc([128, 8, 128], BF16) for _ in range(NB_)]
    ktc = [c.alloc([128, 1024], BF16) for _ in range(NB_)]
    vc = [c.alloc([128, 2048], BF16) for _ in range(NB_)]
    gc = [c.alloc([128, 2048], BF16) for _ in range(NB_)]
    bin_ = [[Buf() for _ in range(5)] for _ in range(NB_)]
    U = c.alloc([128, 4, 2, 512], F32)
    bU = [[Buf(), Buf()] for _ in range(4)]
    Tb = [c.alloc([128, 4, 2, 512], BF16) for _ in range(2)]
    bTb = [[[Buf(), Buf()] for _ in range(4)] for _ in range(2)]
    sT = [c.alloc([128, 128], BF16) for _ in range(4)]
    bsT = [Buf() for _ in range(4)]
    junk = [c.alloc([128, 512], BF16) for _ in range(2)]
    ss = [c.alloc([128, 1], F32) for _ in range(4)]
    rstd = [c.alloc([128, 1], F32) for _ in range(4)]
    bnrm = [[Buf(), Buf(), Buf()] for _ in range(4)]
    yg = [c.alloc([128, 512], BF16) for _ in range(2)]
    byg = [Buf() for _ in range(2)]
    yT_st = [c.alloc([128, 16, 512], BF16) for _ in range(2)]
    byT = [[Buf() for _ in range(16)] for _ in range(2)]
    scb = c.psum[0][:, :].rearrange("p (h n) -> p h n", h=4)
    bsc = [Buf() for _ in range(4)]
    outp = [c.psum[1][:, :], c.psum[2][:, :]]
    bout = [Buf(), Buf()]
    dS = [c.psum[3 + i][:, :] for i in range(3)]
    bdS = [Buf() for _ in range(3)]
    tpb = [c.psum[6][:, :].bitcast(BF16)[:, 0:512], c.psum[7][:, :].bitcast(BF16)[:, 0:512]]
    btp = [Buf(), Buf()]
    cnt = {"o": 0, "d": 0, "t": 0, "y": 0, "j": 0}

    def nxt(key, n):
        v = cnt[key] % n
        cnt[key] += 1
        return v

    for cb in range(NC):
        b = cb % NB_
        t0 = cb * 128
        tile_i = (cb // 4) % 2
        tb = cb % 4
        SP.dma(qTc[b], qT_d[:, :, t0:t0 + 128].rearrange("c p t -> p c t"), w=[bin_[b][0]])
        SP.dma(kTc[b], kT_d[:, :, t0:t0 + 128].rearrange("c p t -> p c t"), w=[bin_[b][1]])
        SP.dma(ktc[b], ktok_d[cb], w=[bin_[b][2]])
        SP.dma(vc[b], v_d[cb], w=[bin_[b][3]])
        SP.dma(gc[b], gs_d[cb], w=[bin_[b][4]])
        par = cb % 2
        for h in range(4):
            for dc in range(2):
                PE.op(lambda e, h=h, dc=dc, b=b: e.matmul(scb[:, h, :], kTc[b][:, 2 * h + dc, :], qTc[b][:, 2 * h + dc, :],
                                                          start=(dc == 0), stop=(dc == 1)),
                      r=[bin_[b][0], bin_[b][1]], w=[bsc[h]], inc=(dc == 1))
            DVE.op(lambda e, h=h: e.tensor_tensor(sT[h], scb[:, h, :], cmask, ALU.mult), r=[bsc[h], bcst], w=[bsT[h]])
            o = nxt("o", 2)
            vh = vc[b][:, h * 512:(h + 1) * 512]
            PE.op(lambda e, h=h, o=o, vh=vh: e.matmul(outp[o], sT[h], vh, start=True, stop=(cb == 0)),
                  r=[bsT[h], bin_[b][3]], w=[bout[o]], inc=(cb == 0))
            if cb > 0:
                for dc in range(2):
                    PE.op(lambda e, h=h, dc=dc, o=o, b=b, par=par: e.matmul(
                        outp[o], qTc[b][:, 2 * h + dc, :], Tb[par][:, h, dc, :], start=False, stop=(dc == 1)),
                        r=[bin_[b][0], bTb[par][h][dc]], w=[bout[o]], inc=(dc == 1))
            if cb < NC - 1:
                for dc in range(2):
                    d = nxt("d", 3)
                    PE.op(lambda e, h=h, dc=dc, d=d, b=b, vh=vh: e.matmul(
                        dS[d], ktc[b][:, (2 * h + dc) * 128:(2 * h + dc + 1) * 128], vh, start=True, stop=True),
                        r=[bin_[b][2], bin_[b][3]], w=[bdS[d]], inc=True)
                    if cb == 0:
                        DVE.op(lambda e, h=h, dc=dc, d=d: e.tensor_copy(U[:, h, dc, :], dS[d]), r=[bdS[d]], w=[bU[h][dc]])
                    else:
                        DVE.op(lambda e, h=h, dc=dc, d=d: e.scalar_tensor_tensor(
                            U[:, h, dc, :], U[:, h, dc, :], float(g128[h]), dS[d], ALU.mult, ALU.add),
                            r=[bdS[d], bU[h][dc]], w=[bU[h][dc]])
                    ACT.op(lambda e, h=h, dc=dc, par=par: e.mul(Tb[1 - par][:, h, dc, :], U[:, h, dc, :], float(g128[h])),
                           r=[bU[h][dc]], w=[bTb[1 - par][h][dc]])
            ji = nxt("j", 2)
            bj, bss, brs = bnrm[h]
            ACT.op(lambda e, h=h, o=o, ji=ji: e.activation(junk[ji], outp[o], AF.Square, scale=float(512 ** -0.5), accum_out=ss[h]),
                   r=[bout[o]], w=[bj, bss])
            ACT.op(lambda e, h=h: e.activation(rstd[h], ss[h], AF.Ln, bias=EPS), r=[bss], w=[brs])
            ACT.op(lambda e, h=h: e.activation(rstd[h], rstd[h], AF.Exp, scale=-0.5), r=[brs], w=[brs])
            y = nxt("y", 2)
            gh = gc[b][:, h * 512:(h + 1) * 512]
            DVE.op(lambda e, h=h, o=o, y=y, gh=gh: e.scalar_tensor_tensor(yg[y], outp[o], rstd[h], gh, ALU.mult, ALU.mult),
                   r=[bout[o], brs, bin_[b][4]], w=[byg[y]])
            t = nxt("t", 2)
            for ec in range(4):
                PE.op(lambda e, ec=ec, y=y, t=t: e.transpose(tpb[t][:, ec * 128:(ec + 1) * 128], yg[y][:, ec * 128:(ec + 1) * 128], ident),
                      r=[byg[y], bcst], w=[btp[t]], inc=(ec == 3))
            ACT.op(lambda e, h=h, t=t, tile_i=tile_i, tb=tb: e.copy(
                yT_st[tile_i][:, h * 4:(h + 1) * 4, tb * 128:(tb + 1) * 128], tpb[t].rearrange("p (k n) -> p k n", k=4)),
                r=[btp[t]], w=[byT[tile_i][h * 4 + tb]])
        if tb == 3:
            tt0 = (cb // 4) * 512
            for g in range(2):
                POOL.dma(yT_d[g * 8:(g + 1) * 8, :, tt0:tt0 + 512].rearrange("c p t -> p c t"),
                         yT_st[tile_i][:, g * 8:(g + 1) * 8, :], r=byT[tile_i])
    c.barrier()
    c.release(mk)


def _consts(S):
    import ml_dtypes
    bf = ml_dtypes.bfloat16
    f32 = np.float32
    j = np.arange(128)
    col = np.arange(512)
    maskb = np.zeros((128, 4, 512), f32)
    for cc in range(4):
        maskb[:, cc, :] = np.where(col[None, :] > cc * 128 + j[:, None], 0.0, -30000.0)
    i = np.arange(128, dtype=f32)
    inv = (f32(10000.0) ** (-(2 * i) / f32(256))).astype(f32)
    pos = np.arange(S, dtype=f32)
    ang = (inv[:, None] * pos[None, :]).astype(f32)
    h = np.arange(4, dtype=np.float64)
    lg = np.log1p(-np.exp2(-5.0 - h))
    n = (np.arange(512) % 128).astype(np.float64)
    dq = np.exp(lg[:, None] * (n[None, :] + 1))
    dk = np.exp(-lg[:, None] * (n[None, :] + 1)) / 16.0
    dqk = np.ascontiguousarray(np.broadcast_to(np.stack([dq, dk], 0).astype(f32)[None], (128, 2, 4, 512)))
    return dict(
        c_ident=np.eye(128, dtype=f32).astype(bf),
        c_negtri=(-(j[:, None] >= j[None, :]).astype(f32)).astype(bf),
        c_negones=(-np.ones((128, 128), f32)).astype(bf),
        c_maskb=maskb.astype(bf),
        c_cmask=(j[:, None] <= j[None, :]).astype(f32),
        c_cos=np.cos(ang).astype(f32), c_sin=np.sin(ang).astype(f32),
        c_dqk=dqk,
    ), [float(v) for v in np.exp(lg * 128)]


WSPEC = [("sb_w_in", [1024, 3072]), ("sb_w_out", [1024, 1024]), ("ret_w_in", [1024, 6144]), ("ret_w_out", [2048, 1024]),
         ("mlp_w_up0", [1024, 4096]), ("mlp_w_down0", [4096, 1024]), ("ple_w_gate0", [1024, 1024]), ("ple_w_up0", [256, 1024]),
         ("mlp_w_up1", [1024, 4096]), ("mlp_w_down1", [4096, 1024]), ("ple_w_gate1", [1024, 1024]), ("ple_w_up1", [256, 1024])]
GSPEC = ["mix_norm0", "mix_norm1", "mlp_norm0", "mlp_norm1", "ple_norm0", "ple_norm1", "final_norm"]


def phase_cast(c, wf, wb, names):
    for nm in names:
        src, dst = wf[nm], wb[nm]
        R, C = src.shape
        cw = min(C, 2048)
        for r0 in range(0, R, 128):
            for c0 in range(0, C, cw):
                c.POOL.dma(dst[r0:r0 + 128, c0:c0 + cw], src[r0:r0 + 128, c0:c0 + cw])


def build(S, g128):
    from contextlib import ExitStack
    nc = bass.Bass("TRN2", target_bir_lowering=False)
    I = lambda n, s, d: nc.dram_tensor(n, s, d, kind="ExternalInput").ap()
    T = lambda n, s, d: nc.dram_tensor(n, s, d).ap()
    x = I("x", [S, 1024], F32)
    p0 = I("p0", [S, 256], F32)
    p1 = I("p1", [S, 256], F32)
    wf = {nm: I(nm, shp, F32) for nm, shp in WSPEC}
    gv = {nm: I(nm, [1024], F32) for nm in GSPEC}
    ident = I("c_ident", [128, 128], BF16)
    negtri = I("c_negtri", [128, 128], BF16)
    negones = I("c_negones", [128, 128], BF16)
    maskb = I("c_maskb", [128, 4, 512], BF16)
    cmask = I("c_cmask", [128, 128], F32)
    cos = I("c_cos", [128, S], F32)
    sin = I("c_sin", [128, S], F32)
    dqk = I("c_dqk", [128, 2, 4, 512], F32)
    y = nc.dram_tensor("y", [S, 1024], F32, kind="ExternalOutput").ap()
    wb = {nm: T("b_" + nm, shp, BF16) for nm, shp in WSPEC if nm != "sb_w_in"}
    NB = S // 128
    qkT = T("s_qkT", [16, 128, S], BF16)
    v0 = T("s_v0", [NB, 128, 1024], BF16)
    oT = T("s_oT", [8, 128, S], BF16)
    h1 = T("s_h1", [S, 1024], F32)
    qT1 = T("s_qT1", [8, 128, S], BF16)
    kT1 = T("s_kT1", [8, 128, S], BF16)
    ktok = T("s_ktok", [NB, 128, 1024], BF16)
    v1 = T("s_v1", [NB, 128, 2048], BF16)
    gs1 = T("s_gs1", [NB, 128, 2048], BF16)
    yT = T("s_yT", [16, 128, S], BF16)
    with ExitStack() as st:
        c = Ctx(nc, st)
        phase_cast(c, wf, wb, [nm for nm, _ in WSPEC if nm != "sb_w_in"])
        phase1(c, S, x, gv["mix_norm0"], wf["sb_w_in"], ident, qkT, v0)
        phase2(c, S, qkT, v0, oT, ident, negtri, negones, maskb)
        phase_post(c, S, x, h1, oT, 8, wb["sb_w_out"], gv["mlp_norm0"], wb["mlp_w_up0"], wb["mlp_w_down0"],
                   gv["ple_norm0"], wb["ple_w_gate0"], p0, wb["ple_w_up0"], ident)
        phase_retpre(c, S, h1, gv["mix_norm1"], wb["ret_w_in"], ident, cos, sin, dqk, qT1, kT1, ktok, v1, gs1)
        phase_ret(c, S, qT1, kT1, ktok, v1, gs1, ident, cmask, yT, g128)
        phase_post(c, S, h1, y, yT, 16, wb["ret_w_out"], gv["mlp_norm1"], wb["mlp_w_up1"], wb["mlp_w_down1"],
                   gv["ple_norm1"], wb["ple_w_gate1"], p1, wb["ple_w_up1"], ident, g_final=gv["final_norm"])
        c.emit()
    return nc


def _core_inputs(b, S, x, p, mix_norm, sb_w_in, sb_w_out, ret_w_in, ret_w_out, mlp_norm, mlp_w_up, mlp_w_down,
                 ple_norm, ple_w_gate, ple_w_up, final_norm, consts):
    A = lambda a: np.ascontiguousarray(np.asarray(a, dtype=np.float32))
    m = dict(consts)
    m["x"] = A(x[b, :S])
    m["p0"] = A(p[0, b, :S])
    m["p1"] = A(p[1, b, :S])
    m["sb_w_in"] = A(sb_w_in[0]); m["sb_w_out"] = A(sb_w_out[0])
    m["ret_w_in"] = A(ret_w_in[0]); m["ret_w_out"] = A(ret_w_out[0])
    for l in range(2):
        m["mlp_w_up%d" % l] = A(mlp_w_up[l]); m["mlp_w_down%d" % l] = A(mlp_w_down[l])
        m["ple_w_gate%d" % l] = A(ple_w_gate[l]); m["ple_w_up%d" % l] = A(ple_w_up[l])
        m["mix_norm%d" % l] = A(mix_norm[l]); m["mlp_norm%d" % l] = A(mlp_norm[l]); m["ple_norm%d" % l] = A(ple_norm[l])
    m["final_norm"] = A(final_norm)
    return m


def kernel(x, p, mix_norm, sb_w_in, sb_w_out, ret_w_in, ret_w_out, mlp_norm, mlp_w_up, mlp_w_down,
           ple_norm, ple_w_gate, ple_w_up, final_norm):
    x = np.asarray(x)
    B, S = x.shape[0], x.shape[1]
    consts, g128 = _consts(S)
    nc = build(S, g128)
    args = [np.asarray(a) for a in (p, mix_norm, sb_w_in, sb_w_out, ret_w_in, ret_w_out, mlp_norm, mlp_w_up, mlp_w_down,
                                    ple_norm, ple_w_gate, ple_w_up, final_norm)]
    in_maps = [_core_inputs(b, S, x, *args, consts) for b in range(B)]
    res = run_bass_kernel_spmd(nc, in_maps, core_ids=list(range(B)))
    return np.stack([np.asarray(r["y"], dtype=np.float32) for r in res.results], axis=0)
```

```python
import numpy as np
import concourse.bass as bass
import concourse.mybir as mybir
from concourse.bass_utils import run_bass_kernel_spmd

F32 = mybir.dt.float32
BF16 = mybir.dt.bfloat16
ALU = mybir.AluOpType
AF = mybir.ActivationFunctionType
AX = mybir.AxisListType

EPOCH = 30000
SAME_ENGINE_SYNC = True


class Sem:
    _n = 0

    def __init__(self, handle):
        self.h = handle
        Sem._n += 1
        self.id = Sem._n


class Buf:
    __slots__ = ("name", "lw", "rd")

    def __init__(self, name=""):
        self.name = name
        self.lw = None
        self.rd = []


class Eng:
    def __init__(self, ctx, name, is_pe=False):
        self.ctx = ctx
        self.name = name
        self.same_sync = SAME_ENGINE_SYNC and not is_pe
        self.prog = []
        self.sem = ctx.new_sem(name + "_s0")
        self.count = 0
        self.nsem = 1
        self.waited = {}
        self.pending = False
        self.dma_sems = None
        self.dma_cum = None
        self.dma_i = 0
        self.dma_k = 8

    def _need(self, dep, waits):
        if dep is None:
            return
        sem, val = dep
        if sem is self.sem and not self.same_sync:
            return
        if self.waited.get(sem.id, 0) >= val:
            return
        self.waited[sem.id] = val
        waits[sem.id] = (sem, val)

    def _collect(self, r, w):
        waits = {}
        for b in r:
            self._need(b.lw, waits)
        for b in w:
            self._need(b.lw, waits)
            for d in b.rd:
                self._need(d, waits)
        return list(waits.values())

    def _rotate(self):
        if self.count >= EPOCH:
            self.sem = self.ctx.new_sem("%s_s%d" % (self.name, self.nsem))
            self.nsem += 1
            self.count = 0

    def op(self, fn, r=(), w=(), inc=True):
        waits = self._collect(r, w)
        if inc:
            self._rotate()
            self.count += 1
            sem = self.sem
            dep = (sem, self.count)
            self.pending = False

            def run(eng, fn=fn, waits=waits, sem=sem):
                for s, v in waits:
                    eng.wait_ge(s.h, v)
                fn(eng).then_inc(sem.h, 1)
        else:
            self._rotate()
            dep = (self.sem, self.count + 1)
            self.pending = True

            def run(eng, fn=fn, waits=waits):
                for s, v in waits:
                    eng.wait_ge(s.h, v)
                fn(eng)
        self.prog.append(run)
        for b in w:
            b.lw = dep
            b.rd = []
        for b in r:
            b.rd.append(dep)
        return dep

    def dma(self, out, in_, r=(), w=(), **kw):
        if self.dma_sems is None:
            K = self.dma_k
            self.dma_sems = [self.ctx.new_sem("%s_d%d" % (self.name, i)) for i in range(K)]
            self.dma_cum = [0] * K
        slot = self.dma_i % len(self.dma_sems)
        self.dma_i += 1
        sem = self.dma_sems[slot]
        waits = {}
        if self.dma_cum[slot] > 0:
            self._need((sem, self.dma_cum[slot]), waits)
        for b in r:
            self._need(b.lw, waits)
        for b in w:
            self._need(b.lw, waits)
            for d in b.rd:
                self._need(d, waits)
        waits = list(waits.values())
        self.dma_cum[slot] += 16
        dep = (sem, self.dma_cum[slot])

        def run(eng, waits=waits, sem=sem, out=out, in_=in_, kw=kw):
            for s, v in waits:
                eng.wait_ge(s.h, v)
            eng.dma_start(out=out, in_=in_, **kw).then_inc(sem.h, 16)
        self.prog.append(run)
        for b in w:
            b.lw = dep
            b.rd = []
        for b in r:
            b.rd.append(dep)
        return dep

    def wait_dep(self, dep):
        waits = {}
        self._need(dep, waits)
        waits = list(waits.values())
        if waits:
            def run(eng, waits=waits):
                for s, v in waits:
                    eng.wait_ge(s.h, v)
            self.prog.append(run)

    def cur(self):
        assert not self.pending, self.name
        deps = [(self.sem, self.count)] if self.count > 0 else []
        if self.dma_sems is not None:
            for s, c in zip(self.dma_sems, self.dma_cum):
                if c > 0:
                    deps.append((s, c))
        return deps


class Ctx:
    def __init__(self, nc, stack, sbuf_bytes=190 * 1024):
        self.nc = nc
        self.stack = stack
        self.nsems = 0
        self.PE = Eng(self, "pe", is_pe=True)
        self.ACT = Eng(self, "act")
        self.DVE = Eng(self, "dve")
        self.POOL = Eng(self, "pool")
        self.POOL.dma_k = 4
        self.SP = Eng(self, "sp")
        self.engs = [self.PE, self.ACT, self.DVE, self.POOL, self.SP]
        self.arena_words = sbuf_bytes // 4
        self.arena = stack.enter_context(nc.sbuf_tensor("arena", [128, self.arena_words], F32))
        self.bump = 0
        self.psum = [stack.enter_context(nc.psum_tensor("ps%d" % i, [128, 512], F32)) for i in range(8)]

    def new_sem(self, name):
        self.nsems += 1
        return Sem(self.stack.enter_context(self.nc.semaphore(name)))

    def alloc(self, shape, dtype, name=""):
        esz = 4 if dtype == F32 else 2
        n = int(np.prod(shape[1:]))
        words = (n * esz + 3) // 4
        words = (words + 7) // 8 * 8
        assert self.bump + words <= self.arena_words, ("SBUF overflow", name, self.bump, words)
        ap = self.arena[0:shape[0], self.bump:self.bump + words]
        self.bump += words
        if dtype != F32:
            ap = ap.bitcast(dtype)
        ap = ap[:, 0:n]
        if len(shape) == 3:
            ap = ap.rearrange("p (a b) -> p a b", a=shape[1])
        elif len(shape) == 4:
            ap = ap.rearrange("p (a b c) -> p a b c", a=shape[1], b=shape[2])
        return ap

    def mark(self):
        return self.bump

    def release(self, mark):
        self.bump = mark

    def barrier(self):
        deps = []
        for e in self.engs:
            deps += e.cur()
        for e in self.engs:
            for d in deps:
                e.wait_dep(d)

    def emit(self):
        nc = self.nc
        with nc.Block() as block:
            @block.tensor
            def _(eng):
                for f in self.PE.prog:
                    f(eng)

            @block.scalar
            def _(eng):
                for f in self.ACT.prog:
                    f(eng)

            @block.vector
            def _(eng):
                for f in self.DVE.prog:
                    f(eng)

            @block.gpsimd
            def _(eng):
                for f in self.POOL.prog:
                    f(eng)

            @block.sync
            def _(eng):
                for f in self.SP.prog:
                    f(eng)


EPS = 1e-6
D = 1024


class NormTmp:
    def __init__(self, c, nbuf=4):
        self.n = nbuf
        self.i = 0
        self.junk = [c.alloc([128, D], BF16) for _ in range(2)]
        self.ss = [c.alloc([128, 1], F32) for _ in range(nbuf)]
        self.rstd = [c.alloc([128, 1], F32) for _ in range(nbuf)]
        self.hn = [c.alloc([128, D], BF16) for _ in range(nbuf)]
        self.b = [[Buf() for _ in range(4)] for _ in range(nbuf)]


def norm_s12(c, xt, bx, g, bg, tmp):
    ACT, DVE = c.ACT, c.DVE
    i = tmp.i % tmp.n
    tmp.i += 1
    junk, ss, rstd, hn = tmp.junk[i % len(tmp.junk)], tmp.ss[i], tmp.rstd[i], tmp.hn[i]
    bj, bss, brs, bhn = tmp.b[i]
    bj = tmp.b[i % len(tmp.junk)][0]
    ACT.op(lambda e: e.activation(junk, xt, AF.Square, scale=1.0 / 32.0, accum_out=ss), r=[bx], w=[bj, bss])
    ACT.op(lambda e: e.activation(rstd, ss, AF.Ln, bias=EPS), r=[bss], w=[brs])
    ACT.op(lambda e: e.activation(rstd, rstd, AF.Exp, scale=-0.5), r=[brs], w=[brs])
    DVE.op(lambda e: e.scalar_tensor_tensor(hn, xt, rstd, g, ALU.mult, ALU.mult), r=[bx, brs, bg], w=[bhn])
    return i


def norm_s3(c, i, ident, bid, out3, bout, tmp, tp, btp):
    PE, ACT = c.PE, c.ACT
    hn = tmp.hn[i]
    bhn = tmp.b[i][3]
    for k in range(8):
        PE.op(lambda e, k=k: e.transpose(tp[:, k * 128:(k + 1) * 128], hn[:, k * 128:(k + 1) * 128], ident),
              r=[bhn, bid], w=[btp], inc=(k == 7))
    ACT.op(lambda e: e.copy(out3, tp.rearrange("p (k t) -> p k t", k=8)), r=[btp], w=[bout])


def norm_T(c, xt, bx, g, bg, ident, bid, out3, bout, tmp, tp, btp):
    i = norm_s12(c, xt, bx, g, bg, tmp)
    norm_s3(c, i, ident, bid, out3, bout, tmp, tp, btp)


def phase1(c, S, x, mixg, w_in, ident_d, qkT_d, v_d):
    PE, ACT, DVE, POOL, SP = c.PE, c.ACT, c.DVE, c.POOL, c.SP
    mk = c.mark()
    NT = S // 512
    W = c.alloc([128, 8, 3072], BF16)
    bW = [Buf() for _ in range(8)]
    for k in range(8):
        POOL.dma(W[:, k, :], w_in[k * 128:(k + 1) * 128, :], w=[bW[k]])
    g = c.alloc([128, D], F32)
    bg = Buf()
    SP.dma(g, mixg.partition_broadcast(128), w=[bg])
    ident = c.alloc([128, 128], BF16)
    bid = Buf()
    SP.dma(ident, ident_d, w=[bid])
    tmp = NormTmp(c)
    xt = [c.alloc([128, D], F32) for _ in range(2)]
    bxt = [Buf() for _ in range(2)]
    hnT = [c.alloc([128, 8, 512], BF16) for _ in range(2)]
    bhnT = [[Buf() for _ in range(4)] for _ in range(2)]
    qk_st = [c.alloc([128, 16, 512], BF16) for _ in range(2)]
    bqk = [[Buf() for _ in range(16)] for _ in range(2)]
    v_st = [c.alloc([128, 4, D], BF16) for _ in range(2)]
    bv = [[Buf() for _ in range(8)] for _ in range(2)]
    tps = [c.psum[0][:, :].bitcast(BF16), c.psum[1][:, :].bitcast(BF16)]
    btps = [Buf(), Buf()]
    acc = [c.psum[2 + i][:, :] for i in range(4)]
    bacc = [Buf() for _ in range(4)]
    ai = 0
    xi = 0
    for tt in range(NT):
        p = tt % 2
        for tb in range(4):
            j = xi % 2
            xi += 1
            r0 = tt * 512 + tb * 128
            SP.dma(xt[j], x[r0:r0 + 128, :], w=[bxt[j]])
            norm_T(c, xt[j], bxt[j], g, bg, ident, bid, hnT[p][:, :, tb * 128:(tb + 1) * 128], bhnT[p][tb],
                   tmp, tps[j], btps[j])
        for oc in range(16):
            a = ai % 4
            ai += 1
            for k in range(8):
                PE.op(lambda e, a=a, k=k, oc=oc, p=p: e.matmul(acc[a], W[:, k, oc * 128:(oc + 1) * 128], hnT[p][:, k, :],
                                                               start=(k == 0), stop=(k == 7)),
                      r=[bW[k]] + bhnT[p], w=[bacc[a]], inc=(k == 7))
            sc = 0.125 if oc < 8 else 1.0
            if oc % 2 == 0:
                ACT.op(lambda e, a=a, oc=oc, p=p, sc=sc: e.mul(qk_st[p][:, oc, :], acc[a], sc), r=[bacc[a]], w=[bqk[p][oc]])
            else:
                DVE.op(lambda e, a=a, oc=oc, p=p, sc=sc: e.tensor_scalar(qk_st[p][:, oc, :], acc[a], sc, None, ALU.mult),
                       r=[bacc[a]], w=[bqk[p][oc]])
        SP.dma(qkT_d[:, :, tt * 512:(tt + 1) * 512].rearrange("c p t -> p c t"), qk_st[p], r=bqk[p])
        for tb in range(4):
            for hf in range(2):
                a = ai % 4
                ai += 1
                for k in range(8):
                    PE.op(lambda e, a=a, k=k, tb=tb, hf=hf, p=p: e.matmul(
                        acc[a], hnT[p][:, k, tb * 128:(tb + 1) * 128], W[:, k, 2048 + hf * 512:2048 + (hf + 1) * 512],
                        start=(k == 0), stop=(k == 7)),
                        r=[bW[k], bhnT[p][tb]], w=[bacc[a]], inc=(k == 7))
                if hf == 0:
                    ACT.op(lambda e, a=a, tb=tb, hf=hf, p=p: e.copy(v_st[p][:, tb, hf * 512:(hf + 1) * 512], acc[a]),
                           r=[bacc[a]], w=[bv[p][tb * 2 + hf]])
                else:
                    DVE.op(lambda e, a=a, tb=tb, hf=hf, p=p: e.tensor_copy(v_st[p][:, tb, hf * 512:(hf + 1) * 512], acc[a]),
                           r=[bacc[a]], w=[bv[p][tb * 2 + hf]])
        SP.dma(v_d[tt * 4:(tt + 1) * 4, :, :].rearrange("b p f -> p b f"), v_st[p], r=bv[p])
    c.barrier()
    c.release(mk)


def phase2(c, S, qkT_d, v_d, oT_d, ident_d, negtri_d, negones_d, maskb_d, heads=8, bg=None):
    PE, ACT, DVE, POOL, SP = c.PE, c.ACT, c.DVE, c.POOL, c.SP
    mk = c.mark()
    NQ = S // 512
    NB = S // 128
    ident = c.alloc([128, 128], BF16)
    negtri = c.alloc([128, 128], BF16)
    negones = c.alloc([128, 128], BF16)
    maskb = c.alloc([128, 4, 512], BF16)
    bcst = Buf()
    SP.dma(ident, ident_d, w=[bcst])
    SP.dma(negtri, negtri_d, w=[bcst])
    SP.dma(negones, negones_d, w=[bcst])
    SP.dma(maskb, maskb_d, w=[bcst])
    q2 = [c.alloc([128, 2, S], BF16) for _ in range(2)]
    k2 = [c.alloc([128, S], BF16) for _ in range(2)]
    v2 = [c.alloc([128, NB, 2, 128], BF16) for _ in range(2)]
    bq2 = [Buf() for _ in range(2)]
    bk2 = [Buf() for _ in range(2)]
    bv2 = [Buf() for _ in range(2)]
    for p in range(2):
        POOL.op(lambda e, p=p: e.memset(v2[p], 0.0), w=[bv2[p]])
        POOL.op(lambda e, p=p: e.memset(q2[p], 0.0), w=[bq2[p]])
    oT_st = [c.alloc([128, S], BF16) for _ in range(2)]
    boT = [Buf() for _ in range(2)]
    MAXI = 32
    mm = c.alloc([128, MAXI, 512], BF16)
    bm = [Buf() for _ in range(MAXI)]
    Sbs = c.alloc([128, MAXI, 512], BF16)
    bSb = [Buf() for _ in range(MAXI)]
    Sf = [c.alloc([128, 512], F32) for _ in range(2)]
    bSf = [Buf() for _ in range(2)]
    NA = 3
    aT = [c.alloc([128, 512], BF16) for _ in range(NA)]
    baT = [Buf() for _ in range(NA)]
    Z = [c.psum[i][:, :] for i in range(3)]
    bZ = [Buf() for _ in range(3)]
    E = [c.psum[3 + i][:, :] for i in range(3)]
    bE = [Buf() for _ in range(3)]
    O = [c.psum[6 + i][:, :] for i in range(2)]
    bO = [Buf() for _ in range(2)]
    cnt = {"z": 0, "e": 0, "a": 0}
    bg = list(bg) if bg else []
    nbatch_total = heads * sum((4 * qt + 4 + 15) // 16 for qt in range(NQ))
    bg_per = (len(bg) + nbatch_total - 1) // max(nbatch_total, 1)

    def nxt(key, n):
        v = cnt[key] % n
        cnt[key] += 1
        return v

    for hp in range(heads):
        p = hp % 2
        for e in range(2):
            SP.dma(q2[p][e * 64:(e + 1) * 64, e, :], qkT_d[hp][e * 64:(e + 1) * 64, :], w=[bq2[p]])
        SP.dma(k2[p], qkT_d[8 + hp], w=[bk2[p]])
        for e in range(2):
            SP.dma(v2[p][:, :, e, e * 64:(e + 1) * 64],
                   v_d[:, :, hp * 128 + e * 64: hp * 128 + (e + 1) * 64].rearrange("b p f -> p b f"), w=[bv2[p]])
        for qt in range(NQ):
            top = 4 * qt + 3
            o = qt % 2
            kbs = list(range(top, -1, -1))
            for b0 in range(0, len(kbs), 16):
                batch = kbs[b0:b0 + 16]
                items = [(kb, e) for kb in batch for e in range(2)]
                for _ in range(bg_per):
                    if bg:
                        bg.pop(0)()

                def qk(bank, bbank, kb, e, last_inc, p=p, qt=qt):
                    diag = kb >= 4 * qt
                    PE.op(lambda en: en.matmul(bank, k2[p][:, kb * 128:(kb + 1) * 128],
                                               q2[p][:, e, qt * 512:(qt + 1) * 512],
                                               start=True, stop=(last_inc and not diag)),
                          r=[bq2[p], bk2[p]], w=[bbank], inc=(last_inc and not diag))
                    if diag:
                        PE.op(lambda en: en.matmul(bank, ident, maskb[:, kb - 4 * qt, :], start=False, stop=last_inc),
                              r=[bcst], w=[bbank], inc=last_inc)

                for i, (kb, e) in enumerate(items):
                    z = nxt("z", 3)
                    qk(Z[z], bZ[z], kb, e, True)
                    ACT.op(lambda en, z=z, i=i: en.activation(mm[:, i, :], Z[z], AF.Softplus), r=[bZ[z]], w=[bm[i]])
                    if kb > 0:
                        eng = DVE if e == 0 else POOL
                        if kb == top:
                            eng.op(lambda en, i=i, e=e: en.tensor_copy(Sf[e], mm[:, i, :]), r=[bm[i]], w=[bSf[e]])
                        else:
                            eng.op(lambda en, i=i, e=e: en.tensor_tensor(Sf[e], Sf[e], mm[:, i, :], ALU.add),
                                   r=[bm[i], bSf[e]], w=[bSf[e]])
                        nslot = i + 2 if i + 2 < len(items) else e
                        DVE.op(lambda en, e=e, nslot=nslot: en.tensor_copy(Sbs[:, nslot, :], Sf[e]), r=[bSf[e]], w=[bSb[nslot]])
                pend = []

                def av(item, p=p, o=o, top=top):
                    kb, e, ai = item
                    first = (kb == top) and e == 0
                    last = kb == 0 and e == 1
                    PE.op(lambda en: en.matmul(O[o], v2[p][:, kb, e, :], aT[ai], start=first, stop=last),
                          r=[bv2[p], baT[ai]], w=[bO[o]], inc=last)
                    if last:
                        DVE.op(lambda en, p=p, qt=qt: en.tensor_copy(oT_st[p][:, qt * 512:(qt + 1) * 512], O[o]),
                               r=[bO[o]], w=[boT[p]])

                for i, (kb, e) in enumerate(items):
                    x = nxt("e", 3)
                    first_chain = kb == top
                    qk(E[x], bE[x], kb, e, False)
                    PE.op(lambda en, x=x, i=i, first_chain=first_chain: en.matmul(E[x], negtri, mm[:, i, :], start=False, stop=first_chain),
                          r=[bcst, bm[i]], w=[bE[x]], inc=first_chain)
                    if not first_chain:
                        PE.op(lambda en, x=x, i=i: en.matmul(E[x], negones, Sbs[:, i, :], start=False, stop=True),
                              r=[bcst, bSb[i]], w=[bE[x]], inc=True)
                    ai = nxt("a", NA)
                    ACT.op(lambda en, x=x, ai=ai: en.activation(aT[ai], E[x], AF.Exp), r=[bE[x]], w=[baT[ai]])
                    pend.append((kb, e, ai))
                    if len(pend) > 2:
                        av(pend.pop(0))
                while pend:
                    av(pend.pop(0))
        SP.dma(oT_d[:, :, hp, :].rearrange("t p c -> p t c"), oT_st[p].rearrange("p (t c) -> p t c", c=512), r=[boT[p]])
    while bg:
        bg.pop(0)()
    c.barrier()
    c.release(mk)


def rstd_of(c, xt, bx, junk, bj, ss, bss, rstd, brs):
    ACT = c.ACT
    ACT.op(lambda e: e.activation(junk, xt, AF.Square, scale=1.0 / 32.0, accum_out=ss), r=[bx], w=[bj, bss])
    ACT.op(lambda e: e.activation(rstd, ss, AF.Ln, bias=EPS), r=[bss], w=[brs])
    ACT.op(lambda e: e.activation(rstd, rstd, AF.Exp, scale=-0.5), r=[brs], w=[brs])


def phase_post(c, S, h_in, h_out, oT_d, KO, wb_out, g_mlp, wb_up, wb_down, g_ple, wb_gate, p_d, wb_pu, ident_d,
               g_final=None):
    PE, ACT, DVE, POOL, SP = c.PE, c.ACT, c.DVE, c.POOL, c.SP
    mk = c.mark()
    NT = S // 512
    ident = c.alloc([128, 128], BF16)
    bid = Buf()
    POOL.dma(ident, ident_d, w=[bid])
    gm = c.alloc([128, D], F32)
    gp = c.alloc([128, D], F32)
    bgm, bgp, bgf = Buf(), Buf(), Buf()
    POOL.dma(gm, g_mlp.partition_broadcast(128), w=[bgm])
    POOL.dma(gp, g_ple.partition_broadcast(128), w=[bgp])
    if g_final is not None:
        gf = c.alloc([128, D], F32)
        POOL.dma(gf, g_final.partition_broadcast(128), w=[bgf])
    tmp = NormTmp(c)
    NW = 3
    wr = [c.alloc([128, 8, 1024], BF16) for _ in range(NW)]
    bwr = [Buf() for _ in range(NW)]
    wi = [0]

    def wload(src3):
        s = wi[0] % NW
        wi[0] += 1
        nk = src3.shape[1]
        SP.dma(wr[s][:, 0:nk, :], src3, w=[bwr[s]])
        return wr[s], bwr[s]

    NH = 2
    ht_all = [[c.alloc([128, D], F32) for _ in range(4)] for _ in range(NH)]
    bht_all = [[Buf() for _ in range(4)] for _ in range(NH)]
    oTt = c.alloc([128, KO, 512], BF16)
    boTt = [Buf() for _ in range(KO // 8)]
    hnT = c.alloc([128, 8, 512], BF16)
    bhnT = [Buf() for _ in range(4)]
    aT = c.alloc([128, 32, 512], BF16)
    baT = [Buf() for _ in range(32)]
    rl = [c.alloc([128, 512], F32) for _ in range(2)]
    brl = [Buf() for _ in range(2)]
    gs = [c.alloc([128, 512], F32) for _ in range(2)]
    bgs = [Buf() for _ in range(2)]
    pt = c.alloc([128, 4, 256], F32)
    bpt = Buf()
    pb = c.alloc([128, 4, 256], BF16)
    bpb = Buf()
    pT = [c.alloc([128, 2, 128], BF16) for _ in range(4)]
    bpT = [Buf() for _ in range(4)]
    yo = [c.alloc([128, D], F32) for _ in range(2)]
    byo = [Buf() for _ in range(2)]
    tps = [c.psum[0][:, :].bitcast(BF16), c.psum[1][:, :].bitcast(BF16)]
    btps = [Buf(), Buf()]
    acc = [c.psum[2 + i][:, :] for i in range(6)]
    bacc = [Buf() for _ in range(6)]
    cnt = {"a": 0, "n": 0, "r": 0, "g": 0, "y": 0}

    def nxt(key, n):
        v = cnt[key] % n
        cnt[key] += 1
        return v

    for tt in range(NT):
        t0 = tt * 512
        ht = ht_all[tt % NH]
        bht = bht_all[tt % NH]
        for g in range(KO // 8):
            SP.dma(oTt[:, g * 8:(g + 1) * 8, :], oT_d[tt][:, g * 8:(g + 1) * 8, :], w=[boTt[g]])
        for tb in range(4):
            SP.dma(ht[tb], h_in[t0 + tb * 128:t0 + (tb + 1) * 128, :], w=[bht[tb]])
        for tb in range(4):
            SP.dma(pt[:, tb, :], p_d[t0 + tb * 128:t0 + (tb + 1) * 128, :], w=[bpt])

        def tok_mm(lhs3, blhs, nk, w3, bw, add, after_tb=None, ht=ht, bht=bht):
            for tb in range(4):
                for hf in range(2):
                    a = nxt("a", 6)
                    for k in range(nk):
                        PE.op(lambda e, a=a, k=k, tb=tb, hf=hf: e.matmul(
                            acc[a], lhs3[:, k, tb * 128:(tb + 1) * 128], w3[:, k, hf * 512:(hf + 1) * 512],
                            start=(k == 0), stop=(k == nk - 1)),
                            r=[bw] + blhs(tb), w=[bacc[a]], inc=(k == nk - 1))
                    add(tb, hf, a)
                if after_tb is not None:
                    after_tb(tb)

        def resid_add(tb, hf, a, ht=ht, bht=bht):
            DVE.op(lambda e: e.tensor_tensor(ht[tb][:, hf * 512:(hf + 1) * 512], ht[tb][:, hf * 512:(hf + 1) * 512],
                                             acc[a], ALU.add), r=[bacc[a], bht[tb]], w=[bht[tb]])

        DVE.op(lambda e: e.tensor_copy(pb, pt), r=[bpt], w=[bpb])
        for tb in range(4):
            j = nxt("n", 2)
            for k in range(2):
                PE.op(lambda e, k=k, tb=tb, j=j: e.transpose(tps[j][:, k * 128:(k + 1) * 128], pb[:, tb, k * 128:(k + 1) * 128], ident),
                      r=[bpb, bid], w=[btps[j]], inc=(k == 1))
            ACT.op(lambda e, tb=tb, j=j: e.copy(pT[tb], tps[j][:, 0:256].rearrange("p (k t) -> p k t", k=2)),
                   r=[btps[j]], w=[bpT[tb]])
        nidx = [None] * 4

        def mk_after(gg, gbuf):
            def after(tb, ht=ht, bht=bht):
                nidx[tb] = norm_s12(c, ht[tb], bht[tb], gg, gbuf, tmp)
            return after

        ng = KO // 8
        for g in range(ng):
            w3, bw = wload(wb_out[g * 1024:(g + 1) * 1024, :].rearrange("(k p) c -> p k c", p=128))
            tok_mm(oTt[:, g * 8:(g + 1) * 8, :], lambda tb, g=g: [boTt[g]], 8, w3, bw, resid_add,
                   after_tb=(mk_after(gm, bgm) if g == ng - 1 else None))
        for tb in range(4):
            j = nxt("n", 2)
            norm_s3(c, nidx[tb], ident, bid, hnT[:, :, tb * 128:(tb + 1) * 128], bhnT[tb], tmp, tps[j], btps[j])
        for cg in range(4):
            w3, bw = wload(wb_up[:, cg * 1024:(cg + 1) * 1024].rearrange("(k p) c -> p k c", p=128))
            for oc in range(8):
                a = nxt("a", 6)
                for k in range(8):
                    PE.op(lambda e, a=a, k=k, oc=oc, w3=w3: e.matmul(acc[a], w3[:, k, oc * 128:(oc + 1) * 128], hnT[:, k, :],
                                                                    start=(k == 0), stop=(k == 7)),
                          r=[bw] + bhnT, w=[bacc[a]], inc=(k == 7))
                r_ = nxt("r", 2)
                ACT.op(lambda e, a=a, r_=r_: e.activation(rl[r_], acc[a], AF.Relu), r=[bacc[a]], w=[brl[r_]])
                ff = cg * 8 + oc
                DVE.op(lambda e, r_=r_, ff=ff: e.tensor_tensor(aT[:, ff, :], rl[r_], rl[r_], ALU.mult),
                       r=[brl[r_]], w=[baT[ff]])
        for g in range(4):
            w3, bw = wload(wb_down[g * 1024:(g + 1) * 1024, :].rearrange("(k p) c -> p k c", p=128))
            tok_mm(aT[:, g * 8:(g + 1) * 8, :], lambda tb, g=g: baT[g * 8:(g + 1) * 8], 8, w3, bw, resid_add,
                   after_tb=(mk_after(gp, bgp) if g == 3 else None))
        wg3, bwg = wload(wb_gate.rearrange("(k p) c -> p k c", p=128))
        wp3, bwp = wload(wb_pu.rearrange("(k p) c -> p k c", p=128))
        for tb in range(4):
            j = nxt("n", 2)
            norm_s3(c, nidx[tb], ident, bid, hnT[:, :, tb * 128:(tb + 1) * 128], bhnT[tb], tmp, tps[j], btps[j])
        for tb in range(4):
            for hf in range(2):
                a = nxt("a", 6)
                for k in range(8):
                    PE.op(lambda e, a=a, k=k, tb=tb, hf=hf, wg3=wg3: e.matmul(
                        acc[a], hnT[:, k, tb * 128:(tb + 1) * 128], wg3[:, k, hf * 512:(hf + 1) * 512],
                        start=(k == 0), stop=(k == 7)), r=[bwg, bhnT[tb]], w=[bacc[a]], inc=(k == 7))
                gi = nxt("g", 2)
                ACT.op(lambda e, a=a, gi=gi: e.activation(gs[gi], acc[a], AF.Sigmoid), r=[bacc[a]], w=[bgs[gi]])
                a2 = nxt("a", 6)
                for k in range(2):
                    PE.op(lambda e, a2=a2, k=k, tb=tb, hf=hf, wp3=wp3: e.matmul(
                        acc[a2], pT[tb][:, k, :], wp3[:, k, hf * 512:(hf + 1) * 512],
                        start=(k == 0), stop=(k == 1)), r=[bwp, bpT[tb]], w=[bacc[a2]], inc=(k == 1))
                DVE.op(lambda e, a2=a2, gi=gi: e.tensor_tensor(gs[gi], gs[gi], acc[a2], ALU.mult),
                       r=[bacc[a2], bgs[gi]], w=[bgs[gi]])
                DVE.op(lambda e, gi=gi, tb=tb, hf=hf, ht=ht: e.tensor_tensor(
                    ht[tb][:, hf * 512:(hf + 1) * 512], ht[tb][:, hf * 512:(hf + 1) * 512], gs[gi], ALU.add),
                    r=[bgs[gi], bht[tb]], w=[bht[tb]])
            if g_final is None:
                POOL.dma(h_out[t0 + tb * 128:t0 + (tb + 1) * 128, :], ht[tb], r=[bht[tb]])
            else:
                i = tmp.i % tmp.n
                tmp.i += 1
                _, bss, brs, _ = tmp.b[i]
                bj = tmp.b[i % 2][0]
                rstd_of(c, ht[tb], bht[tb], tmp.junk[i % 2], bj, tmp.ss[i], bss, tmp.rstd[i], brs)
                yi = nxt("y", 2)
                DVE.op(lambda e, i=i, yi=yi, tb=tb, ht=ht: e.scalar_tensor_tensor(yo[yi], ht[tb], tmp.rstd[i], gf, ALU.mult, ALU.mult),
                       r=[bht[tb], brs, bgf], w=[byo[yi]])
                POOL.dma(h_out[t0 + tb * 128:t0 + (tb + 1) * 128, :], yo[yi], r=[byo[yi]])
    c.barrier()
    c.release(mk)


def phase_retpre(c, S, h_in, g_mix, wb_in, ident_d, cos_d, sin_d, dqk_d, qT_d, kT_d, ktok_d, v_d, gs_d):
    PE, ACT, DVE, POOL, SP = c.PE, c.ACT, c.DVE, c.POOL, c.SP
    mk = c.mark()
    NT = S // 512
    ident = c.alloc([128, 128], BF16)
    bid = Buf()
    POOL.dma(ident, ident_d, w=[bid])
    gm = c.alloc([128, D], F32)
    bgm = Buf()
    POOL.dma(gm, g_mix.partition_broadcast(128), w=[bgm])
    dqk = c.alloc([128, 2, 4, 512], F32)
    bdqk = Buf()
    POOL.dma(dqk[:, 0], dqk_d[:, 0], w=[bdqk])
    POOL.dma(dqk[:, 1], dqk_d[:, 1], w=[bdqk])
    tmp = NormTmp(c)
    NW = 3
    wr = [c.alloc([128, 8, 1024], BF16) for _ in range(NW)]
    bwr = [Buf() for _ in range(NW)]
    wi = [0]

    def wload(src3):
        s = wi[0] % NW
        wi[0] += 1
        SP.dma(wr[s], src3, w=[bwr[s]])
        return wr[s], bwr[s]

    ht = [c.alloc([128, D], F32) for _ in range(2)]
    bht = [Buf() for _ in range(2)]
    hnT = c.alloc([128, 8, 512], BF16)
    bhnT = [Buf() for _ in range(4)]
    cs = [c.alloc([128, 2, 512], F32) for _ in range(2)]
    bcs = [Buf() for _ in range(2)]
    x12 = [c.alloc([128, 2, 512], F32) for _ in range(2)]
    bx12 = [[Buf(), Buf()] for _ in range(2)]
    tA = [c.alloc([128, 3, 512], F32)] * 2
    btA = [Buf()] * 2
    tB = [c.alloc([128, 3, 512], F32)] * 2
    btB = [Buf()] * 2
    qk_st = [c.alloc([128, 8, 512], BF16) for _ in range(2)]
    bqk = [[Buf() for _ in range(8)] for _ in range(2)]
    ktok_st = c.alloc([128, 4, 1024], BF16)
    bktok = [Buf() for _ in range(4)]
    vg_st = [c.alloc([128, 4, 2048], BF16) for _ in range(2)]
    bvg = [[Buf() for _ in range(16)] for _ in range(2)]
    tps = [c.psum[0][:, :].bitcast(BF16), c.psum[1][:, :].bitcast(BF16)]
    btps = [Buf(), Buf()]
    acc = [c.psum[2 + i][:, :] for i in range(6)]
    bacc = [Buf() for _ in range(6)]
    cnt = {"a": 0, "n": 0, "x": 0, "h": 0}

    def nxt(key, n):
        v = cnt[key] % n
        cnt[key] += 1
        return v

    for tt in range(NT):
        t0 = tt * 512
        ci = tt % 2
        POOL.dma(cs[ci][:, 0, :], cos_d[:, t0:t0 + 512], w=[bcs[ci]])
        POOL.dma(cs[ci][:, 1, :], sin_d[:, t0:t0 + 512], w=[bcs[ci]])
        for tb in range(4):
            hi = nxt("h", 2)
            POOL.dma(ht[hi], h_in[t0 + tb * 128:t0 + (tb + 1) * 128, :], w=[bht[hi]])
            j = nxt("n", 2)
            norm_T(c, ht[hi], bht[hi], gm, bgm, ident, bid, hnT[:, :, tb * 128:(tb + 1) * 128], bhnT[tb], tmp,
                   tps[j], btps[j])
        cos_t = cs[ci][:, 0, :]
        sin_t = cs[ci][:, 1, :]
        for which in range(2):
            w3, bw = wload(wb_in[:, which * 1024:(which + 1) * 1024].rearrange("(k p) c -> p k c", p=128))
            st = qk_st[which]
            for h in range(4):
                xi = nxt("x", 2)
                for half in range(2):
                    oc = 2 * h + half
                    a = nxt("a", 6)
                    for k in range(8):
                        PE.op(lambda e, a=a, k=k, oc=oc, w3=w3: e.matmul(acc[a], w3[:, k, oc * 128:(oc + 1) * 128], hnT[:, k, :],
                                                                        start=(k == 0), stop=(k == 7)),
                              r=[bw] + bhnT, w=[bacc[a]], inc=(k == 7))
                    ACT.op(lambda e, a=a, xi=xi, half=half: e.copy(x12[xi][:, half, :], acc[a]),
                           r=[bacc[a]], w=[bx12[xi][half]])
                x1 = x12[xi][:, 0, :]
                x2 = x12[xi][:, 1, :]
                dtab = dqk[:, which, h, :]
                ta, tb_ = tA[xi], tB[xi]
                rd = [bx12[xi][0], bx12[xi][1], bcs[ci], bdqk]
                DVE.op(lambda e, ta=ta, x1=x1, cos_t=cos_t: e.tensor_tensor(ta[:, 0, :], x1, cos_t, ALU.mult), r=rd, w=[btA[xi]])
                DVE.op(lambda e, ta=ta, x2=x2, sin_t=sin_t: e.tensor_tensor(ta[:, 1, :], x2, sin_t, ALU.mult), r=rd, w=[btA[xi]])
                DVE.op(lambda e, ta=ta: e.tensor_tensor(ta[:, 2, :], ta[:, 0, :], ta[:, 1, :], ALU.subtract), r=[btA[xi]], w=[btA[xi]])
                DVE.op(lambda e, ta=ta, st=st, h=h, dtab=dtab: e.tensor_tensor(st[:, 2 * h, :], ta[:, 2, :], dtab, ALU.mult),
                       r=[btA[xi], bdqk], w=[bqk[which][2 * h]])
                POOL.op(lambda e, tb_=tb_, x1=x1, sin_t=sin_t: e.tensor_tensor(tb_[:, 0, :], x1, sin_t, ALU.mult), r=rd, w=[btB[xi]])
                POOL.op(lambda e, tb_=tb_, x2=x2, cos_t=cos_t: e.tensor_tensor(tb_[:, 1, :], x2, cos_t, ALU.mult), r=rd, w=[btB[xi]])
                POOL.op(lambda e, tb_=tb_: e.tensor_tensor(tb_[:, 2, :], tb_[:, 0, :], tb_[:, 1, :], ALU.add), r=[btB[xi]], w=[btB[xi]])
                POOL.op(lambda e, tb_=tb_, st=st, h=h, dtab=dtab: e.tensor_tensor(st[:, 2 * h + 1, :], tb_[:, 2, :], dtab, ALU.mult),
                        r=[btB[xi], bdqk], w=[bqk[which][2 * h + 1]])
            dst = qT_d if which == 0 else kT_d
            SP.dma(dst[tt], st, r=bqk[which])
            if which == 1:
                for tb in range(4):
                    j = nxt("n", 2)
                    for k in range(8):
                        PE.op(lambda e, k=k, tb=tb, j=j, st=st: e.transpose(tps[j][:, k * 128:(k + 1) * 128],
                                                                     st[:, k, tb * 128:(tb + 1) * 128], ident),
                              r=[bqk[1][k], bid], w=[btps[j]], inc=(k == 7))
                    ACT.op(lambda e, tb=tb, j=j: e.copy(ktok_st[:, tb, :], tps[j]), r=[btps[j]], w=[bktok[tb]])
                for tb in range(4):
                    POOL.dma(ktok_d[tt * 4 + tb], ktok_st[:, tb, :], r=[bktok[tb]])
        for which in range(2):
            st = vg_st[which]
            for g in range(2):
                c0 = 2048 + which * 2048 + g * 1024
                w3, bw = wload(wb_in[:, c0:c0 + 1024].rearrange("(k p) c -> p k c", p=128))
                for tb in range(4):
                    for hf in range(2):
                        a = nxt("a", 6)
                        for k in range(8):
                            PE.op(lambda e, a=a, k=k, tb=tb, hf=hf, w3=w3: e.matmul(
                                acc[a], hnT[:, k, tb * 128:(tb + 1) * 128], w3[:, k, hf * 512:(hf + 1) * 512],
                                start=(k == 0), stop=(k == 7)), r=[bw, bhnT[tb]], w=[bacc[a]], inc=(k == 7))
                        col = g * 1024 + hf * 512
                        bi = tb * 4 + g * 2 + hf
                        if which == 1:
                            ACT.op(lambda e, a=a, tb=tb, col=col, st=st: e.activation(st[:, tb, col:col + 512], acc[a], AF.Silu),
                                   r=[bacc[a]], w=[bvg[which][bi]])
                        else:
                            DVE.op(lambda e, a=a, tb=tb, col=col, st=st: e.tensor_copy(st[:, tb, col:col + 512], acc[a]),
                                   r=[bacc[a]], w=[bvg[which][bi]])
            dst = v_d if which == 0 else gs_d
            for tb in range(4):
                POOL.dma(dst[tt * 4 + tb], st[:, tb, :], r=bvg[which][tb * 4:(tb + 1) * 4])
    c.barrier()
    c.release(mk)


def phase_ret(c, S, qT_d, kT_d, ktok_d, v_d, gs_d, ident_d, cmask_d, yT_d, g128):
    PE, ACT, DVE, POOL, SP = c.PE, c.ACT, c.DVE, c.POOL, c.SP
    mk = c.mark()
    NC = S // 128
    ident = c.alloc([128, 128], BF16)
    cmask = c.alloc([128, 128], F32)
    bcst = Buf()
    SP.dma(ident, ident_d, w=[bcst])
    SP.dma(cmask, cmask_d, w=[bcst])
    NB_ = 3
    qTc = [c.alloc([128, 8, 128], BF16) for _ in range(NB_)]
    kTc = [c.alloc([128, 8, 128], BF16) for _ in range(NB_)]
    ktc = [c.alloc([128, 1024], BF16) for _ in range(NB_)]
    vc = [c.alloc([128, 2048], BF16) for _ in range(NB_)]
    gc = [c.alloc([128, 2048], BF16) for _ in range(NB_)]
    bin_ = [[Buf() for _ in range(5)] for _ in range(NB_)]
    U = c.alloc([128, 4, 2, 512], F32)
    bU = [[Buf(), Buf()] for _ in range(4)]
    Tb = [c.alloc([128, 4, 2, 512], BF16) for _ in range(2)]
    bTb = [[[Buf(), Buf()] for _ in range(4)] for _ in range(2)]
    sT = [c.alloc([128, 128], BF16) for _ in range(4)]
    bsT = [Buf() for _ in range(4)]
    junk = [c.alloc([128, 512], BF16) for _ in range(2)]
    ss = [c.alloc([128, 1], F32) for _ in range(4)]
    rstd = [c.alloc([128, 1], F32) for _ in range(4)]
    bnrm = [[Buf(), Buf(), Buf()] for _ in range(4)]
    yg = [c.alloc([128, 512], BF16) for _ in range(2)]
    byg = [Buf() for _ in range(2)]
    yT_st = [c.alloc([128, 16, 512], BF16) for _ in range(2)]
    byT = [[Buf() for _ in range(16)] for _ in range(2)]
    scbk = [c.psum[0][:, 0:128], c.psum[1][:, 0:128]]
    bsck = [Buf(), Buf()]
    outp = [c.psum[2][:, :], c.psum[3][:, :]]
    bout = [Buf(), Buf()]
    dS = [c.psum[4 + i][:, :] for i in range(2)]
    bdS = [Buf() for _ in range(2)]
    tpb = [c.psum[6][:, :].bitcast(BF16)[:, 0:512], c.psum[7][:, :].bitcast(BF16)[:, 0:512]]
    btp = [Buf(), Buf()]
    cnt = {"o": 0, "d": 0, "t": 0, "y": 0, "j": 0}

    def nxt(key, n):
        v = cnt[key] % n
        cnt[key] += 1
        return v

    for cb in range(NC):
        b = cb % NB_
        t0 = cb * 128
        tile_i = (cb // 4) % 2
        tb = cb % 4
        SP.dma(qTc[b], qT_d[cb // 4][:, :, tb * 128:(tb + 1) * 128], w=[bin_[b][0]])
        SP.dma(kTc[b], kT_d[cb // 4][:, :, tb * 128:(tb + 1) * 128], w=[bin_[b][1]])
        SP.dma(ktc[b], ktok_d[cb], w=[bin_[b][2]])
        SP.dma(vc[b], v_d[cb], w=[bin_[b][3]])
        SP.dma(gc[b], gs_d[cb], w=[bin_[b][4]])
        par = cb % 2
        for h in range(4):
            for dc in range(2):
                PE.op(lambda e, h=h, dc=dc, b=b: e.matmul(scbk[h % 2], kTc[b][:, 2 * h + dc, :], qTc[b][:, 2 * h + dc, :],
                                                          start=(dc == 0), stop=(dc == 1)),
                      r=[bin_[b][0], bin_[b][1]], w=[bsck[h % 2]], inc=(dc == 1))
            DVE.op(lambda e, h=h: e.tensor_tensor(sT[h], scbk[h % 2], cmask, ALU.mult), r=[bsck[h % 2], bcst], w=[bsT[h]])
            o = nxt("o", 2)
            vh = vc[b][:, h * 512:(h + 1) * 512]
            PE.op(lambda e, h=h, o=o, vh=vh: e.matmul(outp[o], sT[h], vh, start=True, stop=(cb == 0)),
                  r=[bsT[h], bin_[b][3]], w=[bout[o]], inc=(cb == 0))
            if cb > 0:
                for dc in range(2):
                    PE.op(lambda e, h=h, dc=dc, o=o, b=b, par=par: e.matmul(
                        outp[o], qTc[b][:, 2 * h + dc, :], Tb[par][:, h, dc, :], start=False, stop=(dc == 1)),
                        r=[bin_[b][0], bTb[par][h][dc]], w=[bout[o]], inc=(dc == 1))
            if cb < NC - 1:
                for dc in range(2):
                    d = nxt("d", 2)
                    PE.op(lambda e, h=h, dc=dc, d=d, b=b, vh=vh: e.matmul(
                        dS[d], ktc[b][:, (2 * h + dc) * 128:(2 * h + dc + 1) * 128], vh, start=True, stop=True),
                        r=[bin_[b][2], bin_[b][3]], w=[bdS[d]], inc=True)
                    if cb == 0:
                        DVE.op(lambda e, h=h, dc=dc, d=d: e.tensor_copy(U[:, h, dc, :], dS[d]), r=[bdS[d]], w=[bU[h][dc]])
                    else:
                        DVE.op(lambda e, h=h, dc=dc, d=d: e.scalar_tensor_tensor(
                            U[:, h, dc, :], U[:, h, dc, :], float(g128[h]), dS[d], ALU.mult, ALU.add),
                            r=[bdS[d], bU[h][dc]], w=[bU[h][dc]])
                    ACT.op(lambda e, h=h, dc=dc, par=par: e.mul(Tb[1 - par][:, h, dc, :], U[:, h, dc, :], float(g128[h])),
                           r=[bU[h][dc]], w=[bTb[1 - par][h][dc]])
            ji = nxt("j", 2)
            bj, bss, brs = bnrm[h]
            ACT.op(lambda e, h=h, o=o, ji=ji: e.activation(junk[ji], outp[o], AF.Square, scale=float(512 ** -0.5), accum_out=ss[h]),
                   r=[bout[o]], w=[bj, bss])
            ACT.op(lambda e, h=h: e.activation(rstd[h], ss[h], AF.Ln, bias=EPS), r=[bss], w=[brs])
            ACT.op(lambda e, h=h: e.activation(rstd[h], rstd[h], AF.Exp, scale=-0.5), r=[brs], w=[brs])
            y = nxt("y", 2)
            gh = gc[b][:, h * 512:(h + 1) * 512]
            DVE.op(lambda e, h=h, o=o, y=y, gh=gh: e.scalar_tensor_tensor(yg[y], outp[o], rstd[h], gh, ALU.mult, ALU.mult),
                   r=[bout[o], brs, bin_[b][4]], w=[byg[y]])
            t = nxt("t", 2)
            for ec in range(4):
                PE.op(lambda e, ec=ec, y=y, t=t: e.transpose(tpb[t][:, ec * 128:(ec + 1) * 128], yg[y][:, ec * 128:(ec + 1) * 128], ident),
                      r=[byg[y], bcst], w=[btp[t]], inc=(ec == 3))
            ACT.op(lambda e, h=h, t=t, tile_i=tile_i, tb=tb: e.copy(
                yT_st[tile_i][:, h * 4:(h + 1) * 4, tb * 128:(tb + 1) * 128], tpb[t].rearrange("p (k n) -> p k n", k=4)),
                r=[btp[t]], w=[byT[tile_i][h * 4 + tb]])
        if tb == 3:
            for g in range(2):
                POOL.dma(yT_d[cb // 4][:, g * 8:(g + 1) * 8, :], yT_st[tile_i][:, g * 8:(g + 1) * 8, :], r=byT[tile_i])
    c.barrier()
    c.release(mk)


def _consts(S):
    import ml_dtypes
    bf = ml_dtypes.bfloat16
    f32 = np.float32
    j = np.arange(128)
    col = np.arange(512)
    maskb = np.zeros((128, 4, 512), f32)
    for cc in range(4):
        maskb[:, cc, :] = np.where(col[None, :] > cc * 128 + j[:, None], 0.0, -30000.0)
    i = np.arange(128, dtype=f32)
    inv = (f32(10000.0) ** (-(2 * i) / f32(256))).astype(f32)
    pos = np.arange(S, dtype=f32)
    ang = (inv[:, None] * pos[None, :]).astype(f32)
    h = np.arange(4, dtype=np.float64)
    lg = np.log1p(-np.exp2(-5.0 - h))
    n = (np.arange(512) % 128).astype(np.float64)
    dq = np.exp(lg[:, None] * (n[None, :] + 1))
    dk = np.exp(-lg[:, None] * (n[None, :] + 1)) / 16.0
    dqk = np.ascontiguousarray(np.broadcast_to(np.stack([dq, dk], 0).astype(f32)[None], (128, 2, 4, 512)))
    return dict(
        c_ident=np.eye(128, dtype=f32).astype(bf),
        c_negtri=(-(j[:, None] >= j[None, :]).astype(f32)).astype(bf),
        c_negones=(-np.ones((128, 128), f32)).astype(bf),
        c_maskb=maskb.astype(bf),
        c_cmask=(j[:, None] <= j[None, :]).astype(f32),
        c_cos=np.cos(ang).astype(f32), c_sin=np.sin(ang).astype(f32),
        c_dqk=dqk,
    ), [float(v) for v in np.exp(lg * 128)]


WSPEC = [("sb_w_in", [1024, 3072]), ("sb_w_out", [1024, 1024]), ("ret_w_in", [1024, 6144]), ("ret_w_out", [2048, 1024]),
         ("mlp_w_up0", [1024, 4096]), ("mlp_w_down0", [4096, 1024]), ("ple_w_gate0", [1024, 1024]), ("ple_w_up0", [256, 1024]),
         ("mlp_w_up1", [1024, 4096]), ("mlp_w_down1", [4096, 1024]), ("ple_w_gate1", [1024, 1024]), ("ple_w_up1", [256, 1024])]
GSPEC = ["mix_norm0", "mix_norm1", "mlp_norm0", "mlp_norm1", "ple_norm0", "ple_norm1", "final_norm"]


def phase_cast(c, wf, wb, names):
    out = []
    for nm in names:
        src, dst = wf[nm], wb[nm]
        R, C = src.shape
        cw = min(C, 2048)
        for r0 in range(0, R, 128):
            for c0 in range(0, C, cw):
                out.append(lambda d=dst[r0:r0 + 128, c0:c0 + cw], s_=src[r0:r0 + 128, c0:c0 + cw]: c.POOL.dma(d, s_))
    return out


def build(S, g128):
    from contextlib import ExitStack
    nc = bass.Bass("TRN2", target_bir_lowering=False)
    I = lambda n, s, d: nc.dram_tensor(n, s, d, kind="ExternalInput").ap()
    T = lambda n, s, d: nc.dram_tensor(n, s, d).ap()
    x = I("x", [S, 1024], F32)
    p0 = I("p0", [S, 256], F32)
    p1 = I("p1", [S, 256], F32)
    wf = {nm: I(nm, shp, F32) for nm, shp in WSPEC}
    gv = {nm: I(nm, [1024], F32) for nm in GSPEC}
    ident = I("c_ident", [128, 128], BF16)
    negtri = I("c_negtri", [128, 128], BF16)
    negones = I("c_negones", [128, 128], BF16)
    maskb = I("c_maskb", [128, 4, 512], BF16)
    cmask = I("c_cmask", [128, 128], F32)
    cos = I("c_cos", [128, S], F32)
    sin = I("c_sin", [128, S], F32)
    dqk = I("c_dqk", [128, 2, 4, 512], F32)
    y = nc.dram_tensor("y", [S, 1024], F32, kind="ExternalOutput").ap()
    wb = {nm: T("b_" + nm, shp, BF16) for nm, shp in WSPEC if nm != "sb_w_in"}
    NB = S // 128
    qkT = T("s_qkT", [16, 128, S], BF16)
    v0 = T("s_v0", [NB, 128, 1024], BF16)
    oT = T("s_oT", [S // 512, 128, 8, 512], BF16)
    h1 = T("s_h1", [S, 1024], F32)
    qT1 = T("s_qT1", [S // 512, 128, 8, 512], BF16)
    kT1 = T("s_kT1", [S // 512, 128, 8, 512], BF16)
    ktok = T("s_ktok", [NB, 128, 1024], BF16)
    v1 = T("s_v1", [NB, 128, 2048], BF16)
    gs1 = T("s_gs1", [NB, 128, 2048], BF16)
    yT = T("s_yT", [S // 512, 128, 16, 512], BF16)
    with ExitStack() as st:
        c = Ctx(nc, st)
        order = ["sb_w_out", "mlp_w_up0", "mlp_w_down0", "ple_w_gate0", "ple_w_up0", "ret_w_in", "ret_w_out",
                 "mlp_w_up1", "mlp_w_down1", "ple_w_gate1", "ple_w_up1"]
        casts = phase_cast(c, wf, wb, order)
        phase1(c, S, x, gv["mix_norm0"], wf["sb_w_in"], ident, qkT, v0)
        phase2(c, S, qkT, v0, oT, ident, negtri, negones, maskb, bg=casts)
        phase_post(c, S, x, h1, oT, 8, wb["sb_w_out"], gv["mlp_norm0"], wb["mlp_w_up0"], wb["mlp_w_down0"],
                   gv["ple_norm0"], wb["ple_w_gate0"], p0, wb["ple_w_up0"], ident)
        phase_retpre(c, S, h1, gv["mix_norm1"], wb["ret_w_in"], ident, cos, sin, dqk, qT1, kT1, ktok, v1, gs1)
        phase_ret(c, S, qT1, kT1, ktok, v1, gs1, ident, cmask, yT, g128)
        phase_post(c, S, h1, y, yT, 16, wb["ret_w_out"], gv["mlp_norm1"], wb["mlp_w_up1"], wb["mlp_w_down1"],
                   gv["ple_norm1"], wb["ple_w_gate1"], p1, wb["ple_w_up1"], ident, g_final=gv["final_norm"])
        c.emit()
    return nc


def _core_inputs(b, S, x, p, mix_norm, sb_w_in, sb_w_out, ret_w_in, ret_w_out, mlp_norm, mlp_w_up, mlp_w_down,
                 ple_norm, ple_w_gate, ple_w_up, final_norm, consts):
    A = lambda a: np.ascontiguousarray(np.asarray(a, dtype=np.float32))
    m = dict(consts)
    m["x"] = A(x[b, :S])
    m["p0"] = A(p[0, b, :S])
    m["p1"] = A(p[1, b, :S])
    m["sb_w_in"] = A(sb_w_in[0]); m["sb_w_out"] = A(sb_w_out[0])
    m["ret_w_in"] = A(ret_w_in[0]); m["ret_w_out"] = A(ret_w_out[0])
    for l in range(2):
        m["mlp_w_up%d" % l] = A(mlp_w_up[l]); m["mlp_w_down%d" % l] = A(mlp_w_down[l])
        m["ple_w_gate%d" % l] = A(ple_w_gate[l]); m["ple_w_up%d" % l] = A(ple_w_up[l])
        m["mix_norm%d" % l] = A(mix_norm[l]); m["mlp_norm%d" % l] = A(mlp_norm[l]); m["ple_norm%d" % l] = A(ple_norm[l])
    m["final_norm"] = A(final_norm)
    return m


def kernel(x, p, mix_norm, sb_w_in, sb_w_out, ret_w_in, ret_w_out, mlp_norm, mlp_w_up, mlp_w_down,
           ple_norm, ple_w_gate, ple_w_up, final_norm):
    x = np.asarray(x)
    B, S = x.shape[0], x.shape[1]
    consts, g128 = _consts(S)
    nc = build(S, g128)
    args = [np.asarray(a) for a in (p, mix_norm, sb_w_in, sb_w_out, ret_w_in, ret_w_out, mlp_norm, mlp_w_up, mlp_w_down,
                                    ple_norm, ple_w_gate, ple_w_up, final_norm)]
    in_maps = [_core_inputs(b, S, x, *args, consts) for b in range(B)]
    res = run_bass_kernel_spmd(nc, in_maps, core_ids=list(range(B)))
    return np.stack([np.asarray(r["y"], dtype=np.float32) for r in res.results], axis=0)
```

```python
import numpy as np
import concourse.bass as bass
import concourse.mybir as mybir
from concourse.bass_utils import run_bass_kernel_spmd

F32 = mybir.dt.float32
BF16 = mybir.dt.bfloat16
ALU = mybir.AluOpType
AF = mybir.ActivationFunctionType
AX = mybir.AxisListType

EPOCH = 30000
SAME_ENGINE_SYNC = True


class Sem:
    _n = 0

    def __init__(self, handle):
        self.h = handle
        Sem._n += 1
        self.id = Sem._n


class Buf:
    __slots__ = ("name", "lw", "rd")

    def __init__(self, name=""):
        self.name = name
        self.lw = None
        self.rd = []


class Eng:
    def __init__(self, ctx, name, is_pe=False):
        self.ctx = ctx
        self.name = name
        self.same_sync = SAME_ENGINE_SYNC and not is_pe
        self.prog = []
        self.sem = ctx.new_sem(name + "_s0")
        self.count = 0
        self.nsem = 1
        self.waited = {}
        self.pending = False
        self.dma_sems = None
        self.dma_cum = None
        self.dma_i = 0
        self.dma_k = 8

    def _need(self, dep, waits):
        if dep is None:
            return
        sem, val = dep
        if sem is self.sem and not self.same_sync:
            return
        if self.waited.get(sem.id, 0) >= val:
            return
        self.waited[sem.id] = val
        waits[sem.id] = (sem, val)

    def _collect(self, r, w):
        waits = {}
        for b in r:
            self._need(b.lw, waits)
        for b in w:
            self._need(b.lw, waits)
            for d in b.rd:
                self._need(d, waits)
        return list(waits.values())

    def _rotate(self):
        if self.count >= EPOCH:
            self.sem = self.ctx.new_sem("%s_s%d" % (self.name, self.nsem))
            self.nsem += 1
            self.count = 0

    def op(self, fn, r=(), w=(), inc=True):
        waits = self._collect(r, w)
        if inc:
            self._rotate()
            self.count += 1
            sem = self.sem
            dep = (sem, self.count)
            self.pending = False

            def run(eng, fn=fn, waits=waits, sem=sem):
                for s, v in waits:
                    eng.wait_ge(s.h, v)
                fn(eng).then_inc(sem.h, 1)
        else:
            self._rotate()
            dep = (self.sem, self.count + 1)
            self.pending = True

            def run(eng, fn=fn, waits=waits):
                for s, v in waits:
                    eng.wait_ge(s.h, v)
                fn(eng)
        self.prog.append(run)
        for b in w:
            b.lw = dep
            b.rd = []
        for b in r:
            b.rd.append(dep)
        return dep

    def dma(self, out, in_, r=(), w=(), **kw):
        if self.dma_sems is None:
            K = self.dma_k
            self.dma_sems = [self.ctx.new_sem("%s_d%d" % (self.name, i)) for i in range(K)]
            self.dma_cum = [0] * K
        slot = self.dma_i % len(self.dma_sems)
        self.dma_i += 1
        sem = self.dma_sems[slot]
        waits = {}
        if self.dma_cum[slot] > 0:
            self._need((sem, self.dma_cum[slot]), waits)
        for b in r:
            self._need(b.lw, waits)
        for b in w:
            self._need(b.lw, waits)
            for d in b.rd:
                self._need(d, waits)
        waits = list(waits.values())
        self.dma_cum[slot] += 16
        dep = (sem, self.dma_cum[slot])

        def run(eng, waits=waits, sem=sem, out=out, in_=in_, kw=kw):
            for s, v in waits:
                eng.wait_ge(s.h, v)
            eng.dma_start(out=out, in_=in_, **kw).then_inc(sem.h, 16)
        self.prog.append(run)
        for b in w:
            b.lw = dep
            b.rd = []
        for b in r:
            b.rd.append(dep)
        return dep

    def wait_dep(self, dep):
        waits = {}
        self._need(dep, waits)
        waits = list(waits.values())
        if waits:
            def run(eng, waits=waits):
                for s, v in waits:
                    eng.wait_ge(s.h, v)
            self.prog.append(run)

    def cur(self):
        assert not self.pending, self.name
        deps = [(self.sem, self.count)] if self.count > 0 else []
        if self.dma_sems is not None:
            for s, c in zip(self.dma_sems, self.dma_cum):
                if c > 0:
                    deps.append((s, c))
        return deps


class Ctx:
    def __init__(self, nc, stack, sbuf_bytes=190 * 1024):
        self.nc = nc
        self.stack = stack
        self.nsems = 0
        self.PE = Eng(self, "pe", is_pe=True)
        self.ACT = Eng(self, "act")
        self.DVE = Eng(self, "dve")
        self.POOL = Eng(self, "pool")
        self.POOL.dma_k = 4
        self.SP = Eng(self, "sp")
        self.engs = [self.PE, self.ACT, self.DVE, self.POOL, self.SP]
        self.arena_words = sbuf_bytes // 4
        self.arena = stack.enter_context(nc.sbuf_tensor("arena", [128, self.arena_words], F32))
        self.bump = 0
        self.psum = [stack.enter_context(nc.psum_tensor("ps%d" % i, [128, 512], F32)) for i in range(8)]

    def new_sem(self, name):
        self.nsems += 1
        return Sem(self.stack.enter_context(self.nc.semaphore(name)))

    def alloc(self, shape, dtype, name=""):
        esz = 4 if dtype == F32 else 2
        n = int(np.prod(shape[1:]))
        words = (n * esz + 3) // 4
        words = (words + 7) // 8 * 8
        assert self.bump + words <= self.arena_words, ("SBUF overflow", name, self.bump, words)
        ap = self.arena[0:shape[0], self.bump:self.bump + words]
        self.bump += words
        if dtype != F32:
            ap = ap.bitcast(dtype)
        ap = ap[:, 0:n]
        if len(shape) == 3:
            ap = ap.rearrange("p (a b) -> p a b", a=shape[1])
        elif len(shape) == 4:
            ap = ap.rearrange("p (a b c) -> p a b c", a=shape[1], b=shape[2])
        return ap

    def mark(self):
        return self.bump

    def release(self, mark):
        self.bump = mark

    def barrier(self):
        deps = []
        for e in self.engs:
            deps += e.cur()
        for e in self.engs:
            for d in deps:
                e.wait_dep(d)

    def emit(self):
        nc = self.nc
        with nc.Block() as block:
            @block.tensor
            def _(eng):
                for f in self.PE.prog:
                    f(eng)

            @block.scalar
            def _(eng):
                for f in self.ACT.prog:
                    f(eng)

            @block.vector
            def _(eng):
                for f in self.DVE.prog:
                    f(eng)

            @block.gpsimd
            def _(eng):
                for f in self.POOL.prog:
                    f(eng)

            @block.sync
            def _(eng):
                for f in self.SP.prog:
                    f(eng)


EPS = 1e-6
D = 1024


class NormTmp:
    def __init__(self, c, nbuf=4):
        self.n = nbuf
        self.i = 0
        self.junk = [c.alloc([128, D], BF16) for _ in range(2)]
        self.ss = [c.alloc([128, 1], F32) for _ in range(nbuf)]
        self.rstd = [c.alloc([128, 1], F32) for _ in range(nbuf)]
        self.hn = [c.alloc([128, D], BF16) for _ in range(nbuf)]
        self.b = [[Buf() for _ in range(4)] for _ in range(nbuf)]


def norm_s12(c, xt, bx, g, bg, tmp):
    ACT, DVE = c.ACT, c.DVE
    i = tmp.i % tmp.n
    tmp.i += 1
    junk, ss, rstd, hn = tmp.junk[i % len(tmp.junk)], tmp.ss[i], tmp.rstd[i], tmp.hn[i]
    bj, bss, brs, bhn = tmp.b[i]
    bj = tmp.b[i % len(tmp.junk)][0]
    ACT.op(lambda e: e.activation(junk, xt, AF.Square, scale=1.0 / 32.0, accum_out=ss), r=[bx], w=[bj, bss])
    ACT.op(lambda e: e.activation(rstd, ss, AF.Ln, bias=EPS), r=[bss], w=[brs])
    ACT.op(lambda e: e.activation(rstd, rstd, AF.Exp, scale=-0.5), r=[brs], w=[brs])
    DVE.op(lambda e: e.scalar_tensor_tensor(hn, xt, rstd, g, ALU.mult, ALU.mult), r=[bx, brs, bg], w=[bhn])
    return i


def norm_s3(c, i, ident, bid, out3, bout, tmp, tp, btp):
    PE, ACT = c.PE, c.ACT
    hn = tmp.hn[i]
    bhn = tmp.b[i][3]
    for k in range(8):
        PE.op(lambda e, k=k: e.transpose(tp[:, k * 128:(k + 1) * 128], hn[:, k * 128:(k + 1) * 128], ident),
              r=[bhn, bid], w=[btp], inc=(k == 7))
    ACT.op(lambda e: e.copy(out3, tp.rearrange("p (k t) -> p k t", k=8)), r=[btp], w=[bout])


def norm_T(c, xt, bx, g, bg, ident, bid, out3, bout, tmp, tp, btp):
    i = norm_s12(c, xt, bx, g, bg, tmp)
    norm_s3(c, i, ident, bid, out3, bout, tmp, tp, btp)


def phase1(c, S, x, mixg, w_in, ident_d, qkT_d, v_d):
    PE, ACT, DVE, POOL, SP = c.PE, c.ACT, c.DVE, c.POOL, c.SP
    mk = c.mark()
    NT = S // 512
    W = c.alloc([128, 8, 3072], BF16)
    bW = [Buf() for _ in range(8)]
    for k in range(8):
        POOL.dma(W[:, k, :], w_in[k * 128:(k + 1) * 128, :], w=[bW[k]])
    g = c.alloc([128, D], F32)
    bg = Buf()
    SP.dma(g, mixg.partition_broadcast(128), w=[bg])
    ident = c.alloc([128, 128], BF16)
    bid = Buf()
    SP.dma(ident, ident_d, w=[bid])
    tmp = NormTmp(c)
    xt = [c.alloc([128, D], F32) for _ in range(2)]
    bxt = [Buf() for _ in range(2)]
    hnT = [c.alloc([128, 8, 512], BF16) for _ in range(2)]
    bhnT = [[Buf() for _ in range(4)] for _ in range(2)]
    qk_st = [c.alloc([128, 16, 512], BF16) for _ in range(2)]
    bqk = [[Buf() for _ in range(16)] for _ in range(2)]
    v_st = [c.alloc([128, 4, D], BF16) for _ in range(2)]
    bv = [[Buf() for _ in range(8)] for _ in range(2)]
    tps = [c.psum[0][:, :].bitcast(BF16), c.psum[1][:, :].bitcast(BF16)]
    btps = [Buf(), Buf()]
    acc = [c.psum[2 + i][:, :] for i in range(4)]
    bacc = [Buf() for _ in range(4)]
    ai = 0
    xi = 0
    for tt in range(NT):
        p = tt % 2
        for tb in range(4):
            j = xi % 2
            xi += 1
            r0 = tt * 512 + tb * 128
            SP.dma(xt[j], x[r0:r0 + 128, :], w=[bxt[j]])
            norm_T(c, xt[j], bxt[j], g, bg, ident, bid, hnT[p][:, :, tb * 128:(tb + 1) * 128], bhnT[p][tb],
                   tmp, tps[j], btps[j])
        for oc in range(16):
            a = ai % 4
            ai += 1
            for k in range(8):
                PE.op(lambda e, a=a, k=k, oc=oc, p=p: e.matmul(acc[a], W[:, k, oc * 128:(oc + 1) * 128], hnT[p][:, k, :],
                                                               start=(k == 0), stop=(k == 7)),
                      r=[bW[k]] + bhnT[p], w=[bacc[a]], inc=(k == 7))
            sc = 0.125 if oc < 8 else 1.0
            if oc % 2 == 0:
                ACT.op(lambda e, a=a, oc=oc, p=p, sc=sc: e.mul(qk_st[p][:, oc, :], acc[a], sc), r=[bacc[a]], w=[bqk[p][oc]])
            else:
                DVE.op(lambda e, a=a, oc=oc, p=p, sc=sc: e.tensor_scalar(qk_st[p][:, oc, :], acc[a], sc, None, ALU.mult),
                       r=[bacc[a]], w=[bqk[p][oc]])
        SP.dma(qkT_d[:, :, tt * 512:(tt + 1) * 512].rearrange("c p t -> p c t"), qk_st[p], r=bqk[p])
        for tb in range(4):
            for hf in range(2):
                a = ai % 4
                ai += 1
                for k in range(8):
                    PE.op(lambda e, a=a, k=k, tb=tb, hf=hf, p=p: e.matmul(
                        acc[a], hnT[p][:, k, tb * 128:(tb + 1) * 128], W[:, k, 2048 + hf * 512:2048 + (hf + 1) * 512],
                        start=(k == 0), stop=(k == 7)),
                        r=[bW[k], bhnT[p][tb]], w=[bacc[a]], inc=(k == 7))
                if hf == 0:
                    ACT.op(lambda e, a=a, tb=tb, hf=hf, p=p: e.copy(v_st[p][:, tb, hf * 512:(hf + 1) * 512], acc[a]),
                           r=[bacc[a]], w=[bv[p][tb * 2 + hf]])
                else:
                    DVE.op(lambda e, a=a, tb=tb, hf=hf, p=p: e.tensor_copy(v_st[p][:, tb, hf * 512:(hf + 1) * 512], acc[a]),
                           r=[bacc[a]], w=[bv[p][tb * 2 + hf]])
        SP.dma(v_d[tt * 4:(tt + 1) * 4, :, :].rearrange("b p f -> p b f"), v_st[p], r=bv[p])
    c.barrier()
    c.release(mk)


def phase2(c, S, qkT_d, v_d, oT_d, ident_d, negtri_d, negones_d, maskb_d, heads=8, bg=None):
    PE, ACT, DVE, POOL, SP = c.PE, c.ACT, c.DVE, c.POOL, c.SP
    mk = c.mark()
    NQ = S // 512
    NB = S // 128
    ident = c.alloc([128, 128], BF16)
    negtri = c.alloc([128, 128], BF16)
    negones = c.alloc([128, 128], BF16)
    maskb = c.alloc([128, 4, 512], BF16)
    bcst = Buf()
    SP.dma(ident, ident_d, w=[bcst])
    SP.dma(negtri, negtri_d, w=[bcst])
    SP.dma(negones, negones_d, w=[bcst])
    SP.dma(maskb, maskb_d, w=[bcst])
    q2 = [c.alloc([128, 2, S], BF16) for _ in range(2)]
    k2 = [c.alloc([128, S], BF16) for _ in range(2)]
    v2 = [c.alloc([128, NB, 2, 128], BF16) for _ in range(2)]
    bq2 = [Buf() for _ in range(2)]
    bk2 = [Buf() for _ in range(2)]
    bv2 = [Buf() for _ in range(2)]
    for p in range(2):
        POOL.op(lambda e, p=p: e.memset(v2[p], 0.0), w=[bv2[p]])
        POOL.op(lambda e, p=p: e.memset(q2[p], 0.0), w=[bq2[p]])
    oT_st = [c.alloc([128, S], BF16) for _ in range(2)]
    boT = [Buf() for _ in range(2)]
    MAXI = 32
    mm = c.alloc([128, MAXI, 512], BF16)
    bm = [Buf() for _ in range(MAXI)]
    Sbs = c.alloc([128, MAXI, 512], BF16)
    bSb = [Buf() for _ in range(MAXI)]
    Sf = [c.alloc([128, 512], F32) for _ in range(2)]
    bSf = [Buf() for _ in range(2)]
    NA = 3
    aT = [c.alloc([128, 512], BF16) for _ in range(NA)]
    baT = [Buf() for _ in range(NA)]
    Z = [c.psum[i][:, :] for i in range(3)]
    bZ = [Buf() for _ in range(3)]
    E = [c.psum[3 + i][:, :] for i in range(3)]
    bE = [Buf() for _ in range(3)]
    O = [c.psum[6 + i][:, :] for i in range(2)]
    bO = [Buf() for _ in range(2)]
    cnt = {"z": 0, "e": 0, "a": 0}
    bg = list(bg) if bg else []
    nbatch_total = heads * sum((4 * qt + 4 + 15) // 16 for qt in range(NQ))
    bg_per = (len(bg) + nbatch_total - 1) // max(nbatch_total, 1)

    def nxt(key, n):
        v = cnt[key] % n
        cnt[key] += 1
        return v

    for hp in range(heads):
        p = hp % 2
        for e in range(2):
            SP.dma(q2[p][e * 64:(e + 1) * 64, e, :], qkT_d[hp][e * 64:(e + 1) * 64, :], w=[bq2[p]])
        SP.dma(k2[p], qkT_d[8 + hp], w=[bk2[p]])
        for e in range(2):
            SP.dma(v2[p][:, :, e, e * 64:(e + 1) * 64],
                   v_d[:, :, hp * 128 + e * 64: hp * 128 + (e + 1) * 64].rearrange("b p f -> p b f"), w=[bv2[p]])
        for qt in range(NQ):
            top = 4 * qt + 3
            o = qt % 2
            kbs = list(range(top, -1, -1))
            for b0 in range(0, len(kbs), 16):
                batch = kbs[b0:b0 + 16]
                items = [(kb, e) for kb in batch for e in range(2)]
                for _ in range(bg_per):
                    if bg:
                        bg.pop(0)()

                def qk(bank, bbank, kb, e, last_inc, p=p, qt=qt):
                    diag = kb >= 4 * qt
                    PE.op(lambda en: en.matmul(bank, k2[p][:, kb * 128:(kb + 1) * 128],
                                               q2[p][:, e, qt * 512:(qt + 1) * 512],
                                               start=True, stop=(last_inc and not diag)),
                          r=[bq2[p], bk2[p]], w=[bbank], inc=(last_inc and not diag))
                    if diag:
                        PE.op(lambda en: en.matmul(bank, ident, maskb[:, kb - 4 * qt, :], start=False, stop=last_inc),
                              r=[bcst], w=[bbank], inc=last_inc)

                for i, (kb, e) in enumerate(items):
                    z = nxt("z", 3)
                    qk(Z[z], bZ[z], kb, e, True)
                    ACT.op(lambda en, z=z, i=i: en.activation(mm[:, i, :], Z[z], AF.Softplus), r=[bZ[z]], w=[bm[i]])
                    if kb > 0:
                        eng = DVE if e == 0 else POOL
                        if kb == top:
                            eng.op(lambda en, i=i, e=e: en.tensor_copy(Sf[e], mm[:, i, :]), r=[bm[i]], w=[bSf[e]])
                        else:
                            eng.op(lambda en, i=i, e=e: en.tensor_tensor(Sf[e], Sf[e], mm[:, i, :], ALU.add),
                                   r=[bm[i], bSf[e]], w=[bSf[e]])
                        nslot = i + 2 if i + 2 < len(items) else e
                        DVE.op(lambda en, e=e, nslot=nslot: en.tensor_copy(Sbs[:, nslot, :], Sf[e]), r=[bSf[e]], w=[bSb[nslot]])
                pend = []

                def av(item, p=p, o=o, top=top):
                    kb, e, ai = item
                    first = (kb == top) and e == 0
                    last = kb == 0 and e == 1
                    PE.op(lambda en: en.matmul(O[o], v2[p][:, kb, e, :], aT[ai], start=first, stop=last),
                          r=[bv2[p], baT[ai]], w=[bO[o]], inc=last)
                    if last:
                        DVE.op(lambda en, p=p, qt=qt: en.tensor_copy(oT_st[p][:, qt * 512:(qt + 1) * 512], O[o]),
                               r=[bO[o]], w=[boT[p]])

                for i, (kb, e) in enumerate(items):
                    x = nxt("e", 3)
                    first_chain = kb == top
                    qk(E[x], bE[x], kb, e, False)
                    PE.op(lambda en, x=x, i=i, first_chain=first_chain: en.matmul(E[x], negtri, mm[:, i, :], start=False, stop=first_chain),
                          r=[bcst, bm[i]], w=[bE[x]], inc=first_chain)
                    if not first_chain:
                        PE.op(lambda en, x=x, i=i: en.matmul(E[x], negones, Sbs[:, i, :], start=False, stop=True),
                              r=[bcst, bSb[i]], w=[bE[x]], inc=True)
                    ai = nxt("a", NA)
                    ACT.op(lambda en, x=x, ai=ai: en.activation(aT[ai], E[x], AF.Exp), r=[bE[x]], w=[baT[ai]])
                    pend.append((kb, e, ai))
                    if len(pend) > 2:
                        av(pend.pop(0))
                while pend:
                    av(pend.pop(0))
        SP.dma(oT_d[:, :, hp, :].rearrange("t p c -> p t c"), oT_st[p].rearrange("p (t c) -> p t c", c=512), r=[boT[p]])
    while bg:
        bg.pop(0)()
    c.barrier()
    c.release(mk)


def rstd_of(c, xt, bx, junk, bj, ss, bss, rstd, brs):
    ACT = c.ACT
    ACT.op(lambda e: e.activation(junk, xt, AF.Square, scale=1.0 / 32.0, accum_out=ss), r=[bx], w=[bj, bss])
    ACT.op(lambda e: e.activation(rstd, ss, AF.Ln, bias=EPS), r=[bss], w=[brs])
    ACT.op(lambda e: e.activation(rstd, rstd, AF.Exp, scale=-0.5), r=[brs], w=[brs])


def phase_post(c, S, h_in, h_out, oT_d, KO, wb_out, g_mlp, wb_up, wb_down, g_ple, wb_gate, p_d, wb_pu, ident_d,
               g_final=None):
    PE, ACT, DVE, POOL, SP = c.PE, c.ACT, c.DVE, c.POOL, c.SP
    mk = c.mark()
    NT = S // 512
    ident = c.alloc([128, 128], BF16)
    bid = Buf()
    POOL.dma(ident, ident_d, w=[bid])
    gm = c.alloc([128, D], F32)
    gp = c.alloc([128, D], F32)
    bgm, bgp, bgf = Buf(), Buf(), Buf()
    POOL.dma(gm, g_mlp.partition_broadcast(128), w=[bgm])
    POOL.dma(gp, g_ple.partition_broadcast(128), w=[bgp])
    if g_final is not None:
        gf = c.alloc([128, D], F32)
        POOL.dma(gf, g_final.partition_broadcast(128), w=[bgf])
    tmp = NormTmp(c)
    NW = 3
    wr = [c.alloc([128, 8, 1024], BF16) for _ in range(NW)]
    bwr = [Buf() for _ in range(NW)]
    wi = [0]

    def wload(src3):
        s = wi[0] % NW
        wi[0] += 1
        nk = src3.shape[1]
        SP.dma(wr[s][:, 0:nk, :], src3, w=[bwr[s]])
        return wr[s], bwr[s]

    NH = 2
    ht_all = [[c.alloc([128, D], F32) for _ in range(4)] for _ in range(NH)]
    bht_all = [[Buf() for _ in range(4)] for _ in range(NH)]
    oTt = c.alloc([128, KO, 512], BF16)
    boTt = [Buf() for _ in range(KO // 8)]
    hnT = c.alloc([128, 8, 512], BF16)
    bhnT = [Buf() for _ in range(4)]
    aT = c.alloc([128, 32, 512], BF16)
    baT = [Buf() for _ in range(32)]
    rl = [c.alloc([128, 512], F32) for _ in range(2)]
    brl = [Buf() for _ in range(2)]
    gs = [c.alloc([128, 512], F32) for _ in range(2)]
    bgs = [Buf() for _ in range(2)]
    pt = c.alloc([128, 4, 256], F32)
    bpt = Buf()
    pb = c.alloc([128, 4, 256], BF16)
    bpb = Buf()
    pT = [c.alloc([128, 2, 128], BF16) for _ in range(4)]
    bpT = [Buf() for _ in range(4)]
    yo = [c.alloc([128, D], F32) for _ in range(2)]
    byo = [Buf() for _ in range(2)]
    tps = [c.psum[0][:, :].bitcast(BF16), c.psum[1][:, :].bitcast(BF16)]
    btps = [Buf(), Buf()]
    acc = [c.psum[2 + i][:, :] for i in range(6)]
    bacc = [Buf() for _ in range(6)]
    cnt = {"a": 0, "n": 0, "r": 0, "g": 0, "y": 0}

    def nxt(key, n):
        v = cnt[key] % n
        cnt[key] += 1
        return v

    for tt in range(NT):
        t0 = tt * 512
        ht = ht_all[tt % NH]
        bht = bht_all[tt % NH]
        for g in range(KO // 8):
            SP.dma(oTt[:, g * 8:(g + 1) * 8, :], oT_d[tt][:, g * 8:(g + 1) * 8, :], w=[boTt[g]])
        for tb in range(4):
            SP.dma(ht[tb], h_in[t0 + tb * 128:t0 + (tb + 1) * 128, :], w=[bht[tb]])
        for tb in range(4):
            SP.dma(pt[:, tb, :], p_d[t0 + tb * 128:t0 + (tb + 1) * 128, :], w=[bpt])

        def tok_mm(lhs3, blhs, nk, w3, bw, add, after_tb=None, ht=ht, bht=bht):
            for tb in range(4):
                for hf in range(2):
                    a = nxt("a", 6)
                    for k in range(nk):
                        PE.op(lambda e, a=a, k=k, tb=tb, hf=hf: e.matmul(
                            acc[a], lhs3[:, k, tb * 128:(tb + 1) * 128], w3[:, k, hf * 512:(hf + 1) * 512],
                            start=(k == 0), stop=(k == nk - 1)),
                            r=[bw] + blhs(tb), w=[bacc[a]], inc=(k == nk - 1))
                    add(tb, hf, a)
                if after_tb is not None:
                    after_tb(tb)

        def resid_add(tb, hf, a, ht=ht, bht=bht):
            DVE.op(lambda e: e.tensor_tensor(ht[tb][:, hf * 512:(hf + 1) * 512], ht[tb][:, hf * 512:(hf + 1) * 512],
                                             acc[a], ALU.add), r=[bacc[a], bht[tb]], w=[bht[tb]])

        DVE.op(lambda e: e.tensor_copy(pb, pt), r=[bpt], w=[bpb])
        for tb in range(4):
            j = nxt("n", 2)
            for k in range(2):
                PE.op(lambda e, k=k, tb=tb, j=j: e.transpose(tps[j][:, k * 128:(k + 1) * 128], pb[:, tb, k * 128:(k + 1) * 128], ident),
                      r=[bpb, bid], w=[btps[j]], inc=(k == 1))
            ACT.op(lambda e, tb=tb, j=j: e.copy(pT[tb], tps[j][:, 0:256].rearrange("p (k t) -> p k t", k=2)),
                   r=[btps[j]], w=[bpT[tb]])
        nidx = [None] * 4

        def mk_after(gg, gbuf):
            def after(tb, ht=ht, bht=bht):
                nidx[tb] = norm_s12(c, ht[tb], bht[tb], gg, gbuf, tmp)
            return after

        ng = KO // 8
        for g in range(ng):
            w3, bw = wload(wb_out[g * 1024:(g + 1) * 1024, :].rearrange("(k p) c -> p k c", p=128))
            tok_mm(oTt[:, g * 8:(g + 1) * 8, :], lambda tb, g=g: [boTt[g]], 8, w3, bw, resid_add,
                   after_tb=(mk_after(gm, bgm) if g == ng - 1 else None))
        for tb in range(4):
            j = nxt("n", 2)
            norm_s3(c, nidx[tb], ident, bid, hnT[:, :, tb * 128:(tb + 1) * 128], bhnT[tb], tmp, tps[j], btps[j])
        for cg in range(4):
            w3, bw = wload(wb_up[:, cg * 1024:(cg + 1) * 1024].rearrange("(k p) c -> p k c", p=128))
            for oc in range(8):
                a = nxt("a", 6)
                for k in range(8):
                    PE.op(lambda e, a=a, k=k, oc=oc, w3=w3: e.matmul(acc[a], w3[:, k, oc * 128:(oc + 1) * 128], hnT[:, k, :],
                                                                    start=(k == 0), stop=(k == 7)),
                          r=[bw] + bhnT, w=[bacc[a]], inc=(k == 7))
                r_ = nxt("r", 2)
                ACT.op(lambda e, a=a, r_=r_: e.activation(rl[r_], acc[a], AF.Relu), r=[bacc[a]], w=[brl[r_]])
                ff = cg * 8 + oc
                DVE.op(lambda e, r_=r_, ff=ff: e.tensor_tensor(aT[:, ff, :], rl[r_], rl[r_], ALU.mult),
                       r=[brl[r_]], w=[baT[ff]])
        for g in range(4):
            w3, bw = wload(wb_down[g * 1024:(g + 1) * 1024, :].rearrange("(k p) c -> p k c", p=128))
            tok_mm(aT[:, g * 8:(g + 1) * 8, :], lambda tb, g=g: baT[g * 8:(g + 1) * 8], 8, w3, bw, resid_add,
                   after_tb=(mk_after(gp, bgp) if g == 3 else None))
        wg3, bwg = wload(wb_gate.rearrange("(k p) c -> p k c", p=128))
        wp3, bwp = wload(wb_pu.rearrange("(k p) c -> p k c", p=128))
        for tb in range(4):
            j = nxt("n", 2)
            norm_s3(c, nidx[tb], ident, bid, hnT[:, :, tb * 128:(tb + 1) * 128], bhnT[tb], tmp, tps[j], btps[j])
        for tb in range(4):
            for hf in range(2):
                a = nxt("a", 6)
                for k in range(8):
                    PE.op(lambda e, a=a, k=k, tb=tb, hf=hf, wg3=wg3: e.matmul(
                        acc[a], hnT[:, k, tb * 128:(tb + 1) * 128], wg3[:, k, hf * 512:(hf + 1) * 512],
                        start=(k == 0), stop=(k == 7)), r=[bwg, bhnT[tb]], w=[bacc[a]], inc=(k == 7))
                gi = nxt("g", 2)
                ACT.op(lambda e, a=a, gi=gi: e.activation(gs[gi], acc[a], AF.Sigmoid), r=[bacc[a]], w=[bgs[gi]])
                a2 = nxt("a", 6)
                for k in range(2):
                    PE.op(lambda e, a2=a2, k=k, tb=tb, hf=hf, wp3=wp3: e.matmul(
                        acc[a2], pT[tb][:, k, :], wp3[:, k, hf * 512:(hf + 1) * 512],
                        start=(k == 0), stop=(k == 1)), r=[bwp, bpT[tb]], w=[bacc[a2]], inc=(k == 1))
                DVE.op(lambda e, a2=a2, gi=gi: e.tensor_tensor(gs[gi], gs[gi], acc[a2], ALU.mult),
                       r=[bacc[a2], bgs[gi]], w=[bgs[gi]])
                DVE.op(lambda e, gi=gi, tb=tb, hf=hf, ht=ht: e.tensor_tensor(
                    ht[tb][:, hf * 512:(hf + 1) * 512], ht[tb][:, hf * 512:(hf + 1) * 512], gs[gi], ALU.add),
                    r=[bgs[gi], bht[tb]], w=[bht[tb]])
            if g_final is None:
                POOL.dma(h_out[t0 + tb * 128:t0 + (tb + 1) * 128, :], ht[tb], r=[bht[tb]])
            else:
                i = tmp.i % tmp.n
                tmp.i += 1
                _, bss, brs, _ = tmp.b[i]
                bj = tmp.b[i % 2][0]
                rstd_of(c, ht[tb], bht[tb], tmp.junk[i % 2], bj, tmp.ss[i], bss, tmp.rstd[i], brs)
                yi = nxt("y", 2)
                DVE.op(lambda e, i=i, yi=yi, tb=tb, ht=ht: e.scalar_tensor_tensor(yo[yi], ht[tb], tmp.rstd[i], gf, ALU.mult, ALU.mult),
                       r=[bht[tb], brs, bgf], w=[byo[yi]])
                POOL.dma(h_out[t0 + tb * 128:t0 + (tb + 1) * 128, :], yo[yi], r=[byo[yi]])
    c.barrier()
    c.release(mk)


def phase_retpre(c, S, h_in, g_mix, wb_in, ident_d, cos_d, sin_d, dqk_d, qT_d, kT_d, ktok_d, v_d, gs_d):
    PE, ACT, DVE, POOL, SP = c.PE, c.ACT, c.DVE, c.POOL, c.SP
    mk = c.mark()
    NT = S // 512
    ident = c.alloc([128, 128], BF16)
    bid = Buf()
    POOL.dma(ident, ident_d, w=[bid])
    gm = c.alloc([128, D], F32)
    bgm = Buf()
    POOL.dma(gm, g_mix.partition_broadcast(128), w=[bgm])
    dqk = c.alloc([128, 2, 4, 512], F32)
    bdqk = Buf()
    POOL.dma(dqk[:, 0], dqk_d[:, 0], w=[bdqk])
    POOL.dma(dqk[:, 1], dqk_d[:, 1], w=[bdqk])
    tmp = NormTmp(c)
    NW = 3
    wr = [c.alloc([128, 8, 1024], BF16) for _ in range(NW)]
    bwr = [Buf() for _ in range(NW)]
    wi = [0]

    def wload(src3):
        s = wi[0] % NW
        wi[0] += 1
        SP.dma(wr[s], src3, w=[bwr[s]])
        return wr[s], bwr[s]

    ht = [c.alloc([128, D], F32) for _ in range(2)]
    bht = [Buf() for _ in range(2)]
    hnT = c.alloc([128, 8, 512], BF16)
    bhnT = [Buf() for _ in range(4)]
    cs = [c.alloc([128, 2, 512], F32) for _ in range(2)]
    bcs = [Buf() for _ in range(2)]
    x12 = [c.alloc([128, 2, 512], F32) for _ in range(2)]
    bx12 = [[Buf(), Buf()] for _ in range(2)]
    tA = [c.alloc([128, 3, 512], F32)] * 2
    btA = [Buf()] * 2
    tB = [c.alloc([128, 3, 512], F32)] * 2
    btB = [Buf()] * 2
    qk_st = [c.alloc([128, 8, 512], BF16) for _ in range(2)]
    bqk = [[Buf() for _ in range(8)] for _ in range(2)]
    ktok_st = c.alloc([128, 4, 1024], BF16)
    bktok = [Buf() for _ in range(4)]
    vg_st = [c.alloc([128, 4, 2048], BF16) for _ in range(2)]
    bvg = [[Buf() for _ in range(16)] for _ in range(2)]
    tps = [c.psum[0][:, :].bitcast(BF16), c.psum[1][:, :].bitcast(BF16)]
    btps = [Buf(), Buf()]
    acc = [c.psum[2 + i][:, :] for i in range(6)]
    bacc = [Buf() for _ in range(6)]
    cnt = {"a": 0, "n": 0, "x": 0, "h": 0}

    def nxt(key, n):
        v = cnt[key] % n
        cnt[key] += 1
        return v

    for tt in range(NT):
        t0 = tt * 512
        ci = tt % 2
        POOL.dma(cs[ci][:, 0, :], cos_d[:, t0:t0 + 512], w=[bcs[ci]])
        POOL.dma(cs[ci][:, 1, :], sin_d[:, t0:t0 + 512], w=[bcs[ci]])
        for tb in range(4):
            hi = nxt("h", 2)
            POOL.dma(ht[hi], h_in[t0 + tb * 128:t0 + (tb + 1) * 128, :], w=[bht[hi]])
            j = nxt("n", 2)
            norm_T(c, ht[hi], bht[hi], gm, bgm, ident, bid, hnT[:, :, tb * 128:(tb + 1) * 128], bhnT[tb], tmp,
                   tps[j], btps[j])
        cos_t = cs[ci][:, 0, :]
        sin_t = cs[ci][:, 1, :]
        for which in range(2):
            w3, bw = wload(wb_in[:, which * 1024:(which + 1) * 1024].rearrange("(k p) c -> p k c", p=128))
            st = qk_st[which]
            for h in range(4):
                xi = nxt("x", 2)
                for half in range(2):
                    oc = 2 * h + half
                    a = nxt("a", 6)
                    for k in range(8):
                        PE.op(lambda e, a=a, k=k, oc=oc, w3=w3: e.matmul(acc[a], w3[:, k, oc * 128:(oc + 1) * 128], hnT[:, k, :],
                                                                        start=(k == 0), stop=(k == 7)),
                              r=[bw] + bhnT, w=[bacc[a]], inc=(k == 7))
                    ACT.op(lambda e, a=a, xi=xi, half=half: e.copy(x12[xi][:, half, :], acc[a]),
                           r=[bacc[a]], w=[bx12[xi][half]])
                x1 = x12[xi][:, 0, :]
                x2 = x12[xi][:, 1, :]
                dtab = dqk[:, which, h, :]
                ta, tb_ = tA[xi], tB[xi]
                rd = [bx12[xi][0], bx12[xi][1], bcs[ci], bdqk]
                DVE.op(lambda e, ta=ta, x1=x1, cos_t=cos_t: e.tensor_tensor(ta[:, 0, :], x1, cos_t, ALU.mult), r=rd, w=[btA[xi]])
                DVE.op(lambda e, ta=ta, x2=x2, sin_t=sin_t: e.tensor_tensor(ta[:, 1, :], x2, sin_t, ALU.mult), r=rd, w=[btA[xi]])
                DVE.op(lambda e, ta=ta: e.tensor_tensor(ta[:, 2, :], ta[:, 0, :], ta[:, 1, :], ALU.subtract), r=[btA[xi]], w=[btA[xi]])
                DVE.op(lambda e, ta=ta, st=st, h=h, dtab=dtab: e.tensor_tensor(st[:, 2 * h, :], ta[:, 2, :], dtab, ALU.mult),
                       r=[btA[xi], bdqk], w=[bqk[which][2 * h]])
                POOL.op(lambda e, tb_=tb_, x1=x1, sin_t=sin_t: e.tensor_tensor(tb_[:, 0, :], x1, sin_t, ALU.mult), r=rd, w=[btB[xi]])
                POOL.op(lambda e, tb_=tb_, x2=x2, cos_t=cos_t: e.tensor_tensor(tb_[:, 1, :], x2, cos_t, ALU.mult), r=rd, w=[btB[xi]])
                POOL.op(lambda e, tb_=tb_: e.tensor_tensor(tb_[:, 2, :], tb_[:, 0, :], tb_[:, 1, :], ALU.add), r=[btB[xi]], w=[btB[xi]])
                POOL.op(lambda e, tb_=tb_, st=st, h=h, dtab=dtab: e.tensor_tensor(st[:, 2 * h + 1, :], tb_[:, 2, :], dtab, ALU.mult),
                        r=[btB[xi], bdqk], w=[bqk[which][2 * h + 1]])
            dst = qT_d if which == 0 else kT_d
            SP.dma(dst[tt], st, r=bqk[which])
            if which == 1:
                for tb in range(4):
                    j = nxt("n", 2)
                    for k in range(8):
                        PE.op(lambda e, k=k, tb=tb, j=j, st=st: e.transpose(tps[j][:, k * 128:(k + 1) * 128],
                                                                     st[:, k, tb * 128:(tb + 1) * 128], ident),
                              r=[bqk[1][k], bid], w=[btps[j]], inc=(k == 7))
                    ACT.op(lambda e, tb=tb, j=j: e.copy(ktok_st[:, tb, :], tps[j]), r=[btps[j]], w=[bktok[tb]])
                for tb in range(4):
                    POOL.dma(ktok_d[tt * 4 + tb], ktok_st[:, tb, :], r=[bktok[tb]])
        for which in range(2):
            st = vg_st[which]
            for g in range(2):
                c0 = 2048 + which * 2048 + g * 1024
                w3, bw = wload(wb_in[:, c0:c0 + 1024].rearrange("(k p) c -> p k c", p=128))
                for tb in range(4):
                    for hf in range(2):
                        a = nxt("a", 6)
                        for k in range(8):
                            PE.op(lambda e, a=a, k=k, tb=tb, hf=hf, w3=w3: e.matmul(
                                acc[a], hnT[:, k, tb * 128:(tb + 1) * 128], w3[:, k, hf * 512:(hf + 1) * 512],
                                start=(k == 0), stop=(k == 7)), r=[bw, bhnT[tb]], w=[bacc[a]], inc=(k == 7))
                        col = g * 1024 + hf * 512
                        bi = tb * 4 + g * 2 + hf
                        if which == 1:
                            ACT.op(lambda e, a=a, tb=tb, col=col, st=st: e.activation(st[:, tb, col:col + 512], acc[a], AF.Silu),
                                   r=[bacc[a]], w=[bvg[which][bi]])
                        else:
                            DVE.op(lambda e, a=a, tb=tb, col=col, st=st: e.tensor_copy(st[:, tb, col:col + 512], acc[a]),
                                   r=[bacc[a]], w=[bvg[which][bi]])
            dst = v_d if which == 0 else gs_d
            for tb in range(4):
                POOL.dma(dst[tt * 4 + tb], st[:, tb, :], r=bvg[which][tb * 4:(tb + 1) * 4])
    c.barrier()
    c.release(mk)


def phase_ret(c, S, qT_d, kT_d, ktok_d, v_d, gs_d, ident_d, cmask_d, yT_d, g128):
    PE, ACT, DVE, POOL, SP = c.PE, c.ACT, c.DVE, c.POOL, c.SP
    mk = c.mark()
    NC = S // 128
    ident = c.alloc([128, 128], BF16)
    cmask = c.alloc([128, 128], F32)
    bcst = Buf()
    SP.dma(ident, ident_d, w=[bcst])
    SP.dma(cmask, cmask_d, w=[bcst])
    NB_ = 3
    qTc = [c.alloc([128, 8, 128], BF16) for _ in range(NB_)]
    kTc = [c.alloc([128, 8, 128], BF16) for _ in range(NB_)]
    ktc = [c.alloc([128, 1024], BF16) for _ in range(NB_)]
    vc = [c.alloc([128, 2048], BF16) for _ in range(NB_)]
    gc = [c.alloc([128, 2048], BF16) for _ in range(NB_)]
    bin_ = [[Buf() for _ in range(5)] for _ in range(NB_)]
    U = c.alloc([128, 4, 2, 512], F32)
    bU = [[Buf(), Buf()] for _ in range(4)]
    Tb = [c.alloc([128, 4, 2, 512], BF16) for _ in range(2)]
    bTb = [[[Buf(), Buf()] for _ in range(4)] for _ in range(2)]
    sT = [c.alloc([128, 128], BF16) for _ in range(4)]
    bsT = [Buf() for _ in range(4)]
    junk = [c.alloc([128, 512], BF16) for _ in range(2)]
    ss = [c.alloc([128, 1], F32) for _ in range(4)]
    rstd = [c.alloc([128, 1], F32) for _ in range(4)]
    bnrm = [[Buf(), Buf(), Buf()] for _ in range(4)]
    yg = [c.alloc([128, 512], BF16) for _ in range(2)]
    byg = [Buf() for _ in range(2)]
    yT_st = [c.alloc([128, 16, 512], BF16) for _ in range(2)]
    byT = [[Buf() for _ in range(16)] for _ in range(2)]
    scbk = [c.psum[0][:, 0:128], c.psum[1][:, 0:128]]
    bsck = [Buf(), Buf()]
    outp = [c.psum[2][:, :], c.psum[3][:, :]]
    bout = [Buf(), Buf()]
    dS = [c.psum[4 + i][:, :] for i in range(2)]
    bdS = [Buf() for _ in range(2)]
    tpb = [c.psum[6][:, :].bitcast(BF16)[:, 0:512], c.psum[7][:, :].bitcast(BF16)[:, 0:512]]
    btp = [Buf(), Buf()]
    cnt = {"o": 0, "d": 0, "t": 0, "y": 0, "j": 0}

    def nxt(key, n):
        v = cnt[key] % n
        cnt[key] += 1
        return v

    pend_t = []
    for cb in range(NC):
        b = cb % NB_
        t0 = cb * 128
        tile_i = (cb // 4) % 2
        tb = cb % 4
        SP.dma(qTc[b], qT_d[cb // 4][:, :, tb * 128:(tb + 1) * 128], w=[bin_[b][0]])
        SP.dma(kTc[b], kT_d[cb // 4][:, :, tb * 128:(tb + 1) * 128], w=[bin_[b][1]])
        SP.dma(ktc[b], ktok_d[cb], w=[bin_[b][2]])
        SP.dma(vc[b], v_d[cb], w=[bin_[b][3]])
        SP.dma(gc[b], gs_d[cb], w=[bin_[b][4]])
        par = cb % 2
        for h in range(4):
            for dc in range(2):
                PE.op(lambda e, h=h, dc=dc, b=b: e.matmul(scbk[h % 2], kTc[b][:, 2 * h + dc, :], qTc[b][:, 2 * h + dc, :],
                                                          start=(dc == 0), stop=(dc == 1)),
                      r=[bin_[b][0], bin_[b][1]], w=[bsck[h % 2]], inc=(dc == 1))
            DVE.op(lambda e, h=h: e.tensor_tensor(sT[h], scbk[h % 2], cmask, ALU.mult), r=[bsck[h % 2], bcst], w=[bsT[h]])
            o = nxt("o", 2)
            vh = vc[b][:, h * 512:(h + 1) * 512]
            PE.op(lambda e, h=h, o=o, vh=vh: e.matmul(outp[o], sT[h], vh, start=True, stop=(cb == 0)),
                  r=[bsT[h], bin_[b][3]], w=[bout[o]], inc=(cb == 0))
            if cb > 0:
                for dc in range(2):
                    PE.op(lambda e, h=h, dc=dc, o=o, b=b, par=par: e.matmul(
                        outp[o], qTc[b][:, 2 * h + dc, :], Tb[par][:, h, dc, :], start=False, stop=(dc == 1)),
                        r=[bin_[b][0], bTb[par][h][dc]], w=[bout[o]], inc=(dc == 1))
            ji = nxt("j", 2)
            bj, bss, brs = bnrm[h]
            ACT.op(lambda e, h=h, o=o, ji=ji: e.activation(junk[ji], outp[o], AF.Square, scale=float(512 ** -0.5), accum_out=ss[h]),
                   r=[bout[o]], w=[bj, bss])
            ACT.op(lambda e, h=h: e.activation(rstd[h], ss[h], AF.Ln, bias=EPS), r=[bss], w=[brs])
            ACT.op(lambda e, h=h: e.activation(rstd[h], rstd[h], AF.Exp, scale=-0.5), r=[brs], w=[brs])
            y = nxt("y", 2)
            gh = gc[b][:, h * 512:(h + 1) * 512]
            DVE.op(lambda e, h=h, o=o, y=y, gh=gh: e.scalar_tensor_tensor(yg[y], outp[o], rstd[h], gh, ALU.mult, ALU.mult),
                   r=[bout[o], brs, bin_[b][4]], w=[byg[y]])
            if cb < NC - 1:
                for dc in range(2):
                    d = nxt("d", 2)
                    PE.op(lambda e, h=h, dc=dc, d=d, b=b, vh=vh: e.matmul(
                        dS[d], ktc[b][:, (2 * h + dc) * 128:(2 * h + dc + 1) * 128], vh, start=True, stop=True),
                        r=[bin_[b][2], bin_[b][3]], w=[bdS[d]], inc=True)
                    if cb == 0:
                        DVE.op(lambda e, h=h, dc=dc, d=d: e.tensor_copy(U[:, h, dc, :], dS[d]), r=[bdS[d]], w=[bU[h][dc]])
                    else:
                        DVE.op(lambda e, h=h, dc=dc, d=d: e.scalar_tensor_tensor(
                            U[:, h, dc, :], U[:, h, dc, :], float(g128[h]), dS[d], ALU.mult, ALU.add),
                            r=[bdS[d], bU[h][dc]], w=[bU[h][dc]])
                    ACT.op(lambda e, h=h, dc=dc, par=par: e.mul(Tb[1 - par][:, h, dc, :], U[:, h, dc, :], float(g128[h])),
                           r=[bU[h][dc]], w=[bTb[1 - par][h][dc]])
            if pend_t:
                pend_t.pop(0)()

            def tr(h=h, y=y, tile_i=tile_i, tb=tb):
                t = nxt("t", 2)
                for ec in range(4):
                    PE.op(lambda e, ec=ec, y=y, t=t: e.transpose(tpb[t][:, ec * 128:(ec + 1) * 128], yg[y][:, ec * 128:(ec + 1) * 128], ident),
                          r=[byg[y], bcst], w=[btp[t]], inc=(ec == 3))
                ACT.op(lambda e, h=h, t=t, tile_i=tile_i, tb=tb: e.copy(
                    yT_st[tile_i][:, h * 4:(h + 1) * 4, tb * 128:(tb + 1) * 128], tpb[t].rearrange("p (k n) -> p k n", k=4)),
                    r=[btp[t]], w=[byT[tile_i][h * 4 + tb]])
            pend_t.append(tr)
        if tb == 3:
            while pend_t:
                pend_t.pop(0)()
        if tb == 3:
            for g in range(2):
                POOL.dma(yT_d[cb // 4][:, g * 8:(g + 1) * 8, :], yT_st[tile_i][:, g * 8:(g + 1) * 8, :], r=byT[tile_i])
    c.barrier()
    c.release(mk)


def _consts(S):
    import ml_dtypes
    bf = ml_dtypes.bfloat16
    f32 = np.float32
    j = np.arange(128)
    col = np.arange(512)
    maskb = np.zeros((128, 4, 512), f32)
    for cc in range(4):
        maskb[:, cc, :] = np.where(col[None, :] > cc * 128 + j[:, None], 0.0, -30000.0)
    i = np.arange(128, dtype=f32)
    inv = (f32(10000.0) ** (-(2 * i) / f32(256))).astype(f32)
    pos = np.arange(S, dtype=f32)
    ang = (inv[:, None] * pos[None, :]).astype(f32)
    h = np.arange(4, dtype=np.float64)
    lg = np.log1p(-np.exp2(-5.0 - h))
    n = (np.arange(512) % 128).astype(np.float64)
    dq = np.exp(lg[:, None] * (n[None, :] + 1))
    dk = np.exp(-lg[:, None] * (n[None, :] + 1)) / 16.0
    dqk = np.ascontiguousarray(np.broadcast_to(np.stack([dq, dk], 0).astype(f32)[None], (128, 2, 4, 512)))
    return dict(
        c_ident=np.eye(128, dtype=f32).astype(bf),
        c_negtri=(-(j[:, None] >= j[None, :]).astype(f32)).astype(bf),
        c_negones=(-np.ones((128, 128), f32)).astype(bf),
        c_maskb=maskb.astype(bf),
        c_cmask=(j[:, None] <= j[None, :]).astype(f32),
        c_cos=np.cos(ang).astype(f32), c_sin=np.sin(ang).astype(f32),
        c_dqk=dqk,
    ), [float(v) for v in np.exp(lg * 128)]


WSPEC = [("sb_w_in", [1024, 3072]), ("sb_w_out", [1024, 1024]), ("ret_w_in", [1024, 6144]), ("ret_w_out", [2048, 1024]),
         ("mlp_w_up0", [1024, 4096]), ("mlp_w_down0", [4096, 1024]), ("ple_w_gate0", [1024, 1024]), ("ple_w_up0", [256, 1024]),
         ("mlp_w_up1", [1024, 4096]), ("mlp_w_down1", [4096, 1024]), ("ple_w_gate1", [1024, 1024]), ("ple_w_up1", [256, 1024])]
GSPEC = ["mix_norm0", "mix_norm1", "mlp_norm0", "mlp_norm1", "ple_norm0", "ple_norm1", "final_norm"]


def phase_cast(c, wf, wb, names):
    out = []
    for nm in names:
        src, dst = wf[nm], wb[nm]
        R, C = src.shape
        cw = min(C, 2048)
        for r0 in range(0, R, 128):
            for c0 in range(0, C, cw):
                out.append(lambda d=dst[r0:r0 + 128, c0:c0 + cw], s_=src[r0:r0 + 128, c0:c0 + cw]: c.POOL.dma(d, s_))
    return out


def build(S, g128):
    from contextlib import ExitStack
    nc = bass.Bass("TRN2", target_bir_lowering=False)
    I = lambda n, s, d: nc.dram_tensor(n, s, d, kind="ExternalInput").ap()
    T = lambda n, s, d: nc.dram_tensor(n, s, d).ap()
    x = I("x", [S, 1024], F32)
    p0 = I("p0", [S, 256], F32)
    p1 = I("p1", [S, 256], F32)
    wf = {nm: I(nm, shp, F32) for nm, shp in WSPEC}
    gv = {nm: I(nm, [1024], F32) for nm in GSPEC}
    ident = I("c_ident", [128, 128], BF16)
    negtri = I("c_negtri", [128, 128], BF16)
    negones = I("c_negones", [128, 128], BF16)
    maskb = I("c_maskb", [128, 4, 512], BF16)
    cmask = I("c_cmask", [128, 128], F32)
    cos = I("c_cos", [128, S], F32)
    sin = I("c_sin", [128, S], F32)
    dqk = I("c_dqk", [128, 2, 4, 512], F32)
    y = nc.dram_tensor("y", [S, 1024], F32, kind="ExternalOutput").ap()
    wb = {nm: T("b_" + nm, shp, BF16) for nm, shp in WSPEC if nm != "sb_w_in"}
    NB = S // 128
    qkT = T("s_qkT", [16, 128, S], BF16)
    v0 = T("s_v0", [NB, 128, 1024], BF16)
    oT = T("s_oT", [S // 512, 128, 8, 512], BF16)
    h1 = T("s_h1", [S, 1024], F32)
    qT1 = T("s_qT1", [S // 512, 128, 8, 512], BF16)
    kT1 = T("s_kT1", [S // 512, 128, 8, 512], BF16)
    ktok = T("s_ktok", [NB, 128, 1024], BF16)
    v1 = T("s_v1", [NB, 128, 2048], BF16)
    gs1 = T("s_gs1", [NB, 128, 2048], BF16)
    yT = T("s_yT", [S // 512, 128, 16, 512], BF16)
    with ExitStack() as st:
        c = Ctx(nc, st)
        order = ["sb_w_out", "mlp_w_up0", "mlp_w_down0", "ple_w_gate0", "ple_w_up0", "ret_w_in", "ret_w_out",
                 "mlp_w_up1", "mlp_w_down1", "ple_w_gate1", "ple_w_up1"]
        casts = phase_cast(c, wf, wb, order)
        phase1(c, S, x, gv["mix_norm0"], wf["sb_w_in"], ident, qkT, v0)
        phase2(c, S, qkT, v0, oT, ident, negtri, negones, maskb, bg=casts)
        phase_post(c, S, x, h1, oT, 8, wb["sb_w_out"], gv["mlp_norm0"], wb["mlp_w_up0"], wb["mlp_w_down0"],
                   gv["ple_norm0"], wb["ple_w_gate0"], p0, wb["ple_w_up0"], ident)
        phase_retpre(c, S, h1, gv["mix_norm1"], wb["ret_w_in"], ident, cos, sin, dqk, qT1, kT1, ktok, v1, gs1)
        phase_ret(c, S, qT1, kT1, ktok, v1, gs1, ident, cmask, yT, g128)
        phase_post(c, S, h1, y, yT, 16, wb["ret_w_out"], gv["mlp_norm1"], wb["mlp_w_up1"], wb["mlp_w_down1"],
                   gv["ple_norm1"], wb["ple_w_gate1"], p1, wb["ple_w_up1"], ident, g_final=gv["final_norm"])
        c.emit()
    return nc


def _core_inputs(b, S, x, p, mix_norm, sb_w_in, sb_w_out, ret_w_in, ret_w_out, mlp_norm, mlp_w_up, mlp_w_down,
                 ple_norm, ple_w_gate, ple_w_up, final_norm, consts):
    A = lambda a: np.ascontiguousarray(np.asarray(a, dtype=np.float32))
    m = dict(consts)
    m["x"] = A(x[b, :S])
    m["p0"] = A(p[0, b, :S])
    m["p1"] = A(p[1, b, :S])
    m["sb_w_in"] = A(sb_w_in[0]); m["sb_w_out"] = A(sb_w_out[0])
    m["ret_w_in"] = A(ret_w_in[0]); m["ret_w_out"] = A(ret_w_out[0])
    for l in range(2):
        m["mlp_w_up%d" % l] = A(mlp_w_up[l]); m["mlp_w_down%d" % l] = A(mlp_w_down[l])
        m["ple_w_gate%d" % l] = A(ple_w_gate[l]); m["ple_w_up%d" % l] = A(ple_w_up[l])
        m["mix_norm%d" % l] = A(mix_norm[l]); m["mlp_norm%d" % l] = A(mlp_norm[l]); m["ple_norm%d" % l] = A(ple_norm[l])
    m["final_norm"] = A(final_norm)
    return m


def kernel(x, p, mix_norm, sb_w_in, sb_w_out, ret_w_in, ret_w_out, mlp_norm, mlp_w_up, mlp_w_down,
           ple_norm, ple_w_gate, ple_w_up, final_norm):
    x = np.asarray(x)
    B, S = x.shape[0], x.shape[1]
    consts, g128 = _consts(S)
    nc = build(S, g128)
    args = [np.asarray(a) for a in (p, mix_norm, sb_w_in, sb_w_out, ret_w_in, ret_w_out, mlp_norm, mlp_w_up, mlp_w_down,
                                    ple_norm, ple_w_gate, ple_w_up, final_norm)]
    in_maps = [_core_inputs(b, S, x, *args, consts) for b in range(B)]
    res = run_bass_kernel_spmd(nc, in_maps, core_ids=list(range(B)))
    return np.stack([np.asarray(r["y"], dtype=np.float32) for r in res.results], axis=0)
```

```python
import numpy as np
import concourse.bass as bass
import concourse.mybir as mybir
from concourse.bass_utils import run_bass_kernel_spmd

F32 = mybir.dt.float32
BF16 = mybir.dt.bfloat16
ALU = mybir.AluOpType
AF = mybir.ActivationFunctionType
AX = mybir.AxisListType

EPOCH = 30000
SAME_ENGINE_SYNC = True


class Sem:
    _n = 0

    def __init__(self, handle):
        self.h = handle
        Sem._n += 1
        self.id = Sem._n


class Buf:
    __slots__ = ("name", "lw", "rd")

    def __init__(self, name=""):
        self.name = name
        self.lw = None
        self.rd = []


class Eng:
    def __init__(self, ctx, name, is_pe=False):
        self.ctx = ctx
        self.name = name
        self.same_sync = SAME_ENGINE_SYNC and not is_pe
        self.prog = []
        self.sem = ctx.new_sem(name + "_s0")
        self.count = 0
        self.nsem = 1
        self.waited = {}
        self.pending = False
        self.dma_sems = None
        self.dma_cum = None
        self.dma_i = 0
        self.dma_k = 8

    def _need(self, dep, waits):
        if dep is None:
            return
        sem, val = dep
        if sem is self.sem and not self.same_sync:
            return
        if self.waited.get(sem.id, 0) >= val:
            return
        self.waited[sem.id] = val
        waits[sem.id] = (sem, val)

    def _collect(self, r, w):
        waits = {}
        for b in r:
            self._need(b.lw, waits)
        for b in w:
            self._need(b.lw, waits)
            for d in b.rd:
                self._need(d, waits)
        return list(waits.values())

    def _rotate(self):
        if self.count >= EPOCH:
            self.sem = self.ctx.new_sem("%s_s%d" % (self.name, self.nsem))
            self.nsem += 1
            self.count = 0

    def op(self, fn, r=(), w=(), inc=True):
        waits = self._collect(r, w)
        if inc:
            self._rotate()
            self.count += 1
            sem = self.sem
            dep = (sem, self.count)
            self.pending = False

            def run(eng, fn=fn, waits=waits, sem=sem):
                for s, v in waits:
                    eng.wait_ge(s.h, v)
                fn(eng).then_inc(sem.h, 1)
        else:
            self._rotate()
            dep = (self.sem, self.count + 1)
            self.pending = True

            def run(eng, fn=fn, waits=waits):
                for s, v in waits:
                    eng.wait_ge(s.h, v)
                fn(eng)
        self.prog.append(run)
        for b in w:
            b.lw = dep
            b.rd = []
        for b in r:
            b.rd.append(dep)
        return dep

    def dma(self, out, in_, r=(), w=(), **kw):
        if self.dma_sems is None:
            K = self.dma_k
            self.dma_sems = [self.ctx.new_sem("%s_d%d" % (self.name, i)) for i in range(K)]
            self.dma_cum = [0] * K
        slot = self.dma_i % len(self.dma_sems)
        self.dma_i += 1
        sem = self.dma_sems[slot]
        waits = {}
        if self.dma_cum[slot] > 0:
            self._need((sem, self.dma_cum[slot]), waits)
        for b in r:
            self._need(b.lw, waits)
        for b in w:
            self._need(b.lw, waits)
            for d in b.rd:
                self._need(d, waits)
        waits = list(waits.values())
        self.dma_cum[slot] += 16
        dep = (sem, self.dma_cum[slot])

        def run(eng, waits=waits, sem=sem, out=out, in_=in_, kw=kw):
            for s, v in waits:
                eng.wait_ge(s.h, v)
            eng.dma_start(out=out, in_=in_, **kw).then_inc(sem.h, 16)
        self.prog.append(run)
        for b in w:
            b.lw = dep
            b.rd = []
        for b in r:
            b.rd.append(dep)
        return dep

    def wait_dep(self, dep):
        waits = {}
        self._need(dep, waits)
        waits = list(waits.values())
        if waits:
            def run(eng, waits=waits):
                for s, v in waits:
                    eng.wait_ge(s.h, v)
            self.prog.append(run)

    def cur(self):
        assert not self.pending, self.name
        deps = [(self.sem, self.count)] if self.count > 0 else []
        if self.dma_sems is not None:
            for s, c in zip(self.dma_sems, self.dma_cum):
                if c > 0:
                    deps.append((s, c))
        return deps


class Ctx:
    def __init__(self, nc, stack, sbuf_bytes=190 * 1024):
        self.nc = nc
        self.stack = stack
        self.nsems = 0
        self.PE = Eng(self, "pe", is_pe=True)
        self.ACT = Eng(self, "act")
        self.DVE = Eng(self, "dve")
        self.POOL = Eng(self, "pool")
        self.POOL.dma_k = 4
        self.SP = Eng(self, "sp")
        self.engs = [self.PE, self.ACT, self.DVE, self.POOL, self.SP]
        self.arena_words = sbuf_bytes // 4
        self.arena = stack.enter_context(nc.sbuf_tensor("arena", [128, self.arena_words], F32))
        self.bump = 0
        self.psum = [stack.enter_context(nc.psum_tensor("ps%d" % i, [128, 512], F32)) for i in range(8)]

    def new_sem(self, name):
        self.nsems += 1
        return Sem(self.stack.enter_context(self.nc.semaphore(name)))

    def alloc(self, shape, dtype, name=""):
        esz = 4 if dtype == F32 else 2
        n = int(np.prod(shape[1:]))
        words = (n * esz + 3) // 4
        words = (words + 7) // 8 * 8
        assert self.bump + words <= self.arena_words, ("SBUF overflow", name, self.bump, words)
        ap = self.arena[0:shape[0], self.bump:self.bump + words]
        self.bump += words
        if dtype != F32:
            ap = ap.bitcast(dtype)
        ap = ap[:, 0:n]
        if len(shape) == 3:
            ap = ap.rearrange("p (a b) -> p a b", a=shape[1])
        elif len(shape) == 4:
            ap = ap.rearrange("p (a b c) -> p a b c", a=shape[1], b=shape[2])
        return ap

    def mark(self):
        return self.bump

    def release(self, mark):
        self.bump = mark

    def barrier(self):
        deps = []
        for e in self.engs:
            deps += e.cur()
        for e in self.engs:
            for d in deps:
                e.wait_dep(d)

    def emit(self):
        nc = self.nc
        with nc.Block() as block:
            @block.tensor
            def _(eng):
                for f in self.PE.prog:
                    f(eng)

            @block.scalar
            def _(eng):
                for f in self.ACT.prog:
                    f(eng)

            @block.vector
            def _(eng):
                for f in self.DVE.prog:
                    f(eng)

            @block.gpsimd
            def _(eng):
                for f in self.POOL.prog:
                    f(eng)

            @block.sync
            def _(eng):
                for f in self.SP.prog:
                    f(eng)


EPS = 1e-6
D = 1024


class NormTmp:
    def __init__(self, c, nbuf=4):
        self.n = nbuf
        self.i = 0
        self.junk = [c.alloc([128, D], BF16) for _ in range(2)]
        self.ss = [c.alloc([128, 1], F32) for _ in range(nbuf)]
        self.rstd = [c.alloc([128, 1], F32) for _ in range(nbuf)]
        self.hn = [c.alloc([128, D], BF16) for _ in range(nbuf)]
        self.b = [[Buf() for _ in range(4)] for _ in range(nbuf)]


def norm_s12(c, xt, bx, g, bg, tmp):
    ACT, DVE = c.ACT, c.DVE
    i = tmp.i % tmp.n
    tmp.i += 1
    junk, ss, rstd, hn = tmp.junk[i % len(tmp.junk)], tmp.ss[i], tmp.rstd[i], tmp.hn[i]
    bj, bss, brs, bhn = tmp.b[i]
    bj = tmp.b[i % len(tmp.junk)][0]
    ACT.op(lambda e: e.activation(junk, xt, AF.Square, scale=1.0 / 32.0, accum_out=ss), r=[bx], w=[bj, bss])
    ACT.op(lambda e: e.activation(rstd, ss, AF.Ln, bias=EPS), r=[bss], w=[brs])
    ACT.op(lambda e: e.activation(rstd, rstd, AF.Exp, scale=-0.5), r=[brs], w=[brs])
    DVE.op(lambda e: e.scalar_tensor_tensor(hn, xt, rstd, g, ALU.mult, ALU.mult), r=[bx, brs, bg], w=[bhn])
    return i


def norm_s3(c, i, ident, bid, out3, bout, tmp, tp, btp):
    PE, ACT = c.PE, c.ACT
    hn = tmp.hn[i]
    bhn = tmp.b[i][3]
    for k in range(8):
        PE.op(lambda e, k=k: e.transpose(tp[:, k * 128:(k + 1) * 128], hn[:, k * 128:(k + 1) * 128], ident),
              r=[bhn, bid], w=[btp], inc=(k == 7))
    ACT.op(lambda e: e.copy(out3, tp.rearrange("p (k t) -> p k t", k=8)), r=[btp], w=[bout])


def norm_T(c, xt, bx, g, bg, ident, bid, out3, bout, tmp, tp, btp):
    i = norm_s12(c, xt, bx, g, bg, tmp)
    norm_s3(c, i, ident, bid, out3, bout, tmp, tp, btp)


def phase1(c, S, x, mixg, w_in, ident_d, qkT_d, v_d):
    PE, ACT, DVE, POOL, SP = c.PE, c.ACT, c.DVE, c.POOL, c.SP
    mk = c.mark()
    NT = S // 512
    W = c.alloc([128, 8, 3072], BF16)
    bW = [Buf() for _ in range(8)]
    for k in range(8):
        POOL.dma(W[:, k, :], w_in[k * 128:(k + 1) * 128, :], w=[bW[k]])
    g = c.alloc([128, D], F32)
    bg = Buf()
    SP.dma(g, mixg.partition_broadcast(128), w=[bg])
    ident = c.alloc([128, 128], BF16)
    bid = Buf()
    SP.dma(ident, ident_d, w=[bid])
    tmp = NormTmp(c)
    xt = [c.alloc([128, D], F32) for _ in range(2)]
    bxt = [Buf() for _ in range(2)]
    hnT = [c.alloc([128, 8, 512], BF16) for _ in range(2)]
    bhnT = [[Buf() for _ in range(4)] for _ in range(2)]
    qk_st = [c.alloc([128, 16, 512], BF16) for _ in range(2)]
    bqk = [[Buf() for _ in range(16)] for _ in range(2)]
    v_st = [c.alloc([128, 4, D], BF16) for _ in range(2)]
    bv = [[Buf() for _ in range(8)] for _ in range(2)]
    tps = [c.psum[0][:, :].bitcast(BF16), c.psum[1][:, :].bitcast(BF16)]
    btps = [Buf(), Buf()]
    acc = [c.psum[2 + i][:, :] for i in range(4)]
    bacc = [Buf() for _ in range(4)]
    ai = 0
    xi = 0
    for tt in range(NT):
        p = tt % 2
        for tb in range(4):
            j = xi % 2
            xi += 1
            r0 = tt * 512 + tb * 128
            SP.dma(xt[j], x[r0:r0 + 128, :], w=[bxt[j]])
            norm_T(c, xt[j], bxt[j], g, bg, ident, bid, hnT[p][:, :, tb * 128:(tb + 1) * 128], bhnT[p][tb],
                   tmp, tps[j], btps[j])
        for oc in range(16):
            a = ai % 4
            ai += 1
            for k in range(8):
                PE.op(lambda e, a=a, k=k, oc=oc, p=p: e.matmul(acc[a], W[:, k, oc * 128:(oc + 1) * 128], hnT[p][:, k, :],
                                                               start=(k == 0), stop=(k == 7)),
                      r=[bW[k]] + bhnT[p], w=[bacc[a]], inc=(k == 7))
            sc = 0.125 if oc < 8 else 1.0
            if oc % 2 == 0:
                ACT.op(lambda e, a=a, oc=oc, p=p, sc=sc: e.mul(qk_st[p][:, oc, :], acc[a], sc), r=[bacc[a]], w=[bqk[p][oc]])
            else:
                DVE.op(lambda e, a=a, oc=oc, p=p, sc=sc: e.tensor_scalar(qk_st[p][:, oc, :], acc[a], sc, None, ALU.mult),
                       r=[bacc[a]], w=[bqk[p][oc]])
        SP.dma(qkT_d[:, :, tt * 512:(tt + 1) * 512].rearrange("c p t -> p c t"), qk_st[p], r=bqk[p])
        for tb in range(4):
            for hf in range(2):
                a = ai % 4
                ai += 1
                for k in range(8):
                    PE.op(lambda e, a=a, k=k, tb=tb, hf=hf, p=p: e.matmul(
                        acc[a], hnT[p][:, k, tb * 128:(tb + 1) * 128], W[:, k, 2048 + hf * 512:2048 + (hf + 1) * 512],
                        start=(k == 0), stop=(k == 7)),
                        r=[bW[k], bhnT[p][tb]], w=[bacc[a]], inc=(k == 7))
                if hf == 0:
                    ACT.op(lambda e, a=a, tb=tb, hf=hf, p=p: e.copy(v_st[p][:, tb, hf * 512:(hf + 1) * 512], acc[a]),
                           r=[bacc[a]], w=[bv[p][tb * 2 + hf]])
                else:
                    DVE.op(lambda e, a=a, tb=tb, hf=hf, p=p: e.tensor_copy(v_st[p][:, tb, hf * 512:(hf + 1) * 512], acc[a]),
                           r=[bacc[a]], w=[bv[p][tb * 2 + hf]])
        SP.dma(v_d[tt * 4:(tt + 1) * 4, :, :].rearrange("b p f -> p b f"), v_st[p], r=bv[p])
    c.barrier()
    c.release(mk)


def phase2(c, S, qkT_d, v_d, oT_d, ident_d, negtri_d, negones_d, maskb_d, heads=8, bg=None):
    PE, ACT, DVE, POOL, SP = c.PE, c.ACT, c.DVE, c.POOL, c.SP
    mk = c.mark()
    NQ = S // 512
    NB = S // 128
    ident = c.alloc([128, 128], BF16)
    negtri = c.alloc([128, 128], BF16)
    negones = c.alloc([128, 128], BF16)
    maskb = c.alloc([128, 4, 512], BF16)
    bcst = Buf()
    SP.dma(ident, ident_d, w=[bcst])
    SP.dma(negtri, negtri_d, w=[bcst])
    SP.dma(negones, negones_d, w=[bcst])
    SP.dma(maskb, maskb_d, w=[bcst])
    q2 = [c.alloc([128, 2, S], BF16) for _ in range(2)]
    k2 = [c.alloc([128, S], BF16) for _ in range(2)]
    v2 = [c.alloc([128, NB, 2, 128], BF16) for _ in range(2)]
    bq2 = [Buf() for _ in range(2)]
    bk2 = [Buf() for _ in range(2)]
    bv2 = [Buf() for _ in range(2)]
    for p in range(2):
        POOL.op(lambda e, p=p: e.memset(v2[p], 0.0), w=[bv2[p]])
        POOL.op(lambda e, p=p: e.memset(q2[p], 0.0), w=[bq2[p]])
    oT_st = [c.alloc([128, S], BF16) for _ in range(2)]
    boT = [Buf() for _ in range(2)]
    MAXI = 32
    mm = c.alloc([128, MAXI, 512], BF16)
    bm = [Buf() for _ in range(MAXI)]
    Sbs = c.alloc([128, MAXI, 512], BF16)
    bSb = [Buf() for _ in range(MAXI)]
    Sf = [c.alloc([128, 512], F32) for _ in range(2)]
    bSf = [Buf() for _ in range(2)]
    NA = 3
    aT = [c.alloc([128, 512], BF16) for _ in range(NA)]
    baT = [Buf() for _ in range(NA)]
    Z = [c.psum[i][:, :] for i in range(3)]
    bZ = [Buf() for _ in range(3)]
    E = [c.psum[3 + i][:, :] for i in range(3)]
    bE = [Buf() for _ in range(3)]
    O = [c.psum[6 + i][:, :] for i in range(2)]
    bO = [Buf() for _ in range(2)]
    cnt = {"z": 0, "e": 0, "a": 0}
    bg = list(bg) if bg else []
    nbatch_total = heads * sum((4 * qt + 4 + 15) // 16 for qt in range(NQ))
    bg_per = (len(bg) + nbatch_total - 1) // max(nbatch_total, 1)

    def nxt(key, n):
        v = cnt[key] % n
        cnt[key] += 1
        return v

    for hp in range(heads):
        p = hp % 2
        for e in range(2):
            SP.dma(q2[p][e * 64:(e + 1) * 64, e, :], qkT_d[hp][e * 64:(e + 1) * 64, :], w=[bq2[p]])
        SP.dma(k2[p], qkT_d[8 + hp], w=[bk2[p]])
        for e in range(2):
            SP.dma(v2[p][:, :, e, e * 64:(e + 1) * 64],
                   v_d[:, :, hp * 128 + e * 64: hp * 128 + (e + 1) * 64].rearrange("b p f -> p b f"), w=[bv2[p]])
        for qt in range(NQ):
            top = 4 * qt + 3
            o = qt % 2
            kbs = list(range(top, -1, -1))
            for b0 in range(0, len(kbs), 16):
                batch = kbs[b0:b0 + 16]
                items = [(kb, e) for kb in batch for e in range(2)]
                for _ in range(bg_per):
                    if bg:
                        bg.pop(0)()

                def qk(bank, bbank, kb, e, last_inc, p=p, qt=qt):
                    diag = kb >= 4 * qt
                    PE.op(lambda en: en.matmul(bank, k2[p][:, kb * 128:(kb + 1) * 128],
                                               q2[p][:, e, qt * 512:(qt + 1) * 512],
                                               start=True, stop=(last_inc and not diag)),
                          r=[bq2[p], bk2[p]], w=[bbank], inc=(last_inc and not diag))
                    if diag:
                        PE.op(lambda en: en.matmul(bank, ident, maskb[:, kb - 4 * qt, :], start=False, stop=last_inc),
                              r=[bcst], w=[bbank], inc=last_inc)

                for i, (kb, e) in enumerate(items):
                    z = nxt("z", 3)
                    qk(Z[z], bZ[z], kb, e, True)
                    ACT.op(lambda en, z=z, i=i: en.activation(mm[:, i, :], Z[z], AF.Softplus), r=[bZ[z]], w=[bm[i]])
                    if kb > 0:
                        eng = DVE if e == 0 else POOL
                        if kb == top:
                            eng.op(lambda en, i=i, e=e: en.tensor_copy(Sf[e], mm[:, i, :]), r=[bm[i]], w=[bSf[e]])
                        else:
                            eng.op(lambda en, i=i, e=e: en.tensor_tensor(Sf[e], Sf[e], mm[:, i, :], ALU.add),
                                   r=[bm[i], bSf[e]], w=[bSf[e]])
                        nslot = i + 2 if i + 2 < len(items) else e
                        DVE.op(lambda en, e=e, nslot=nslot: en.tensor_copy(Sbs[:, nslot, :], Sf[e]), r=[bSf[e]], w=[bSb[nslot]])
                pend = []

                def av(item, p=p, o=o, top=top):
                    kb, e, ai = item
                    first = (kb == top) and e == 0
                    last = kb == 0 and e == 1
                    PE.op(lambda en: en.matmul(O[o], v2[p][:, kb, e, :], aT[ai], start=first, stop=last),
                          r=[bv2[p], baT[ai]], w=[bO[o]], inc=last)
                    if last:
                        DVE.op(lambda en, p=p, qt=qt: en.tensor_copy(oT_st[p][:, qt * 512:(qt + 1) * 512], O[o]),
                               r=[bO[o]], w=[boT[p]])

                for i, (kb, e) in enumerate(items):
                    x = nxt("e", 3)
                    first_chain = kb == top
                    qk(E[x], bE[x], kb, e, False)
                    PE.op(lambda en, x=x, i=i, first_chain=first_chain: en.matmul(E[x], negtri, mm[:, i, :], start=False, stop=first_chain),
                          r=[bcst, bm[i]], w=[bE[x]], inc=first_chain)
                    if not first_chain:
                        PE.op(lambda en, x=x, i=i: en.matmul(E[x], negones, Sbs[:, i, :], start=False, stop=True),
                              r=[bcst, bSb[i]], w=[bE[x]], inc=True)
                    ai = nxt("a", NA)
                    ACT.op(lambda en, x=x, ai=ai: en.activation(aT[ai], E[x], AF.Exp), r=[bE[x]], w=[baT[ai]])
                    pend.append((kb, e, ai))
                    if len(pend) > 2:
                        av(pend.pop(0))
                while pend:
                    av(pend.pop(0))
        SP.dma(oT_d[:, :, hp, :].rearrange("t p c -> p t c"), oT_st[p].rearrange("p (t c) -> p t c", c=512), r=[boT[p]])
    while bg:
        bg.pop(0)()
    c.barrier()
    c.release(mk)


def rstd_of(c, xt, bx, junk, bj, ss, bss, rstd, brs):
    ACT = c.ACT
    ACT.op(lambda e: e.activation(junk, xt, AF.Square, scale=1.0 / 32.0, accum_out=ss), r=[bx], w=[bj, bss])
    ACT.op(lambda e: e.activation(rstd, ss, AF.Ln, bias=EPS), r=[bss], w=[brs])
    ACT.op(lambda e: e.activation(rstd, rstd, AF.Exp, scale=-0.5), r=[brs], w=[brs])


def phase_post(c, S, h_in, h_out, oT_d, KO, wb_out, g_mlp, wb_up, wb_down, g_ple, wb_gate, p_d, wb_pu, ident_d,
               g_final=None):
    PE, ACT, DVE, POOL, SP = c.PE, c.ACT, c.DVE, c.POOL, c.SP
    mk = c.mark()
    NT = S // 512
    ident = c.alloc([128, 128], BF16)
    bid = Buf()
    POOL.dma(ident, ident_d, w=[bid])
    gm = c.alloc([128, D], F32)
    gp = c.alloc([128, D], F32)
    bgm, bgp, bgf = Buf(), Buf(), Buf()
    POOL.dma(gm, g_mlp.partition_broadcast(128), w=[bgm])
    POOL.dma(gp, g_ple.partition_broadcast(128), w=[bgp])
    if g_final is not None:
        gf = c.alloc([128, D], F32)
        POOL.dma(gf, g_final.partition_broadcast(128), w=[bgf])
    tmp = NormTmp(c)
    NW = 3
    wr = [c.alloc([128, 8, 1024], BF16) for _ in range(NW)]
    bwr = [Buf() for _ in range(NW)]
    wi = [0]

    def wload(src3):
        s = wi[0] % NW
        wi[0] += 1
        nk = src3.shape[1]
        SP.dma(wr[s][:, 0:nk, :], src3, w=[bwr[s]])
        return wr[s], bwr[s]

    NH = 2
    ht_all = [[c.alloc([128, D], F32) for _ in range(4)] for _ in range(NH)]
    bht_all = [[Buf() for _ in range(4)] for _ in range(NH)]
    oTt = c.alloc([128, KO, 512], BF16)
    boTt = [Buf() for _ in range(KO // 8)]
    hnT = c.alloc([128, 8, 512], BF16)
    bhnT = [Buf() for _ in range(4)]
    aT = c.alloc([128, 32, 512], BF16)
    baT = [Buf() for _ in range(32)]
    rl = [c.alloc([128, 512], F32) for _ in range(2)]
    brl = [Buf() for _ in range(2)]
    gs = [c.alloc([128, 512], F32) for _ in range(2)]
    bgs = [Buf() for _ in range(2)]
    pt = c.alloc([128, 4, 256], F32)
    bpt = Buf()
    pb = c.alloc([128, 4, 256], BF16)
    bpb = Buf()
    pT = [c.alloc([128, 2, 128], BF16) for _ in range(4)]
    bpT = [Buf() for _ in range(4)]
    yo = [c.alloc([128, D], F32) for _ in range(2)]
    byo = [Buf() for _ in range(2)]
    tps = [c.psum[0][:, :].bitcast(BF16), c.psum[1][:, :].bitcast(BF16)]
    btps = [Buf(), Buf()]
    acc = [c.psum[2 + i][:, :] for i in range(6)]
    bacc = [Buf() for _ in range(6)]
    cnt = {"a": 0, "n": 0, "r": 0, "g": 0, "y": 0}

    def nxt(key, n):
        v = cnt[key] % n
        cnt[key] += 1
        return v

    for tt in range(NT):
        t0 = tt * 512
        ht = ht_all[tt % NH]
        bht = bht_all[tt % NH]
        for g in range(KO // 8):
            SP.dma(oTt[:, g * 8:(g + 1) * 8, :], oT_d[tt][:, g * 8:(g + 1) * 8, :], w=[boTt[g]])
        for tb in range(4):
            SP.dma(ht[tb], h_in[t0 + tb * 128:t0 + (tb + 1) * 128, :], w=[bht[tb]])
        for tb in range(4):
            SP.dma(pt[:, tb, :], p_d[t0 + tb * 128:t0 + (tb + 1) * 128, :], w=[bpt])

        def tok_mm(lhs3, blhs, nk, w3, bw, add, after_tb=None, ht=ht, bht=bht):
            for tb in range(4):
                for hf in range(2):
                    a = nxt("a", 6)
                    for k in range(nk):
                        PE.op(lambda e, a=a, k=k, tb=tb, hf=hf: e.matmul(
                            acc[a], lhs3[:, k, tb * 128:(tb + 1) * 128], w3[:, k, hf * 512:(hf + 1) * 512],
                            start=(k == 0), stop=(k == nk - 1)),
                            r=[bw] + blhs(tb), w=[bacc[a]], inc=(k == nk - 1))
                    add(tb, hf, a)
                if after_tb is not None:
                    after_tb(tb)

        def resid_add(tb, hf, a, ht=ht, bht=bht):
            DVE.op(lambda e: e.tensor_tensor(ht[tb][:, hf * 512:(hf + 1) * 512], ht[tb][:, hf * 512:(hf + 1) * 512],
                                             acc[a], ALU.add), r=[bacc[a], bht[tb]], w=[bht[tb]])

        DVE.op(lambda e: e.tensor_copy(pb, pt), r=[bpt], w=[bpb])
        for tb in range(4):
            j = nxt("n", 2)
            for k in range(2):
                PE.op(lambda e, k=k, tb=tb, j=j: e.transpose(tps[j][:, k * 128:(k + 1) * 128], pb[:, tb, k * 128:(k + 1) * 128], ident),
                      r=[bpb, bid], w=[btps[j]], inc=(k == 1))
            ACT.op(lambda e, tb=tb, j=j: e.copy(pT[tb], tps[j][:, 0:256].rearrange("p (k t) -> p k t", k=2)),
                   r=[btps[j]], w=[bpT[tb]])
        nidx = [None] * 4

        def mk_after(gg, gbuf):
            def after(tb, ht=ht, bht=bht):
                nidx[tb] = norm_s12(c, ht[tb], bht[tb], gg, gbuf, tmp)
            return after

        ng = KO // 8
        for g in range(ng):
            w3, bw = wload(wb_out[g * 1024:(g + 1) * 1024, :].rearrange("(k p) c -> p k c", p=128))
            tok_mm(oTt[:, g * 8:(g + 1) * 8, :], lambda tb, g=g: [boTt[g]], 8, w3, bw, resid_add,
                   after_tb=(mk_after(gm, bgm) if g == ng - 1 else None))
        for tb in range(4):
            j = nxt("n", 2)
            norm_s3(c, nidx[tb], ident, bid, hnT[:, :, tb * 128:(tb + 1) * 128], bhnT[tb], tmp, tps[j], btps[j])
        for cg in range(4):
            w3, bw = wload(wb_up[:, cg * 1024:(cg + 1) * 1024].rearrange("(k p) c -> p k c", p=128))
            for oc in range(8):
                a = nxt("a", 6)
                for k in range(8):
                    PE.op(lambda e, a=a, k=k, oc=oc, w3=w3: e.matmul(acc[a], w3[:, k, oc * 128:(oc + 1) * 128], hnT[:, k, :],
                                                                    start=(k == 0), stop=(k == 7)),
                          r=[bw] + bhnT, w=[bacc[a]], inc=(k == 7))
                r_ = nxt("r", 2)
                ACT.op(lambda e, a=a, r_=r_: e.activation(rl[r_], acc[a], AF.Relu), r=[bacc[a]], w=[brl[r_]])
                ff = cg * 8 + oc
                DVE.op(lambda e, r_=r_, ff=ff: e.tensor_tensor(aT[:, ff, :], rl[r_], rl[r_], ALU.mult),
                       r=[brl[r_]], w=[baT[ff]])
        for g in range(4):
            w3, bw = wload(wb_down[g * 1024:(g + 1) * 1024, :].rearrange("(k p) c -> p k c", p=128))
            tok_mm(aT[:, g * 8:(g + 1) * 8, :], lambda tb, g=g: baT[g * 8:(g + 1) * 8], 8, w3, bw, resid_add,
                   after_tb=(mk_after(gp, bgp) if g == 3 else None))
        wg3, bwg = wload(wb_gate.rearrange("(k p) c -> p k c", p=128))
        wp3, bwp = wload(wb_pu.rearrange("(k p) c -> p k c", p=128))
        for tb in range(4):
            j = nxt("n", 2)
            norm_s3(c, nidx[tb], ident, bid, hnT[:, :, tb * 128:(tb + 1) * 128], bhnT[tb], tmp, tps[j], btps[j])
        for tb in range(4):
            for hf in range(2):
                a = nxt("a", 6)
                for k in range(8):
                    PE.op(lambda e, a=a, k=k, tb=tb, hf=hf, wg3=wg3: e.matmul(
                        acc[a], hnT[:, k, tb * 128:(tb + 1) * 128], wg3[:, k, hf * 512:(hf + 1) * 512],
                        start=(k == 0), stop=(k == 7)), r=[bwg, bhnT[tb]], w=[bacc[a]], inc=(k == 7))
                gi = nxt("g", 2)
                ACT.op(lambda e, a=a, gi=gi: e.activation(gs[gi], acc[a], AF.Sigmoid), r=[bacc[a]], w=[bgs[gi]])
                a2 = nxt("a", 6)
                for k in range(2):
                    PE.op(lambda e, a2=a2, k=k, tb=tb, hf=hf, wp3=wp3: e.matmul(
                        acc[a2], pT[tb][:, k, :], wp3[:, k, hf * 512:(hf + 1) * 512],
                        start=(k == 0), stop=(k == 1)), r=[bwp, bpT[tb]], w=[bacc[a2]], inc=(k == 1))
                DVE.op(lambda e, a2=a2, gi=gi: e.tensor_tensor(gs[gi], gs[gi], acc[a2], ALU.mult),
                       r=[bacc[a2], bgs[gi]], w=[bgs[gi]])
                DVE.op(lambda e, gi=gi, tb=tb, hf=hf, ht=ht: e.tensor_tensor(
                    ht[tb][:, hf * 512:(hf + 1) * 512], ht[tb][:, hf * 512:(hf + 1) * 512], gs[gi], ALU.add),
                    r=[bgs[gi], bht[tb]], w=[bht[tb]])
            if g_final is None:
                POOL.dma(h_out[t0 + tb * 128:t0 + (tb + 1) * 128, :], ht[tb], r=[bht[tb]])
            else:
                i = tmp.i % tmp.n
                tmp.i += 1
                _, bss, brs, _ = tmp.b[i]
                bj = tmp.b[i % 2][0]
                rstd_of(c, ht[tb], bht[tb], tmp.junk[i % 2], bj, tmp.ss[i], bss, tmp.rstd[i], brs)
                yi = nxt("y", 2)
                DVE.op(lambda e, i=i, yi=yi, tb=tb, ht=ht: e.scalar_tensor_tensor(yo[yi], ht[tb], tmp.rstd[i], gf, ALU.mult, ALU.mult),
                       r=[bht[tb], brs, bgf], w=[byo[yi]])
                POOL.dma(h_out[t0 + tb * 128:t0 + (tb + 1) * 128, :], yo[yi], r=[byo[yi]])
    c.barrier()
    c.release(mk)


def phase_retpre(c, S, h_in, g_mix, wb_in, ident_d, cos_d, sin_d, dqk_d, qT_d, kT_d, ktok_d, v_d, gs_d):
    PE, ACT, DVE, POOL, SP = c.PE, c.ACT, c.DVE, c.POOL, c.SP
    mk = c.mark()
    NT = S // 512
    ident = c.alloc([128, 128], BF16)
    bid = Buf()
    POOL.dma(ident, ident_d, w=[bid])
    gm = c.alloc([128, D], F32)
    bgm = Buf()
    POOL.dma(gm, g_mix.partition_broadcast(128), w=[bgm])
    dqk = c.alloc([128, 2, 4, 512], F32)
    bdqk = Buf()
    POOL.dma(dqk[:, 0], dqk_d[:, 0], w=[bdqk])
    POOL.dma(dqk[:, 1], dqk_d[:, 1], w=[bdqk])
    tmp = NormTmp(c)
    NW = 3
    wr = [c.alloc([128, 8, 1024], BF16) for _ in range(NW)]
    bwr = [Buf() for _ in range(NW)]
    wi = [0]

    def wload(src3):
        s = wi[0] % NW
        wi[0] += 1
        SP.dma(wr[s], src3, w=[bwr[s]])
        return wr[s], bwr[s]

    ht = [c.alloc([128, D], F32) for _ in range(2)]
    bht = [Buf() for _ in range(2)]
    hnT = c.alloc([128, 8, 512], BF16)
    bhnT = [Buf() for _ in range(4)]
    cs = [c.alloc([128, 2, 512], F32) for _ in range(2)]
    bcs = [Buf() for _ in range(2)]
    x12 = [c.alloc([128, 2, 512], F32) for _ in range(2)]
    bx12 = [[Buf(), Buf()] for _ in range(2)]
    tA = [c.alloc([128, 3, 512], F32)] * 2
    btA = [Buf()] * 2
    tB = [c.alloc([128, 3, 512], F32)] * 2
    btB = [Buf()] * 2
    qk_st = [c.alloc([128, 8, 512], BF16) for _ in range(2)]
    bqk = [[Buf() for _ in range(8)] for _ in range(2)]
    ktok_st = c.alloc([128, 4, 1024], BF16)
    bktok = [Buf() for _ in range(4)]
    vg_st = [c.alloc([128, 4, 2048], BF16) for _ in range(2)]
    bvg = [[Buf() for _ in range(16)] for _ in range(2)]
    tps = [c.psum[0][:, :].bitcast(BF16), c.psum[1][:, :].bitcast(BF16)]
    btps = [Buf(), Buf()]
    acc = [c.psum[2 + i][:, :] for i in range(6)]
    bacc = [Buf() for _ in range(6)]
    cnt = {"a": 0, "n": 0, "x": 0, "h": 0}

    def nxt(key, n):
        v = cnt[key] % n
        cnt[key] += 1
        return v

    for tt in range(NT):
        t0 = tt * 512
        ci = tt % 2
        POOL.dma(cs[ci][:, 0, :], cos_d[:, t0:t0 + 512], w=[bcs[ci]])
        POOL.dma(cs[ci][:, 1, :], sin_d[:, t0:t0 + 512], w=[bcs[ci]])
        for tb in range(4):
            hi = nxt("h", 2)
            POOL.dma(ht[hi], h_in[t0 + tb * 128:t0 + (tb + 1) * 128, :], w=[bht[hi]])
            j = nxt("n", 2)
            norm_T(c, ht[hi], bht[hi], gm, bgm, ident, bid, hnT[:, :, tb * 128:(tb + 1) * 128], bhnT[tb], tmp,
                   tps[j], btps[j])
        cos_t = cs[ci][:, 0, :]
        sin_t = cs[ci][:, 1, :]
        for which in range(2):
            w3, bw = wload(wb_in[:, which * 1024:(which + 1) * 1024].rearrange("(k p) c -> p k c", p=128))
            st = qk_st[which]
            for h in range(4):
                xi = nxt("x", 2)
                for half in range(2):
                    oc = 2 * h + half
                    a = nxt("a", 6)
                    for k in range(8):
                        PE.op(lambda e, a=a, k=k, oc=oc, w3=w3: e.matmul(acc[a], w3[:, k, oc * 128:(oc + 1) * 128], hnT[:, k, :],
                                                                        start=(k == 0), stop=(k == 7)),
                              r=[bw] + bhnT, w=[bacc[a]], inc=(k == 7))
                    ACT.op(lambda e, a=a, xi=xi, half=half: e.copy(x12[xi][:, half, :], acc[a]),
                           r=[bacc[a]], w=[bx12[xi][half]])
                x1 = x12[xi][:, 0, :]
                x2 = x12[xi][:, 1, :]
                dtab = dqk[:, which, h, :]
                ta, tb_ = tA[xi], tB[xi]
                rd = [bx12[xi][0], bx12[xi][1], bcs[ci], bdqk]
                DVE.op(lambda e, ta=ta, x1=x1, cos_t=cos_t: e.tensor_tensor(ta[:, 0, :], x1, cos_t, ALU.mult), r=rd, w=[btA[xi]])
                DVE.op(lambda e, ta=ta, x2=x2, sin_t=sin_t: e.tensor_tensor(ta[:, 1, :], x2, sin_t, ALU.mult), r=rd, w=[btA[xi]])
                DVE.op(lambda e, ta=ta: e.tensor_tensor(ta[:, 2, :], ta[:, 0, :], ta[:, 1, :], ALU.subtract), r=[btA[xi]], w=[btA[xi]])
                DVE.op(lambda e, ta=ta, st=st, h=h, dtab=dtab: e.tensor_tensor(st[:, 2 * h, :], ta[:, 2, :], dtab, ALU.mult),
                       r=[btA[xi], bdqk], w=[bqk[which][2 * h]])
                POOL.op(lambda e, tb_=tb_, x1=x1, sin_t=sin_t: e.tensor_tensor(tb_[:, 0, :], x1, sin_t, ALU.mult), r=rd, w=[btB[xi]])
                POOL.op(lambda e, tb_=tb_, x2=x2, cos_t=cos_t: e.tensor_tensor(tb_[:, 1, :], x2, cos_t, ALU.mult), r=rd, w=[btB[xi]])
                POOL.op(lambda e, tb_=tb_: e.tensor_tensor(tb_[:, 2, :], tb_[:, 0, :], tb_[:, 1, :], ALU.add), r=[btB[xi]], w=[btB[xi]])
                POOL.op(lambda e, tb_=tb_, st=st, h=h, dtab=dtab: e.tensor_tensor(st[:, 2 * h + 1, :], tb_[:, 2, :], dtab, ALU.mult),
                        r=[btB[xi], bdqk], w=[bqk[which][2 * h + 1]])
            dst = qT_d if which == 0 else kT_d
            POOL.dma(dst[tt], st, r=bqk[which])
            if which == 1:
                for tb in range(4):
                    j = nxt("n", 2)
                    for k in range(8):
                        PE.op(lambda e, k=k, tb=tb, j=j, st=st: e.transpose(tps[j][:, k * 128:(k + 1) * 128],
                                                                     st[:, k, tb * 128:(tb + 1) * 128], ident),
                              r=[bqk[1][k], bid], w=[btps[j]], inc=(k == 7))
                    ACT.op(lambda e, tb=tb, j=j: e.copy(ktok_st[:, tb, :], tps[j]), r=[btps[j]], w=[bktok[tb]])
                for tb in range(4):
                    POOL.dma(ktok_d[tt * 4 + tb], ktok_st[:, tb, :], r=[bktok[tb]])
        for which in range(2):
            st = vg_st[which]
            for g in range(2):
                c0 = 2048 + which * 2048 + g * 1024
                w3, bw = wload(wb_in[:, c0:c0 + 1024].rearrange("(k p) c -> p k c", p=128))
                for tb in range(4):
                    for hf in range(2):
                        a = nxt("a", 6)
                        for k in range(8):
                            PE.op(lambda e, a=a, k=k, tb=tb, hf=hf, w3=w3: e.matmul(
                                acc[a], hnT[:, k, tb * 128:(tb + 1) * 128], w3[:, k, hf * 512:(hf + 1) * 512],
                                start=(k == 0), stop=(k == 7)), r=[bw, bhnT[tb]], w=[bacc[a]], inc=(k == 7))
                        col = g * 1024 + hf * 512
                        bi = tb * 4 + g * 2 + hf
                        if which == 1:
                            ACT.op(lambda e, a=a, tb=tb, col=col, st=st: e.activation(st[:, tb, col:col + 512], acc[a], AF.Silu),
                                   r=[bacc[a]], w=[bvg[which][bi]])
                        else:
                            DVE.op(lambda e, a=a, tb=tb, col=col, st=st: e.tensor_copy(st[:, tb, col:col + 512], acc[a]),
                                   r=[bacc[a]], w=[bvg[which][bi]])
            dst = v_d if which == 0 else gs_d
            for tb in range(4):
                POOL.dma(dst[tt * 4 + tb], st[:, tb, :], r=bvg[which][tb * 4:(tb + 1) * 4])
    c.barrier()
    c.release(mk)


def phase_ret(c, S, qT_d, kT_d, ktok_d, v_d, gs_d, ident_d, cmask_d, yT_d, g128):
    PE, ACT, DVE, POOL, SP = c.PE, c.ACT, c.DVE, c.POOL, c.SP
    mk = c.mark()
    NC = S // 128
    ident = c.alloc([128, 128], BF16)
    cmask = c.alloc([128, 128], F32)
    bcst = Buf()
    SP.dma(ident, ident_d, w=[bcst])
    SP.dma(cmask, cmask_d, w=[bcst])
    NB_ = 3
    qTc = [c.alloc([128, 8, 128], BF16) for _ in range(NB_)]
    kTc = [c.alloc([128, 8, 128], BF16) for _ in range(NB_)]
    ktc = [c.alloc([128, 1024], BF16) for _ in range(NB_)]
    vc = [c.alloc([128, 2048], BF16) for _ in range(NB_)]
    gc = [c.alloc([128, 2048], BF16) for _ in range(NB_)]
    bin_ = [[Buf() for _ in range(5)] for _ in range(NB_)]
    U = c.alloc([128, 4, 2, 512], F32)
    bU = [[Buf(), Buf()] for _ in range(4)]
    Tb = [c.alloc([128, 4, 2, 512], BF16) for _ in range(2)]
    bTb = [[[Buf(), Buf()] for _ in range(4)] for _ in range(2)]
    sT = [c.alloc([128, 128], BF16) for _ in range(4)]
    bsT = [Buf() for _ in range(4)]
    junk = [c.alloc([128, 512], BF16) for _ in range(2)]
    ss = [c.alloc([128, 1], F32) for _ in range(4)]
    rstd = [c.alloc([128, 1], F32) for _ in range(4)]
    bnrm = [[Buf(), Buf(), Buf()] for _ in range(4)]
    yg = [c.alloc([128, 512], BF16) for _ in range(2)]
    byg = [Buf() for _ in range(2)]
    yT_st = [c.alloc([128, 16, 512], BF16) for _ in range(2)]
    byT = [[Buf() for _ in range(16)] for _ in range(2)]
    scbk = [c.psum[0][:, 0:128], c.psum[1][:, 0:128]]
    bsck = [Buf(), Buf()]
    outp = [c.psum[2][:, :], c.psum[3][:, :]]
    bout = [Buf(), Buf()]
    dS = [c.psum[4 + i][:, :] for i in range(2)]
    bdS = [Buf() for _ in range(2)]
    tpb = [c.psum[6][:, :].bitcast(BF16)[:, 0:512], c.psum[7][:, :].bitcast(BF16)[:, 0:512]]
    btp = [Buf(), Buf()]
    cnt = {"o": 0, "d": 0, "t": 0, "y": 0, "j": 0}

    def nxt(key, n):
        v = cnt[key] % n
        cnt[key] += 1
        return v

    pend_t = []
    for cb in range(NC):
        b = cb % NB_
        t0 = cb * 128
        tile_i = (cb // 4) % 2
        tb = cb % 4
        SP.dma(qTc[b], qT_d[cb // 4][:, :, tb * 128:(tb + 1) * 128], w=[bin_[b][0]])
        SP.dma(kTc[b], kT_d[cb // 4][:, :, tb * 128:(tb + 1) * 128], w=[bin_[b][1]])
        SP.dma(ktc[b], ktok_d[cb], w=[bin_[b][2]])
        SP.dma(vc[b], v_d[cb], w=[bin_[b][3]])
        SP.dma(gc[b], gs_d[cb], w=[bin_[b][4]])
        par = cb % 2
        for h in range(4):
            for dc in range(2):
                PE.op(lambda e, h=h, dc=dc, b=b: e.matmul(scbk[h % 2], kTc[b][:, 2 * h + dc, :], qTc[b][:, 2 * h + dc, :],
                                                          start=(dc == 0), stop=(dc == 1)),
                      r=[bin_[b][0], bin_[b][1]], w=[bsck[h % 2]], inc=(dc == 1))
            DVE.op(lambda e, h=h: e.tensor_tensor(sT[h], scbk[h % 2], cmask, ALU.mult), r=[bsck[h % 2], bcst], w=[bsT[h]])
            o = nxt("o", 2)
            vh = vc[b][:, h * 512:(h + 1) * 512]
            PE.op(lambda e, h=h, o=o, vh=vh: e.matmul(outp[o], sT[h], vh, start=True, stop=(cb == 0)),
                  r=[bsT[h], bin_[b][3]], w=[bout[o]], inc=(cb == 0))
            if cb > 0:
                for dc in range(2):
                    PE.op(lambda e, h=h, dc=dc, o=o, b=b, par=par: e.matmul(
                        outp[o], qTc[b][:, 2 * h + dc, :], Tb[par][:, h, dc, :], start=False, stop=(dc == 1)),
                        r=[bin_[b][0], bTb[par][h][dc]], w=[bout[o]], inc=(dc == 1))
            ji = nxt("j", 2)
            bj, bss, brs = bnrm[h]
            ACT.op(lambda e, h=h, o=o, ji=ji: e.activation(junk[ji], outp[o], AF.Square, scale=float(512 ** -0.5), accum_out=ss[h]),
                   r=[bout[o]], w=[bj, bss])
            ACT.op(lambda e, h=h: e.activation(rstd[h], ss[h], AF.Ln, bias=EPS), r=[bss], w=[brs])
            ACT.op(lambda e, h=h: e.activation(rstd[h], rstd[h], AF.Exp, scale=-0.5), r=[brs], w=[brs])
            y = nxt("y", 2)
            gh = gc[b][:, h * 512:(h + 1) * 512]
            DVE.op(lambda e, h=h, o=o, y=y, gh=gh: e.scalar_tensor_tensor(yg[y], outp[o], rstd[h], gh, ALU.mult, ALU.mult),
                   r=[bout[o], brs, bin_[b][4]], w=[byg[y]])
            if cb < NC - 1:
                for dc in range(2):
                    d = nxt("d", 2)
                    PE.op(lambda e, h=h, dc=dc, d=d, b=b, vh=vh: e.matmul(
                        dS[d], ktc[b][:, (2 * h + dc) * 128:(2 * h + dc + 1) * 128], vh, start=True, stop=True),
                        r=[bin_[b][2], bin_[b][3]], w=[bdS[d]], inc=True)
                    if cb == 0:
                        DVE.op(lambda e, h=h, dc=dc, d=d: e.tensor_copy(U[:, h, dc, :], dS[d]), r=[bdS[d]], w=[bU[h][dc]])
                    else:
                        DVE.op(lambda e, h=h, dc=dc, d=d: e.scalar_tensor_tensor(
                            U[:, h, dc, :], U[:, h, dc, :], float(g128[h]), dS[d], ALU.mult, ALU.add),
                            r=[bdS[d], bU[h][dc]], w=[bU[h][dc]])
                    ACT.op(lambda e, h=h, dc=dc, par=par: e.mul(Tb[1 - par][:, h, dc, :], U[:, h, dc, :], float(g128[h])),
                           r=[bU[h][dc]], w=[bTb[1 - par][h][dc]])
            if pend_t:
                pend_t.pop(0)()

            def tr(h=h, y=y, tile_i=tile_i, tb=tb):
                t = nxt("t", 2)
                for ec in range(4):
                    PE.op(lambda e, ec=ec, y=y, t=t: e.transpose(tpb[t][:, ec * 128:(ec + 1) * 128], yg[y][:, ec * 128:(ec + 1) * 128], ident),
                          r=[byg[y], bcst], w=[btp[t]], inc=(ec == 3))
                ACT.op(lambda e, h=h, t=t, tile_i=tile_i, tb=tb: e.copy(
                    yT_st[tile_i][:, h * 4:(h + 1) * 4, tb * 128:(tb + 1) * 128], tpb[t].rearrange("p (k n) -> p k n", k=4)),
                    r=[btp[t]], w=[byT[tile_i][h * 4 + tb]])
            pend_t.append(tr)
        if tb == 3:
            while pend_t:
                pend_t.pop(0)()
        if tb == 3:
            for g in range(2):
                POOL.dma(yT_d[cb // 4][:, g * 8:(g + 1) * 8, :], yT_st[tile_i][:, g * 8:(g + 1) * 8, :], r=byT[tile_i])
    c.barrier()
    c.release(mk)


def _consts(S):
    import ml_dtypes
    bf = ml_dtypes.bfloat16
    f32 = np.float32
    j = np.arange(128)
    col = np.arange(512)
    maskb = np.zeros((128, 4, 512), f32)
    for cc in range(4):
        maskb[:, cc, :] = np.where(col[None, :] > cc * 128 + j[:, None], 0.0, -30000.0)
    i = np.arange(128, dtype=f32)
    inv = (f32(10000.0) ** (-(2 * i) / f32(256))).astype(f32)
    pos = np.arange(S, dtype=f32)
    ang = (inv[:, None] * pos[None, :]).astype(f32)
    h = np.arange(4, dtype=np.float64)
    lg = np.log1p(-np.exp2(-5.0 - h))
    n = (np.arange(512) % 128).astype(np.float64)
    dq = np.exp(lg[:, None] * (n[None, :] + 1))
    dk = np.exp(-lg[:, None] * (n[None, :] + 1)) / 16.0
    dqk = np.ascontiguousarray(np.broadcast_to(np.stack([dq, dk], 0).astype(f32)[None], (128, 2, 4, 512)))
    return dict(
        c_ident=np.eye(128, dtype=f32).astype(bf),
        c_negtri=(-(j[:, None] >= j[None, :]).astype(f32)).astype(bf),
        c_negones=(-np.ones((128, 128), f32)).astype(bf),
        c_maskb=maskb.astype(bf),
        c_cmask=(j[:, None] <= j[None, :]).astype(f32),
        c_cos=np.cos(ang).astype(f32), c_sin=np.sin(ang).astype(f32),
        c_dqk=dqk,
    ), [float(v) for v in np.exp(lg * 128)]


WSPEC = [("sb_w_in", [1024, 3072]), ("sb_w_out", [1024, 1024]), ("ret_w_in", [1024, 6144]), ("ret_w_out", [2048, 1024]),
         ("mlp_w_up0", [1024, 4096]), ("mlp_w_down0", [4096, 1024]), ("ple_w_gate0", [1024, 1024]), ("ple_w_up0", [256, 1024]),
         ("mlp_w_up1", [1024, 4096]), ("mlp_w_down1", [4096, 1024]), ("ple_w_gate1", [1024, 1024]), ("ple_w_up1", [256, 1024])]
GSPEC = ["mix_norm0", "mix_norm1", "mlp_norm0", "mlp_norm1", "ple_norm0", "ple_norm1", "final_norm"]


def phase_cast(c, wf, wb, names):
    out = []
    for nm in names:
        src, dst = wf[nm], wb[nm]
        R, C = src.shape
        cw = min(C, 2048)
        for r0 in range(0, R, 128):
            for c0 in range(0, C, cw):
                out.append(lambda d=dst[r0:r0 + 128, c0:c0 + cw], s_=src[r0:r0 + 128, c0:c0 + cw]: c.POOL.dma(d, s_))
    return out


def build(S, g128):
    from contextlib import ExitStack
    nc = bass.Bass("TRN2", target_bir_lowering=False)
    I = lambda n, s, d: nc.dram_tensor(n, s, d, kind="ExternalInput").ap()
    T = lambda n, s, d: nc.dram_tensor(n, s, d).ap()
    x = I("x", [S, 1024], F32)
    p0 = I("p0", [S, 256], F32)
    p1 = I("p1", [S, 256], F32)
    wf = {nm: I(nm, shp, F32) for nm, shp in WSPEC}
    gv = {nm: I(nm, [1024], F32) for nm in GSPEC}
    ident = I("c_ident", [128, 128], BF16)
    negtri = I("c_negtri", [128, 128], BF16)
    negones = I("c_negones", [128, 128], BF16)
    maskb = I("c_maskb", [128, 4, 512], BF16)
    cmask = I("c_cmask", [128, 128], F32)
    cos = I("c_cos", [128, S], F32)
    sin = I("c_sin", [128, S], F32)
    dqk = I("c_dqk", [128, 2, 4, 512], F32)
    y = nc.dram_tensor("y", [S, 1024], F32, kind="ExternalOutput").ap()
    wb = {nm: T("b_" + nm, shp, BF16) for nm, shp in WSPEC if nm != "sb_w_in"}
    NB = S // 128
    qkT = T("s_qkT", [16, 128, S], BF16)
    v0 = T("s_v0", [NB, 128, 1024], BF16)
    oT = T("s_oT", [S // 512, 128, 8, 512], BF16)
    h1 = T("s_h1", [S, 1024], F32)
    qT1 = T("s_qT1", [S // 512, 128, 8, 512], BF16)
    kT1 = T("s_kT1", [S // 512, 128, 8, 512], BF16)
    ktok = T("s_ktok", [NB, 128, 1024], BF16)
    v1 = T("s_v1", [NB, 128, 2048], BF16)
    gs1 = T("s_gs1", [NB, 128, 2048], BF16)
    yT = T("s_yT", [S // 512, 128, 16, 512], BF16)
    with ExitStack() as st:
        c = Ctx(nc, st)
        order = ["sb_w_out", "mlp_w_up0", "mlp_w_down0", "ple_w_gate0", "ple_w_up0", "ret_w_in", "ret_w_out",
                 "mlp_w_up1", "mlp_w_down1", "ple_w_gate1", "ple_w_up1"]
        casts = phase_cast(c, wf, wb, order)
        phase1(c, S, x, gv["mix_norm0"], wf["sb_w_in"], ident, qkT, v0)
        phase2(c, S, qkT, v0, oT, ident, negtri, negones, maskb, bg=casts)
        phase_post(c, S, x, h1, oT, 8, wb["sb_w_out"], gv["mlp_norm0"], wb["mlp_w_up0"], wb["mlp_w_down0"],
                   gv["ple_norm0"], wb["ple_w_gate0"], p0, wb["ple_w_up0"], ident)
        phase_retpre(c, S, h1, gv["mix_norm1"], wb["ret_w_in"], ident, cos, sin, dqk, qT1, kT1, ktok, v1, gs1)
        phase_ret(c, S, qT1, kT1, ktok, v1, gs1, ident, cmask, yT, g128)
        phase_post(c, S, h1, y, yT, 16, wb["ret_w_out"], gv["mlp_norm1"], wb["mlp_w_up1"], wb["mlp_w_down1"],
                   gv["ple_norm1"], wb["ple_w_gate1"], p1, wb["ple_w_up1"], ident, g_final=gv["final_norm"])
        c.emit()
    return nc


def _core_inputs(b, S, x, p, mix_norm, sb_w_in, sb_w_out, ret_w_in, ret_w_out, mlp_norm, mlp_w_up, mlp_w_down,
                 ple_norm, ple_w_gate, ple_w_up, final_norm, consts):
    A = lambda a: np.ascontiguousarray(np.asarray(a, dtype=np.float32))
    m = dict(consts)
    m["x"] = A(x[b, :S])
    m["p0"] = A(p[0, b, :S])
    m["p1"] = A(p[1, b, :S])
    m["sb_w_in"] = A(sb_w_in[0]); m["sb_w_out"] = A(sb_w_out[0])
    m["ret_w_in"] = A(ret_w_in[0]); m["ret_w_out"] = A(ret_w_out[0])
    for l in range(2):
        m["mlp_w_up%d" % l] = A(mlp_w_up[l]); m["mlp_w_down%d" % l] = A(mlp_w_down[l])
        m["ple_w_gate%d" % l] = A(ple_w_gate[l]); m["ple_w_up%d" % l] = A(ple_w_up[l])
        m["mix_norm%d" % l] = A(mix_norm[l]); m["mlp_norm%d" % l] = A(mlp_norm[l]); m["ple_norm%d" % l] = A(ple_norm[l])
    m["final_norm"] = A(final_norm)
    return m


def kernel(x, p, mix_norm, sb_w_in, sb_w_out, ret_w_in, ret_w_out, mlp_norm, mlp_w_up, mlp_w_down,
           ple_norm, ple_w_gate, ple_w_up, final_norm):
    x = np.asarray(x)
    B, S = x.shape[0], x.shape[1]
    consts, g128 = _consts(S)
    nc = build(S, g128)
    args = [np.asarray(a) for a in (p, mix_norm, sb_w_in, sb_w_out, ret_w_in, ret_w_out, mlp_norm, mlp_w_up, mlp_w_down,
                                    ple_norm, ple_w_gate, ple_w_up, final_norm)]
    in_maps = [_core_inputs(b, S, x, *args, consts) for b in range(B)]
    res = run_bass_kernel_spmd(nc, in_maps, core_ids=list(range(B)))
    return np.stack([np.asarray(r["y"], dtype=np.float32) for r in res.results], axis=0)
```

```python
import numpy as np
import concourse.bass as bass
import concourse.mybir as mybir
from concourse.bass_utils import run_bass_kernel_spmd

F32 = mybir.dt.float32
BF16 = mybir.dt.bfloat16
ALU = mybir.AluOpType
AF = mybir.ActivationFunctionType
AX = mybir.AxisListType

EPOCH = 30000
SAME_ENGINE_SYNC = True


class Sem:
    _n = 0

    def __init__(self, handle):
        self.h = handle
        Sem._n += 1
        self.id = Sem._n


class Buf:
    __slots__ = ("name", "lw", "rd")

    def __init__(self, name=""):
        self.name = name
        self.lw = None
        self.rd = []


class Eng:
    def __init__(self, ctx, name, is_pe=False):
        self.ctx = ctx
        self.name = name
        self.same_sync = SAME_ENGINE_SYNC and not is_pe
        self.prog = []
        self.sem = ctx.new_sem(name + "_s0")
        self.count = 0
        self.nsem = 1
        self.waited = {}
        self.pending = False
        self.dma_sems = None
        self.dma_cum = None
        self.dma_i = 0
        self.dma_k = 8

    def _need(self, dep, waits):
        if dep is None:
            return
        sem, val = dep
        if sem is self.sem and not self.same_sync:
            return
        if self.waited.get(sem.id, 0) >= val:
            return
        self.waited[sem.id] = val
        waits[sem.id] = (sem, val)

    def _collect(self, r, w):
        waits = {}
        for b in r:
            self._need(b.lw, waits)
        for b in w:
            self._need(b.lw, waits)
            for d in b.rd:
                self._need(d, waits)
        return list(waits.values())

    def _rotate(self):
        if self.count >= EPOCH:
            self.sem = self.ctx.new_sem("%s_s%d" % (self.name, self.nsem))
            self.nsem += 1
            self.count = 0

    def op(self, fn, r=(), w=(), inc=True):
        waits = self._collect(r, w)
        if inc:
            self._rotate()
            self.count += 1
            sem = self.sem
            dep = (sem, self.count)
            self.pending = False

            def run(eng, fn=fn, waits=waits, sem=sem):
                for s, v in waits:
                    eng.wait_ge(s.h, v)
                fn(eng).then_inc(sem.h, 1)
        else:
            self._rotate()
            dep = (self.sem, self.count + 1)
            self.pending = True

            def run(eng, fn=fn, waits=waits):
                for s, v in waits:
                    eng.wait_ge(s.h, v)
                fn(eng)
        self.prog.append(run)
        for b in w:
            b.lw = dep
            b.rd = []
        for b in r:
            b.rd.append(dep)
        return dep

    def dma(self, out, in_, r=(), w=(), **kw):
        if self.dma_sems is None:
            K = self.dma_k
            self.dma_sems = [self.ctx.new_sem("%s_d%d" % (self.name, i)) for i in range(K)]
            self.dma_cum = [0] * K
        slot = self.dma_i % len(self.dma_sems)
        self.dma_i += 1
        sem = self.dma_sems[slot]
        waits = {}
        if self.dma_cum[slot] > 0:
            self._need((sem, self.dma_cum[slot]), waits)
        for b in r:
            self._need(b.lw, waits)
        for b in w:
            self._need(b.lw, waits)
            for d in b.rd:
                self._need(d, waits)
        waits = list(waits.values())
        self.dma_cum[slot] += 16
        dep = (sem, self.dma_cum[slot])

        def run(eng, waits=waits, sem=sem, out=out, in_=in_, kw=kw):
            for s, v in waits:
                eng.wait_ge(s.h, v)
            eng.dma_start(out=out, in_=in_, **kw).then_inc(sem.h, 16)
        self.prog.append(run)
        for b in w:
            b.lw = dep
            b.rd = []
        for b in r:
            b.rd.append(dep)
        return dep

    def wait_dep(self, dep):
        waits = {}
        self._need(dep, waits)
        waits = list(waits.values())
        if waits:
            def run(eng, waits=waits):
                for s, v in waits:
                    eng.wait_ge(s.h, v)
            self.prog.append(run)

    def cur(self):
        assert not self.pending, self.name
        deps = [(self.sem, self.count)] if self.count > 0 else []
        if self.dma_sems is not None:
            for s, c in zip(self.dma_sems, self.dma_cum):
                if c > 0:
                    deps.append((s, c))
        return deps


class Ctx:
    def __init__(self, nc, stack, sbuf_bytes=190 * 1024):
        self.nc = nc
        self.stack = stack
        self.nsems = 0
        self.PE = Eng(self, "pe", is_pe=True)
        self.ACT = Eng(self, "act")
        self.DVE = Eng(self, "dve")
        self.POOL = Eng(self, "pool")
        self.POOL.dma_k = 4
        self.SP = Eng(self, "sp")
        self.engs = [self.PE, self.ACT, self.DVE, self.POOL, self.SP]
        self.arena_words = sbuf_bytes // 4
        self.arena = stack.enter_context(nc.sbuf_tensor("arena", [128, self.arena_words], F32))
        self.bump = 0
        self.psum = [stack.enter_context(nc.psum_tensor("ps%d" % i, [128, 512], F32)) for i in range(8)]

    def new_sem(self, name):
        self.nsems += 1
        return Sem(self.stack.enter_context(self.nc.semaphore(name)))

    def alloc(self, shape, dtype, name=""):
        esz = 4 if dtype == F32 else 2
        n = int(np.prod(shape[1:]))
        words = (n * esz + 3) // 4
        words = (words + 7) // 8 * 8
        assert self.bump + words <= self.arena_words, ("SBUF overflow", name, self.bump, words)
        ap = self.arena[0:shape[0], self.bump:self.bump + words]
        self.bump += words
        if dtype != F32:
            ap = ap.bitcast(dtype)
        ap = ap[:, 0:n]
        if len(shape) == 3:
            ap = ap.rearrange("p (a b) -> p a b", a=shape[1])
        elif len(shape) == 4:
            ap = ap.rearrange("p (a b c) -> p a b c", a=shape[1], b=shape[2])
        return ap

    def mark(self):
        return self.bump

    def release(self, mark):
        self.bump = mark

    def barrier(self):
        deps = []
        for e in self.engs:
            deps += e.cur()
        for e in self.engs:
            for d in deps:
                e.wait_dep(d)

    def emit(self):
        nc = self.nc
        with nc.Block() as block:
            @block.tensor
            def _(eng):
                for f in self.PE.prog:
                    f(eng)

            @block.scalar
            def _(eng):
                for f in self.ACT.prog:
                    f(eng)

            @block.vector
            def _(eng):
                for f in self.DVE.prog:
                    f(eng)

            @block.gpsimd
            def _(eng):
                for f in self.POOL.prog:
                    f(eng)

            @block.sync
            def _(eng):
                for f in self.SP.prog:
                    f(eng)


EPS = 1e-6
D = 1024


class NormTmp:
    def __init__(self, c, nbuf=4):
        self.n = nbuf
        self.i = 0
        self.junk = [c.alloc([128, D], BF16) for _ in range(2)]
        self.ss = [c.alloc([128, 1], F32) for _ in range(nbuf)]
        self.rstd = [c.alloc([128, 1], F32) for _ in range(nbuf)]
        self.hn = [c.alloc([128, D], BF16) for _ in range(nbuf)]
        self.b = [[Buf() for _ in range(4)] for _ in range(nbuf)]


def norm_s12(c, xt, bx, g, bg, tmp):
    ACT, DVE = c.ACT, c.DVE
    i = tmp.i % tmp.n
    tmp.i += 1
    junk, ss, rstd, hn = tmp.junk[i % len(tmp.junk)], tmp.ss[i], tmp.rstd[i], tmp.hn[i]
    bj, bss, brs, bhn = tmp.b[i]
    bj = tmp.b[i % len(tmp.junk)][0]
    ACT.op(lambda e: e.activation(junk, xt, AF.Square, scale=1.0 / 32.0, accum_out=ss), r=[bx], w=[bj, bss])
    ACT.op(lambda e: e.activation(rstd, ss, AF.Ln, bias=EPS), r=[bss], w=[brs])
    ACT.op(lambda e: e.activation(rstd, rstd, AF.Exp, scale=-0.5), r=[brs], w=[brs])
    DVE.op(lambda e: e.scalar_tensor_tensor(hn, xt, rstd, g, ALU.mult, ALU.mult), r=[bx, brs, bg], w=[bhn])
    return i


def norm_s3(c, i, ident, bid, out3, bout, tmp, tp, btp):
    PE, ACT = c.PE, c.ACT
    hn = tmp.hn[i]
    bhn = tmp.b[i][3]
    for k in range(8):
        PE.op(lambda e, k=k: e.transpose(tp[:, k * 128:(k + 1) * 128], hn[:, k * 128:(k + 1) * 128], ident),
              r=[bhn, bid], w=[btp], inc=(k == 7))
    ACT.op(lambda e: e.copy(out3, tp.rearrange("p (k t) -> p k t", k=8)), r=[btp], w=[bout])


def norm_T(c, xt, bx, g, bg, ident, bid, out3, bout, tmp, tp, btp):
    i = norm_s12(c, xt, bx, g, bg, tmp)
    norm_s3(c, i, ident, bid, out3, bout, tmp, tp, btp)


def phase1(c, S, x, mixg, w_in, ident_d, qkT_d, v_d):
    PE, ACT, DVE, POOL, SP = c.PE, c.ACT, c.DVE, c.POOL, c.SP
    mk = c.mark()
    NT = S // 512
    W = c.alloc([128, 8, 3072], BF16)
    bW = [Buf() for _ in range(8)]
    for k in range(8):
        POOL.dma(W[:, k, :], w_in[k * 128:(k + 1) * 128, :], w=[bW[k]])
    g = c.alloc([128, D], F32)
    bg = Buf()
    SP.dma(g, mixg.partition_broadcast(128), w=[bg])
    ident = c.alloc([128, 128], BF16)
    bid = Buf()
    SP.dma(ident, ident_d, w=[bid])
    tmp = NormTmp(c)
    xt = [c.alloc([128, D], F32) for _ in range(2)]
    bxt = [Buf() for _ in range(2)]
    hnT = [c.alloc([128, 8, 512], BF16) for _ in range(2)]
    bhnT = [[Buf() for _ in range(4)] for _ in range(2)]
    qk_st = [c.alloc([128, 16, 512], BF16) for _ in range(2)]
    bqk = [[Buf() for _ in range(16)] for _ in range(2)]
    v_st = [c.alloc([128, 4, D], BF16) for _ in range(2)]
    bv = [[Buf() for _ in range(8)] for _ in range(2)]
    tps = [c.psum[0][:, :].bitcast(BF16), c.psum[1][:, :].bitcast(BF16)]
    btps = [Buf(), Buf()]
    acc = [c.psum[2 + i][:, :] for i in range(4)]
    bacc = [Buf() for _ in range(4)]
    ai = 0
    xi = 0
    for tt in range(NT):
        p = tt % 2
        for tb in range(4):
            j = xi % 2
            xi += 1
            r0 = tt * 512 + tb * 128
            SP.dma(xt[j], x[r0:r0 + 128, :], w=[bxt[j]])
            norm_T(c, xt[j], bxt[j], g, bg, ident, bid, hnT[p][:, :, tb * 128:(tb + 1) * 128], bhnT[p][tb],
                   tmp, tps[j], btps[j])
        for oc in range(16):
            a = ai % 4
            ai += 1
            for k in range(8):
                PE.op(lambda e, a=a, k=k, oc=oc, p=p: e.matmul(acc[a], W[:, k, oc * 128:(oc + 1) * 128], hnT[p][:, k, :],
                                                               start=(k == 0), stop=(k == 7)),
                      r=[bW[k]] + bhnT[p], w=[bacc[a]], inc=(k == 7))
            sc = 0.125 if oc < 8 else 1.0
            if oc % 2 == 0:
                ACT.op(lambda e, a=a, oc=oc, p=p, sc=sc: e.mul(qk_st[p][:, oc, :], acc[a], sc), r=[bacc[a]], w=[bqk[p][oc]])
            else:
                DVE.op(lambda e, a=a, oc=oc, p=p, sc=sc: e.tensor_scalar(qk_st[p][:, oc, :], acc[a], sc, None, ALU.mult),
                       r=[bacc[a]], w=[bqk[p][oc]])
        SP.dma(qkT_d[:, :, tt * 512:(tt + 1) * 512].rearrange("c p t -> p c t"), qk_st[p], r=bqk[p])
        for tb in range(4):
            for hf in range(2):
                a = ai % 4
                ai += 1
                for k in range(8):
                    PE.op(lambda e, a=a, k=k, tb=tb, hf=hf, p=p: e.matmul(
                        acc[a], hnT[p][:, k, tb * 128:(tb + 1) * 128], W[:, k, 2048 + hf * 512:2048 + (hf + 1) * 512],
                        start=(k == 0), stop=(k == 7)),
                        r=[bW[k], bhnT[p][tb]], w=[bacc[a]], inc=(k == 7))
                if hf == 0:
                    ACT.op(lambda e, a=a, tb=tb, hf=hf, p=p: e.copy(v_st[p][:, tb, hf * 512:(hf + 1) * 512], acc[a]),
                           r=[bacc[a]], w=[bv[p][tb * 2 + hf]])
                else:
                    DVE.op(lambda e, a=a, tb=tb, hf=hf, p=p: e.tensor_copy(v_st[p][:, tb, hf * 512:(hf + 1) * 512], acc[a]),
                           r=[bacc[a]], w=[bv[p][tb * 2 + hf]])
        SP.dma(v_d[tt * 4:(tt + 1) * 4, :, :].rearrange("b p f -> p b f"), v_st[p], r=bv[p])
    c.barrier()
    c.release(mk)


def phase2(c, S, qkT_d, v_d, oT_d, ident_d, negtri_d, negones_d, maskb_d, heads=8, bg=None):
    PE, ACT, DVE, POOL, SP = c.PE, c.ACT, c.DVE, c.POOL, c.SP
    mk = c.mark()
    NQ = S // 512
    NB = S // 128
    ident = c.alloc([128, 128], BF16)
    negtri = c.alloc([128, 128], BF16)
    negones = c.alloc([128, 128], BF16)
    maskb = c.alloc([128, 4, 512], BF16)
    bcst = Buf()
    SP.dma(ident, ident_d, w=[bcst])
    SP.dma(negtri, negtri_d, w=[bcst])
    SP.dma(negones, negones_d, w=[bcst])
    SP.dma(maskb, maskb_d, w=[bcst])
    q2 = [c.alloc([128, 2, S], BF16) for _ in range(2)]
    k2 = [c.alloc([128, S], BF16) for _ in range(2)]
    v2 = [c.alloc([128, NB, 2, 128], BF16) for _ in range(2)]
    bq2 = [Buf() for _ in range(2)]
    bk2 = [Buf() for _ in range(2)]
    bv2 = [Buf() for _ in range(2)]
    for p in range(2):
        POOL.op(lambda e, p=p: e.memset(v2[p], 0.0), w=[bv2[p]])
        POOL.op(lambda e, p=p: e.memset(q2[p], 0.0), w=[bq2[p]])
    oT_st = [c.alloc([128, S], BF16) for _ in range(2)]
    boT = [Buf() for _ in range(2)]
    MAXI = 32
    mm = c.alloc([128, MAXI, 512], BF16)
    bm = [Buf() for _ in range(MAXI)]
    Sbs = c.alloc([128, MAXI, 512], BF16)
    bSb = [Buf() for _ in range(MAXI)]
    Sf = [c.alloc([128, 512], F32) for _ in range(2)]
    bSf = [Buf() for _ in range(2)]
    NA = 3
    aT = [c.alloc([128, 512], BF16) for _ in range(NA)]
    baT = [Buf() for _ in range(NA)]
    Z = [c.psum[i][:, :] for i in range(3)]
    bZ = [Buf() for _ in range(3)]
    E = [c.psum[3 + i][:, :] for i in range(3)]
    bE = [Buf() for _ in range(3)]
    O = [c.psum[6 + i][:, :] for i in range(2)]
    bO = [Buf() for _ in range(2)]
    cnt = {"z": 0, "e": 0, "a": 0}
    bg = list(bg) if bg else []
    nbatch_total = heads * sum((4 * qt + 4 + 15) // 16 for qt in range(NQ))
    bg_per = (len(bg) + nbatch_total - 1) // max(nbatch_total, 1)

    def nxt(key, n):
        v = cnt[key] % n
        cnt[key] += 1
        return v

    def load_hp(hp):
        p = hp % 2
        for e in range(2):
            SP.dma(q2[p][e * 64:(e + 1) * 64, e, :], qkT_d[hp][e * 64:(e + 1) * 64, :], w=[bq2[p]])
        SP.dma(k2[p], qkT_d[8 + hp], w=[bk2[p]])
        for e in range(2):
            SP.dma(v2[p][:, :, e, e * 64:(e + 1) * 64],
                   v_d[:, :, hp * 128 + e * 64: hp * 128 + (e + 1) * 64].rearrange("b p f -> p b f"), w=[bv2[p]])

    load_hp(0)
    for hp in range(heads):
        p = hp % 2
        for qt in range(NQ):
            if qt == 1 and hp + 1 < heads:
                load_hp(hp + 1)
            top = 4 * qt + 3
            o = qt % 2
            kbs = list(range(top, -1, -1))
            for b0 in range(0, len(kbs), 16):
                batch = kbs[b0:b0 + 16]
                items = [(kb, e) for kb in batch for e in range(2)]
                for _ in range(bg_per):
                    if bg:
                        bg.pop(0)()

                def qk(bank, bbank, kb, e, last_inc, p=p, qt=qt):
                    diag = kb >= 4 * qt
                    PE.op(lambda en: en.matmul(bank, k2[p][:, kb * 128:(kb + 1) * 128],
                                               q2[p][:, e, qt * 512:(qt + 1) * 512],
                                               start=True, stop=(last_inc and not diag)),
                          r=[bq2[p], bk2[p]], w=[bbank], inc=(last_inc and not diag))
                    if diag:
                        PE.op(lambda en: en.matmul(bank, ident, maskb[:, kb - 4 * qt, :], start=False, stop=last_inc),
                              r=[bcst], w=[bbank], inc=last_inc)

                for i, (kb, e) in enumerate(items):
                    z = nxt("z", 3)
                    qk(Z[z], bZ[z], kb, e, True)
                    ACT.op(lambda en, z=z, i=i: en.activation(mm[:, i, :], Z[z], AF.Softplus), r=[bZ[z]], w=[bm[i]])
                    if kb > 0:
                        eng = DVE if e == 0 else POOL
                        if kb == top:
                            eng.op(lambda en, i=i, e=e: en.tensor_copy(Sf[e], mm[:, i, :]), r=[bm[i]], w=[bSf[e]])
                        else:
                            eng.op(lambda en, i=i, e=e: en.tensor_tensor(Sf[e], Sf[e], mm[:, i, :], ALU.add),
                                   r=[bm[i], bSf[e]], w=[bSf[e]])
                        nslot = i + 2 if i + 2 < len(items) else e
                        DVE.op(lambda en, e=e, nslot=nslot: en.tensor_copy(Sbs[:, nslot, :], Sf[e]), r=[bSf[e]], w=[bSb[nslot]])
                pend = []

                def av(item, p=p, o=o, top=top):
                    kb, e, ai = item
                    first = (kb == top) and e == 0
                    last = kb == 0 and e == 1
                    PE.op(lambda en: en.matmul(O[o], v2[p][:, kb, e, :], aT[ai], start=first, stop=last),
                          r=[bv2[p], baT[ai]], w=[bO[o]], inc=last)
                    if last:
                        DVE.op(lambda en, p=p, qt=qt: en.tensor_copy(oT_st[p][:, qt * 512:(qt + 1) * 512], O[o]),
                               r=[bO[o]], w=[boT[p]])

                for i, (kb, e) in enumerate(items):
                    x = nxt("e", 3)
                    first_chain = kb == top
                    qk(E[x], bE[x], kb, e, False)
                    PE.op(lambda en, x=x, i=i, first_chain=first_chain: en.matmul(E[x], negtri, mm[:, i, :], start=False, stop=first_chain),
                          r=[bcst, bm[i]], w=[bE[x]], inc=first_chain)
                    if not first_chain:
                        PE.op(lambda en, x=x, i=i: en.matmul(E[x], negones, Sbs[:, i, :], start=False, stop=True),
                              r=[bcst, bSb[i]], w=[bE[x]], inc=True)
                    ai = nxt("a", NA)
                    ACT.op(lambda en, x=x, ai=ai: en.activation(aT[ai], E[x], AF.Exp), r=[bE[x]], w=[baT[ai]])
                    pend.append((kb, e, ai))
                    if len(pend) > 2:
                        av(pend.pop(0))
                while pend:
                    av(pend.pop(0))
        SP.dma(oT_d[:, :, hp, :].rearrange("t p c -> p t c"), oT_st[p].rearrange("p (t c) -> p t c", c=512), r=[boT[p]])
    while bg:
        bg.pop(0)()
    c.barrier()
    c.release(mk)


def rstd_of(c, xt, bx, junk, bj, ss, bss, rstd, brs):
    ACT = c.ACT
    ACT.op(lambda e: e.activation(junk, xt, AF.Square, scale=1.0 / 32.0, accum_out=ss), r=[bx], w=[bj, bss])
    ACT.op(lambda e: e.activation(rstd, ss, AF.Ln, bias=EPS), r=[bss], w=[brs])
    ACT.op(lambda e: e.activation(rstd, rstd, AF.Exp, scale=-0.5), r=[brs], w=[brs])


def phase_post(c, S, h_in, h_out, oT_d, KO, wb_out, g_mlp, wb_up, wb_down, g_ple, wb_gate, p_d, wb_pu, ident_d,
               g_final=None):
    PE, ACT, DVE, POOL, SP = c.PE, c.ACT, c.DVE, c.POOL, c.SP
    mk = c.mark()
    NT = S // 512
    ident = c.alloc([128, 128], BF16)
    bid = Buf()
    POOL.dma(ident, ident_d, w=[bid])
    gm = c.alloc([128, D], F32)
    gp = c.alloc([128, D], F32)
    bgm, bgp, bgf = Buf(), Buf(), Buf()
    POOL.dma(gm, g_mlp.partition_broadcast(128), w=[bgm])
    POOL.dma(gp, g_ple.partition_broadcast(128), w=[bgp])
    if g_final is not None:
        gf = c.alloc([128, D], F32)
        POOL.dma(gf, g_final.partition_broadcast(128), w=[bgf])
    tmp = NormTmp(c)
    NW = 3
    wr = [c.alloc([128, 8, 1024], BF16) for _ in range(NW)]
    bwr = [Buf() for _ in range(NW)]
    wi = [0]

    def wload(src3):
        s = wi[0] % NW
        wi[0] += 1
        nk = src3.shape[1]
        SP.dma(wr[s][:, 0:nk, :], src3, w=[bwr[s]])
        return wr[s], bwr[s]

    NH = 2
    ht_all = [[c.alloc([128, D], F32) for _ in range(4)] for _ in range(NH)]
    bht_all = [[Buf() for _ in range(4)] for _ in range(NH)]
    oTt = c.alloc([128, KO, 512], BF16)
    boTt = [Buf() for _ in range(KO // 8)]
    hnT = c.alloc([128, 8, 512], BF16)
    bhnT = [Buf() for _ in range(4)]
    aT = c.alloc([128, 32, 512], BF16)
    baT = [Buf() for _ in range(32)]
    rl = [c.alloc([128, 512], F32) for _ in range(2)]
    brl = [Buf() for _ in range(2)]
    gs = [c.alloc([128, 512], F32) for _ in range(2)]
    bgs = [Buf() for _ in range(2)]
    pt = c.alloc([128, 4, 256], F32)
    bpt = Buf()
    pb = c.alloc([128, 4, 256], BF16)
    bpb = Buf()
    pT = [c.alloc([128, 2, 128], BF16) for _ in range(4)]
    bpT = [Buf() for _ in range(4)]
    yo = [c.alloc([128, D], F32) for _ in range(2)]
    byo = [Buf() for _ in range(2)]
    tps = [c.psum[0][:, :].bitcast(BF16), c.psum[1][:, :].bitcast(BF16)]
    btps = [Buf(), Buf()]
    acc = [c.psum[2 + i][:, :] for i in range(6)]
    bacc = [Buf() for _ in range(6)]
    cnt = {"a": 0, "n": 0, "r": 0, "g": 0, "y": 0}

    def nxt(key, n):
        v = cnt[key] % n
        cnt[key] += 1
        return v

    for tt in range(NT):
        t0 = tt * 512
        ht = ht_all[tt % NH]
        bht = bht_all[tt % NH]
        for g in range(KO // 8):
            SP.dma(oTt[:, g * 8:(g + 1) * 8, :], oT_d[tt][:, g * 8:(g + 1) * 8, :], w=[boTt[g]])
        for tb in range(4):
            SP.dma(ht[tb], h_in[t0 + tb * 128:t0 + (tb + 1) * 128, :], w=[bht[tb]])
        for tb in range(4):
            SP.dma(pt[:, tb, :], p_d[t0 + tb * 128:t0 + (tb + 1) * 128, :], w=[bpt])

        def tok_mm(lhs3, blhs, nk, w3, bw, add, after_tb=None, ht=ht, bht=bht):
            for tb in range(4):
                for hf in range(2):
                    a = nxt("a", 6)
                    for k in range(nk):
                        PE.op(lambda e, a=a, k=k, tb=tb, hf=hf: e.matmul(
                            acc[a], lhs3[:, k, tb * 128:(tb + 1) * 128], w3[:, k, hf * 512:(hf + 1) * 512],
                            start=(k == 0), stop=(k == nk - 1)),
                            r=[bw] + blhs(tb), w=[bacc[a]], inc=(k == nk - 1))
                    add(tb, hf, a)
                if after_tb is not None:
                    after_tb(tb)

        def resid_add(tb, hf, a, ht=ht, bht=bht):
            DVE.op(lambda e: e.tensor_tensor(ht[tb][:, hf * 512:(hf + 1) * 512], ht[tb][:, hf * 512:(hf + 1) * 512],
                                             acc[a], ALU.add), r=[bacc[a], bht[tb]], w=[bht[tb]])

        DVE.op(lambda e: e.tensor_copy(pb, pt), r=[bpt], w=[bpb])
        for tb in range(4):
            j = nxt("n", 2)
            for k in range(2):
                PE.op(lambda e, k=k, tb=tb, j=j: e.transpose(tps[j][:, k * 128:(k + 1) * 128], pb[:, tb, k * 128:(k + 1) * 128], ident),
                      r=[bpb, bid], w=[btps[j]], inc=(k == 1))
            ACT.op(lambda e, tb=tb, j=j: e.copy(pT[tb], tps[j][:, 0:256].rearrange("p (k t) -> p k t", k=2)),
                   r=[btps[j]], w=[bpT[tb]])
        nidx = [None] * 4

        def mk_after(gg, gbuf):
            def after(tb, ht=ht, bht=bht):
                nidx[tb] = norm_s12(c, ht[tb], bht[tb], gg, gbuf, tmp)
            return after

        ng = KO // 8
        for g in range(ng):
            w3, bw = wload(wb_out[g * 1024:(g + 1) * 1024, :].rearrange("(k p) c -> p k c", p=128))
            tok_mm(oTt[:, g * 8:(g + 1) * 8, :], lambda tb, g=g: [boTt[g]], 8, w3, bw, resid_add,
                   after_tb=(mk_after(gm, bgm) if g == ng - 1 else None))
        for tb in range(4):
            j = nxt("n", 2)
            norm_s3(c, nidx[tb], ident, bid, hnT[:, :, tb * 128:(tb + 1) * 128], bhnT[tb], tmp, tps[j], btps[j])
        for cg in range(4):
            w3, bw = wload(wb_up[:, cg * 1024:(cg + 1) * 1024].rearrange("(k p) c -> p k c", p=128))
            for oc in range(8):
                a = nxt("a", 6)
                for k in range(8):
                    PE.op(lambda e, a=a, k=k, oc=oc, w3=w3: e.matmul(acc[a], w3[:, k, oc * 128:(oc + 1) * 128], hnT[:, k, :],
                                                                    start=(k == 0), stop=(k == 7)),
                          r=[bw] + bhnT, w=[bacc[a]], inc=(k == 7))
                r_ = nxt("r", 2)
                ACT.op(lambda e, a=a, r_=r_: e.activation(rl[r_], acc[a], AF.Relu), r=[bacc[a]], w=[brl[r_]])
                ff = cg * 8 + oc
                DVE.op(lambda e, r_=r_, ff=ff: e.tensor_tensor(aT[:, ff, :], rl[r_], rl[r_], ALU.mult),
                       r=[brl[r_]], w=[baT[ff]])
        for g in range(4):
            w3, bw = wload(wb_down[g * 1024:(g + 1) * 1024, :].rearrange("(k p) c -> p k c", p=128))
            tok_mm(aT[:, g * 8:(g + 1) * 8, :], lambda tb, g=g: baT[g * 8:(g + 1) * 8], 8, w3, bw, resid_add,
                   after_tb=(mk_after(gp, bgp) if g == 3 else None))
        wg3, bwg = wload(wb_gate.rearrange("(k p) c -> p k c", p=128))
        wp3, bwp = wload(wb_pu.rearrange("(k p) c -> p k c", p=128))
        for tb in range(4):
            j = nxt("n", 2)
            norm_s3(c, nidx[tb], ident, bid, hnT[:, :, tb * 128:(tb + 1) * 128], bhnT[tb], tmp, tps[j], btps[j])
        for tb in range(4):
            for hf in range(2):
                a = nxt("a", 6)
                for k in range(8):
                    PE.op(lambda e, a=a, k=k, tb=tb, hf=hf, wg3=wg3: e.matmul(
                        acc[a], hnT[:, k, tb * 128:(tb + 1) * 128], wg3[:, k, hf * 512:(hf + 1) * 512],
                        start=(k == 0), stop=(k == 7)), r=[bwg, bhnT[tb]], w=[bacc[a]], inc=(k == 7))
                gi = nxt("g", 2)
                ACT.op(lambda e, a=a, gi=gi: e.activation(gs[gi], acc[a], AF.Sigmoid), r=[bacc[a]], w=[bgs[gi]])
                a2 = nxt("a", 6)
                for k in range(2):
                    PE.op(lambda e, a2=a2, k=k, tb=tb, hf=hf, wp3=wp3: e.matmul(
                        acc[a2], pT[tb][:, k, :], wp3[:, k, hf * 512:(hf + 1) * 512],
                        start=(k == 0), stop=(k == 1)), r=[bwp, bpT[tb]], w=[bacc[a2]], inc=(k == 1))
                DVE.op(lambda e, a2=a2, gi=gi: e.tensor_tensor(gs[gi], gs[gi], acc[a2], ALU.mult),
                       r=[bacc[a2], bgs[gi]], w=[bgs[gi]])
                DVE.op(lambda e, gi=gi, tb=tb, hf=hf, ht=ht: e.tensor_tensor(
                    ht[tb][:, hf * 512:(hf + 1) * 512], ht[tb][:, hf * 512:(hf + 1) * 512], gs[gi], ALU.add),
                    r=[bgs[gi], bht[tb]], w=[bht[tb]])
            if g_final is None:
                POOL.dma(h_out[t0 + tb * 128:t0 + (tb + 1) * 128, :], ht[tb], r=[bht[tb]])
            else:
                i = tmp.i % tmp.n
                tmp.i += 1
                _, bss, brs, _ = tmp.b[i]
                bj = tmp.b[i % 2][0]
                rstd_of(c, ht[tb], bht[tb], tmp.junk[i % 2], bj, tmp.ss[i], bss, tmp.rstd[i], brs)
                yi = nxt("y", 2)
                DVE.op(lambda e, i=i, yi=yi, tb=tb, ht=ht: e.scalar_tensor_tensor(yo[yi], ht[tb], tmp.rstd[i], gf, ALU.mult, ALU.mult),
                       r=[bht[tb], brs, bgf], w=[byo[yi]])
                POOL.dma(h_out[t0 + tb * 128:t0 + (tb + 1) * 128, :], yo[yi], r=[byo[yi]])
    c.barrier()
    c.release(mk)


def phase_retpre(c, S, h_in, g_mix, wb_in, ident_d, cos_d, sin_d, dqk_d, qT_d, kT_d, ktok_d, v_d, gs_d):
    PE, ACT, DVE, POOL, SP = c.PE, c.ACT, c.DVE, c.POOL, c.SP
    mk = c.mark()
    NT = S // 512
    ident = c.alloc([128, 128], BF16)
    bid = Buf()
    POOL.dma(ident, ident_d, w=[bid])
    gm = c.alloc([128, D], F32)
    bgm = Buf()
    POOL.dma(gm, g_mix.partition_broadcast(128), w=[bgm])
    dqk = c.alloc([128, 2, 4, 512], F32)
    bdqk = Buf()
    POOL.dma(dqk[:, 0], dqk_d[:, 0], w=[bdqk])
    POOL.dma(dqk[:, 1], dqk_d[:, 1], w=[bdqk])
    tmp = NormTmp(c)
    NW = 3
    wr = [c.alloc([128, 8, 1024], BF16) for _ in range(NW)]
    bwr = [Buf() for _ in range(NW)]
    wi = [0]

    def wload(src3):
        s = wi[0] % NW
        wi[0] += 1
        SP.dma(wr[s], src3, w=[bwr[s]])
        return wr[s], bwr[s]

    ht = [c.alloc([128, D], F32) for _ in range(2)]
    bht = [Buf() for _ in range(2)]
    hnT = c.alloc([128, 8, 512], BF16)
    bhnT = [Buf() for _ in range(4)]
    cs = [c.alloc([128, 2, 512], F32) for _ in range(2)]
    bcs = [Buf() for _ in range(2)]
    x12 = [c.alloc([128, 2, 512], F32) for _ in range(2)]
    bx12 = [[Buf(), Buf()] for _ in range(2)]
    tA = [c.alloc([128, 3, 512], F32)] * 2
    btA = [Buf()] * 2
    tB = [c.alloc([128, 3, 512], F32)] * 2
    btB = [Buf()] * 2
    qk_st = [c.alloc([128, 8, 512], BF16) for _ in range(2)]
    bqk = [[Buf() for _ in range(8)] for _ in range(2)]
    ktok_st = c.alloc([128, 4, 1024], BF16)
    bktok = [Buf() for _ in range(4)]
    vg_st = [c.alloc([128, 4, 2048], BF16) for _ in range(2)]
    bvg = [[Buf() for _ in range(16)] for _ in range(2)]
    tps = [c.psum[0][:, :].bitcast(BF16), c.psum[1][:, :].bitcast(BF16)]
    btps = [Buf(), Buf()]
    acc = [c.psum[2 + i][:, :] for i in range(6)]
    bacc = [Buf() for _ in range(6)]
    cnt = {"a": 0, "n": 0, "x": 0, "h": 0}

    def nxt(key, n):
        v = cnt[key] % n
        cnt[key] += 1
        return v

    for tt in range(NT):
        t0 = tt * 512
        ci = tt % 2
        POOL.dma(cs[ci][:, 0, :], cos_d[:, t0:t0 + 512], w=[bcs[ci]])
        POOL.dma(cs[ci][:, 1, :], sin_d[:, t0:t0 + 512], w=[bcs[ci]])
        for tb in range(4):
            hi = nxt("h", 2)
            POOL.dma(ht[hi], h_in[t0 + tb * 128:t0 + (tb + 1) * 128, :], w=[bht[hi]])
            j = nxt("n", 2)
            norm_T(c, ht[hi], bht[hi], gm, bgm, ident, bid, hnT[:, :, tb * 128:(tb + 1) * 128], bhnT[tb], tmp,
                   tps[j], btps[j])
        cos_t = cs[ci][:, 0, :]
        sin_t = cs[ci][:, 1, :]
        for which in range(2):
            w3, bw = wload(wb_in[:, which * 1024:(which + 1) * 1024].rearrange("(k p) c -> p k c", p=128))
            st = qk_st[which]
            for h in range(4):
                xi = nxt("x", 2)
                for half in range(2):
                    oc = 2 * h + half
                    a = nxt("a", 6)
                    for k in range(8):
                        PE.op(lambda e, a=a, k=k, oc=oc, w3=w3: e.matmul(acc[a], w3[:, k, oc * 128:(oc + 1) * 128], hnT[:, k, :],
                                                                        start=(k == 0), stop=(k == 7)),
                              r=[bw] + bhnT, w=[bacc[a]], inc=(k == 7))
                    ACT.op(lambda e, a=a, xi=xi, half=half: e.copy(x12[xi][:, half, :], acc[a]),
                           r=[bacc[a]], w=[bx12[xi][half]])
                x1 = x12[xi][:, 0, :]
                x2 = x12[xi][:, 1, :]
                dtab = dqk[:, which, h, :]
                ta, tb_ = tA[xi], tB[xi]
                rd = [bx12[xi][0], bx12[xi][1], bcs[ci], bdqk]
                DVE.op(lambda e, ta=ta, x1=x1, cos_t=cos_t: e.tensor_tensor(ta[:, 0, :], x1, cos_t, ALU.mult), r=rd, w=[btA[xi]])
                DVE.op(lambda e, ta=ta, x2=x2, sin_t=sin_t: e.tensor_tensor(ta[:, 1, :], x2, sin_t, ALU.mult), r=rd, w=[btA[xi]])
                DVE.op(lambda e, ta=ta: e.tensor_tensor(ta[:, 2, :], ta[:, 0, :], ta[:, 1, :], ALU.subtract), r=[btA[xi]], w=[btA[xi]])
                DVE.op(lambda e, ta=ta, st=st, h=h, dtab=dtab: e.tensor_tensor(st[:, 2 * h, :], ta[:, 2, :], dtab, ALU.mult),
                       r=[btA[xi], bdqk], w=[bqk[which][2 * h]])
                POOL.op(lambda e, tb_=tb_, x1=x1, sin_t=sin_t: e.tensor_tensor(tb_[:, 0, :], x1, sin_t, ALU.mult), r=rd, w=[btB[xi]])
                POOL.op(lambda e, tb_=tb_, x2=x2, cos_t=cos_t: e.tensor_tensor(tb_[:, 1, :], x2, cos_t, ALU.mult), r=rd, w=[btB[xi]])
                POOL.op(lambda e, tb_=tb_: e.tensor_tensor(tb_[:, 2, :], tb_[:, 0, :], tb_[:, 1, :], ALU.add), r=[btB[xi]], w=[btB[xi]])
                POOL.op(lambda e, tb_=tb_, st=st, h=h, dtab=dtab: e.tensor_tensor(st[:, 2 * h + 1, :], tb_[:, 2, :], dtab, ALU.mult),
                        r=[btB[xi], bdqk], w=[bqk[which][2 * h + 1]])
            dst = qT_d if which == 0 else kT_d
            POOL.dma(dst[tt], st, r=bqk[which])
            if which == 1:
                for tb in range(4):
                    j = nxt("n", 2)
                    for k in range(8):
                        PE.op(lambda e, k=k, tb=tb, j=j, st=st: e.transpose(tps[j][:, k * 128:(k + 1) * 128],
                                                                     st[:, k, tb * 128:(tb + 1) * 128], ident),
                              r=[bqk[1][k], bid], w=[btps[j]], inc=(k == 7))
                    ACT.op(lambda e, tb=tb, j=j: e.copy(ktok_st[:, tb, :], tps[j]), r=[btps[j]], w=[bktok[tb]])
                for tb in range(4):
                    POOL.dma(ktok_d[tt * 4 + tb], ktok_st[:, tb, :], r=[bktok[tb]])
        for which in range(2):
            st = vg_st[which]
            for g in range(2):
                c0 = 2048 + which * 2048 + g * 1024
                w3, bw = wload(wb_in[:, c0:c0 + 1024].rearrange("(k p) c -> p k c", p=128))
                for tb in range(4):
                    for hf in range(2):
                        a = nxt("a", 6)
                        for k in range(8):
                            PE.op(lambda e, a=a, k=k, tb=tb, hf=hf, w3=w3: e.matmul(
                                acc[a], hnT[:, k, tb * 128:(tb + 1) * 128], w3[:, k, hf * 512:(hf + 1) * 512],
                                start=(k == 0), stop=(k == 7)), r=[bw, bhnT[tb]], w=[bacc[a]], inc=(k == 7))
                        col = g * 1024 + hf * 512
                        bi = tb * 4 + g * 2 + hf
                        if which == 1:
                            ACT.op(lambda e, a=a, tb=tb, col=col, st=st: e.activation(st[:, tb, col:col + 512], acc[a], AF.Silu),
                                   r=[bacc[a]], w=[bvg[which][bi]])
                        else:
                            DVE.op(lambda e, a=a, tb=tb, col=col, st=st: e.tensor_copy(st[:, tb, col:col + 512], acc[a]),
                                   r=[bacc[a]], w=[bvg[which][bi]])
            dst = v_d if which == 0 else gs_d
            for tb in range(4):
                POOL.dma(dst[tt * 4 + tb], st[:, tb, :], r=bvg[which][tb * 4:(tb + 1) * 4])
    c.barrier()
    c.release(mk)


def phase_ret(c, S, qT_d, kT_d, ktok_d, v_d, gs_d, ident_d, cmask_d, yT_d, g128):
    PE, ACT, DVE, POOL, SP = c.PE, c.ACT, c.DVE, c.POOL, c.SP
    mk = c.mark()
    NC = S // 128
    ident = c.alloc([128, 128], BF16)
    cmask = c.alloc([128, 128], F32)
    bcst = Buf()
    SP.dma(ident, ident_d, w=[bcst])
    SP.dma(cmask, cmask_d, w=[bcst])
    NB_ = 3
    qTc = [c.alloc([128, 8, 128], BF16) for _ in range(NB_)]
    kTc = [c.alloc([128, 8, 128], BF16) for _ in range(NB_)]
    ktc = [c.alloc([128, 1024], BF16) for _ in range(NB_)]
    vc = [c.alloc([128, 2048], BF16) for _ in range(NB_)]
    gc = [c.alloc([128, 2048], BF16) for _ in range(NB_)]
    bin_ = [[Buf() for _ in range(5)] for _ in range(NB_)]
    U = c.alloc([128, 4, 2, 512], F32)
    bU = [[Buf(), Buf()] for _ in range(4)]
    Tb = [c.alloc([128, 4, 2, 512], BF16) for _ in range(2)]
    bTb = [[[Buf(), Buf()] for _ in range(4)] for _ in range(2)]
    sT = [c.alloc([128, 128], BF16) for _ in range(4)]
    bsT = [Buf() for _ in range(4)]
    junk = [c.alloc([128, 512], BF16) for _ in range(2)]
    ss = [c.alloc([128, 1], F32) for _ in range(4)]
    rstd = [c.alloc([128, 1], F32) for _ in range(4)]
    bnrm = [[Buf(), Buf(), Buf()] for _ in range(4)]
    yg = [c.alloc([128, 512], BF16) for _ in range(2)]
    byg = [Buf() for _ in range(2)]
    yT_st = [c.alloc([128, 16, 512], BF16) for _ in range(2)]
    byT = [[Buf() for _ in range(16)] for _ in range(2)]
    scbk = [c.psum[0][:, 0:128], c.psum[1][:, 0:128]]
    bsck = [Buf(), Buf()]
    outp = [c.psum[2][:, :], c.psum[3][:, :]]
    bout = [Buf(), Buf()]
    dS = [c.psum[4 + i][:, :] for i in range(2)]
    bdS = [Buf() for _ in range(2)]
    tpb = [c.psum[6][:, :].bitcast(BF16)[:, 0:512], c.psum[7][:, :].bitcast(BF16)[:, 0:512]]
    btp = [Buf(), Buf()]
    cnt = {"o": 0, "d": 0, "t": 0, "y": 0, "j": 0}

    def nxt(key, n):
        v = cnt[key] % n
        cnt[key] += 1
        return v

    pend_t = []
    for cb in range(NC):
        b = cb % NB_
        t0 = cb * 128
        tile_i = (cb // 4) % 2
        tb = cb % 4
        SP.dma(qTc[b], qT_d[cb // 4][:, :, tb * 128:(tb + 1) * 128], w=[bin_[b][0]])
        SP.dma(kTc[b], kT_d[cb // 4][:, :, tb * 128:(tb + 1) * 128], w=[bin_[b][1]])
        SP.dma(ktc[b], ktok_d[cb], w=[bin_[b][2]])
        SP.dma(vc[b], v_d[cb], w=[bin_[b][3]])
        SP.dma(gc[b], gs_d[cb], w=[bin_[b][4]])
        par = cb % 2
        for h in range(4):
            for dc in range(2):
                PE.op(lambda e, h=h, dc=dc, b=b: e.matmul(scbk[h % 2], kTc[b][:, 2 * h + dc, :], qTc[b][:, 2 * h + dc, :],
                                                          start=(dc == 0), stop=(dc == 1)),
                      r=[bin_[b][0], bin_[b][1]], w=[bsck[h % 2]], inc=(dc == 1))
            DVE.op(lambda e, h=h: e.tensor_tensor(sT[h], scbk[h % 2], cmask, ALU.mult), r=[bsck[h % 2], bcst], w=[bsT[h]])
            o = nxt("o", 2)
            vh = vc[b][:, h * 512:(h + 1) * 512]
            PE.op(lambda e, h=h, o=o, vh=vh: e.matmul(outp[o], sT[h], vh, start=True, stop=(cb == 0)),
                  r=[bsT[h], bin_[b][3]], w=[bout[o]], inc=(cb == 0))
            if cb > 0:
                for dc in range(2):
                    PE.op(lambda e, h=h, dc=dc, o=o, b=b, par=par: e.matmul(
                        outp[o], qTc[b][:, 2 * h + dc, :], Tb[par][:, h, dc, :], start=False, stop=(dc == 1)),
                        r=[bin_[b][0], bTb[par][h][dc]], w=[bout[o]], inc=(dc == 1))
            ji = nxt("j", 2)
            bj, bss, brs = bnrm[h]
            ACT.op(lambda e, h=h, o=o, ji=ji: e.activation(junk[ji], outp[o], AF.Square, scale=float(512 ** -0.5), accum_out=ss[h]),
                   r=[bout[o]], w=[bj, bss])
            ACT.op(lambda e, h=h: e.activation(rstd[h], ss[h], AF.Ln, bias=EPS), r=[bss], w=[brs])
            ACT.op(lambda e, h=h: e.activation(rstd[h], rstd[h], AF.Exp, scale=-0.5), r=[brs], w=[brs])
            y = nxt("y", 2)
            gh = gc[b][:, h * 512:(h + 1) * 512]
            DVE.op(lambda e, h=h, o=o, y=y, gh=gh: e.scalar_tensor_tensor(yg[y], outp[o], rstd[h], gh, ALU.mult, ALU.mult),
                   r=[bout[o], brs, bin_[b][4]], w=[byg[y]])
            if cb < NC - 1:
                for dc in range(2):
                    d = nxt("d", 2)
                    PE.op(lambda e, h=h, dc=dc, d=d, b=b, vh=vh: e.matmul(
                        dS[d], ktc[b][:, (2 * h + dc) * 128:(2 * h + dc + 1) * 128], vh, start=True, stop=True),
                        r=[bin_[b][2], bin_[b][3]], w=[bdS[d]], inc=True)
                    if cb == 0:
                        DVE.op(lambda e, h=h, dc=dc, d=d: e.tensor_copy(U[:, h, dc, :], dS[d]), r=[bdS[d]], w=[bU[h][dc]])
                    else:
                        DVE.op(lambda e, h=h, dc=dc, d=d: e.scalar_tensor_tensor(
                            U[:, h, dc, :], U[:, h, dc, :], float(g128[h]), dS[d], ALU.mult, ALU.add),
                            r=[bdS[d], bU[h][dc]], w=[bU[h][dc]])
                    ACT.op(lambda e, h=h, dc=dc, par=par: e.mul(Tb[1 - par][:, h, dc, :], U[:, h, dc, :], float(g128[h])),
                           r=[bU[h][dc]], w=[bTb[1 - par][h][dc]])
            if pend_t:
                pend_t.pop(0)()

            def tr(h=h, y=y, tile_i=tile_i, tb=tb):
                t = nxt("t", 2)
                for ec in range(4):
                    PE.op(lambda e, ec=ec, y=y, t=t: e.transpose(tpb[t][:, ec * 128:(ec + 1) * 128], yg[y][:, ec * 128:(ec + 1) * 128], ident),
                          r=[byg[y], bcst], w=[btp[t]], inc=(ec == 3))
                ACT.op(lambda e, h=h, t=t, tile_i=tile_i, tb=tb: e.copy(
                    yT_st[tile_i][:, h * 4:(h + 1) * 4, tb * 128:(tb + 1) * 128], tpb[t].rearrange("p (k n) -> p k n", k=4)),
                    r=[btp[t]], w=[byT[tile_i][h * 4 + tb]])
            pend_t.append(tr)
        if tb == 3:
            while pend_t:
                pend_t.pop(0)()
        if tb == 3:
            for g in range(2):
                POOL.dma(yT_d[cb // 4][:, g * 8:(g + 1) * 8, :], yT_st[tile_i][:, g * 8:(g + 1) * 8, :], r=byT[tile_i])
    c.barrier()
    c.release(mk)


def _consts(S):
    import ml_dtypes
    bf = ml_dtypes.bfloat16
    f32 = np.float32
    j = np.arange(128)
    col = np.arange(512)
    maskb = np.zeros((128, 4, 512), f32)
    for cc in range(4):
        maskb[:, cc, :] = np.where(col[None, :] > cc * 128 + j[:, None], 0.0, -30000.0)
    i = np.arange(128, dtype=f32)
    inv = (f32(10000.0) ** (-(2 * i) / f32(256))).astype(f32)
    pos = np.arange(S, dtype=f32)
    ang = (inv[:, None] * pos[None, :]).astype(f32)
    h = np.arange(4, dtype=np.float64)
    lg = np.log1p(-np.exp2(-5.0 - h))
    n = (np.arange(512) % 128).astype(np.float64)
    dq = np.exp(lg[:, None] * (n[None, :] + 1))
    dk = np.exp(-lg[:, None] * (n[None, :] + 1)) / 16.0
    dqk = np.ascontiguousarray(np.broadcast_to(np.stack([dq, dk], 0).astype(f32)[None], (128, 2, 4, 512)))
    return dict(
        c_ident=np.eye(128, dtype=f32).astype(bf),
        c_negtri=(-(j[:, None] >= j[None, :]).astype(f32)).astype(bf),
        c_negones=(-np.ones((128, 128), f32)).astype(bf),
        c_maskb=maskb.astype(bf),
        c_cmask=(j[:, None] <= j[None, :]).astype(f32),
        c_cos=np.cos(ang).astype(f32), c_sin=np.sin(ang).astype(f32),
        c_dqk=dqk,
    ), [float(v) for v in np.exp(lg * 128)]


WSPEC = [("sb_w_in", [1024, 3072]), ("sb_w_out", [1024, 1024]), ("ret_w_in", [1024, 6144]), ("ret_w_out", [2048, 1024]),
         ("mlp_w_up0", [1024, 4096]), ("mlp_w_down0", [4096, 1024]), ("ple_w_gate0", [1024, 1024]), ("ple_w_up0", [256, 1024]),
         ("mlp_w_up1", [1024, 4096]), ("mlp_w_down1", [4096, 1024]), ("ple_w_gate1", [1024, 1024]), ("ple_w_up1", [256, 1024])]
GSPEC = ["mix_norm0", "mix_norm1", "mlp_norm0", "mlp_norm1", "ple_norm0", "ple_norm1", "final_norm"]


def phase_cast(c, wf, wb, names):
    out = []
    for nm in names:
        src, dst = wf[nm], wb[nm]
        R, C = src.shape
        cw = min(C, 2048)
        for r0 in range(0, R, 128):
            for c0 in range(0, C, cw):
                out.append(lambda d=dst[r0:r0 + 128, c0:c0 + cw], s_=src[r0:r0 + 128, c0:c0 + cw]: c.POOL.dma(d, s_))
    return out


def build(S, g128):
    from contextlib import ExitStack
    nc = bass.Bass("TRN2", target_bir_lowering=False)
    I = lambda n, s, d: nc.dram_tensor(n, s, d, kind="ExternalInput").ap()
    T = lambda n, s, d: nc.dram_tensor(n, s, d).ap()
    x = I("x", [S, 1024], F32)
    p0 = I("p0", [S, 256], F32)
    p1 = I("p1", [S, 256], F32)
    wf = {nm: I(nm, shp, F32) for nm, shp in WSPEC}
    gv = {nm: I(nm, [1024], F32) for nm in GSPEC}
    ident = I("c_ident", [128, 128], BF16)
    negtri = I("c_negtri", [128, 128], BF16)
    negones = I("c_negones", [128, 128], BF16)
    maskb = I("c_maskb", [128, 4, 512], BF16)
    cmask = I("c_cmask", [128, 128], F32)
    cos = I("c_cos", [128, S], F32)
    sin = I("c_sin", [128, S], F32)
    dqk = I("c_dqk", [128, 2, 4, 512], F32)
    y = nc.dram_tensor("y", [S, 1024], F32, kind="ExternalOutput").ap()
    wb = {nm: T("b_" + nm, shp, BF16) for nm, shp in WSPEC if nm != "sb_w_in"}
    NB = S // 128
    qkT = T("s_qkT", [16, 128, S], BF16)
    v0 = T("s_v0", [NB, 128, 1024], BF16)
    oT = T("s_oT", [S // 512, 128, 8, 512], BF16)
    h1 = T("s_h1", [S, 1024], F32)
    qT1 = T("s_qT1", [S // 512, 128, 8, 512], BF16)
    kT1 = T("s_kT1", [S // 512, 128, 8, 512], BF16)
    ktok = T("s_ktok", [NB, 128, 1024], BF16)
    v1 = T("s_v1", [NB, 128, 2048], BF16)
    gs1 = T("s_gs1", [NB, 128, 2048], BF16)
    yT = T("s_yT", [S // 512, 128, 16, 512], BF16)
    with ExitStack() as st:
        c = Ctx(nc, st)
        order = ["sb_w_out", "mlp_w_up0", "mlp_w_down0", "ple_w_gate0", "ple_w_up0", "ret_w_in", "ret_w_out",
                 "mlp_w_up1", "mlp_w_down1", "ple_w_gate1", "ple_w_up1"]
        casts = phase_cast(c, wf, wb, order)
        phase1(c, S, x, gv["mix_norm0"], wf["sb_w_in"], ident, qkT, v0)
        phase2(c, S, qkT, v0, oT, ident, negtri, negones, maskb, bg=casts)
        phase_post(c, S, x, h1, oT, 8, wb["sb_w_out"], gv["mlp_norm0"], wb["mlp_w_up0"], wb["mlp_w_down0"],
                   gv["ple_norm0"], wb["ple_w_gate0"], p0, wb["ple_w_up0"], ident)
        phase_retpre(c, S, h1, gv["mix_norm1"], wb["ret_w_in"], ident, cos, sin, dqk, qT1, kT1, ktok, v1, gs1)
        phase_ret(c, S, qT1, kT1, ktok, v1, gs1, ident, cmask, yT, g128)
        phase_post(c, S, h1, y, yT, 16, wb["ret_w_out"], gv["mlp_norm1"], wb["mlp_w_up1"], wb["mlp_w_down1"],
                   gv["ple_norm1"], wb["ple_w_gate1"], p1, wb["ple_w_up1"], ident, g_final=gv["final_norm"])
        c.emit()
    return nc


def _core_inputs(b, S, x, p, mix_norm, sb_w_in, sb_w_out, ret_w_in, ret_w_out, mlp_norm, mlp_w_up, mlp_w_down,
                 ple_norm, ple_w_gate, ple_w_up, final_norm, consts):
    A = lambda a: np.ascontiguousarray(np.asarray(a, dtype=np.float32))
    m = dict(consts)
    m["x"] = A(x[b, :S])
    m["p0"] = A(p[0, b, :S])
    m["p1"] = A(p[1, b, :S])
    m["sb_w_in"] = A(sb_w_in[0]); m["sb_w_out"] = A(sb_w_out[0])
    m["ret_w_in"] = A(ret_w_in[0]); m["ret_w_out"] = A(ret_w_out[0])
    for l in range(2):
        m["mlp_w_up%d" % l] = A(mlp_w_up[l]); m["mlp_w_down%d" % l] = A(mlp_w_down[l])
        m["ple_w_gate%d" % l] = A(ple_w_gate[l]); m["ple_w_up%d" % l] = A(ple_w_up[l])
        m["mix_norm%d" % l] = A(mix_norm[l]); m["mlp_norm%d" % l] = A(mlp_norm[l]); m["ple_norm%d" % l] = A(ple_norm[l])
    m["final_norm"] = A(final_norm)
    return m


def kernel(x, p, mix_norm, sb_w_in, sb_w_out, ret_w_in, ret_w_out, mlp_norm, mlp_w_up, mlp_w_down,
           ple_norm, ple_w_gate, ple_w_up, final_norm):
    x = np.asarray(x)
    B, S = x.shape[0], x.shape[1]
    consts, g128 = _consts(S)
    nc = build(S, g128)
    args = [np.asarray(a) for a in (p, mix_norm, sb_w_in, sb_w_out, ret_w_in, ret_w_out, mlp_norm, mlp_w_up, mlp_w_down,
                                    ple_norm, ple_w_gate, ple_w_up, final_norm)]
    in_maps = [_core_inputs(b, S, x, *args, consts) for b in range(B)]
    res = run_bass_kernel_spmd(nc, in_maps, core_ids=list(range(B)))
    return np.stack([np.asarray(r["y"], dtype=np.float32) for r in res.results], axis=0)
```
